# Optimizing a Trainium2 kernel written in Bass

```python
import math
import jax, jax.numpy as jnp
from jax import lax
import numpy as np

D_MODEL = 1024
BATCH = 4
SEQ = 4096
DEPTH = 1
DEC_BATCH = 32
DEC_SEQ = 1
PAST_LEN = 16384
PAGE_SIZE = 128

D_SSM = 512
SSM_GROUP = 16
N_SSM_GROUPS = D_SSM // SSM_GROUP
SSM_STATE = 64
N_HEADS = 8
HEAD_DIM = 64
N_KV_HEADS = 2
GQA = N_HEADS // N_KV_HEADS
D_ATT = N_HEADS * HEAD_DIM
D_KV = 2 * N_KV_HEADS * HEAD_DIM
CMP_STRIDE = 16
CMP_BLOCK = 2 * CMP_STRIDE
SEL_BLOCK = 64
N_SEL = 16
WINDOW = 512
Q_BLOCK = 128
NUM_BUCKETS = 32
REL_MAX_DIST = 1024
N_EXPERTS = 32
TOP_K = 4
D_FF = 1024
SWIGLU_LIMIT = 7.0
SWIGLU_ALPHA = 1.702
MOE_BLOCK_MAX = 256
DN_ALPHA = (2 * DEPTH) ** 0.25
DN_BETA = (8 * DEPTH) ** -0.25
D_IN = D_SSM + D_ATT + 3 * D_KV + 3 * N_HEADS
NEG = -1e30
F32 = jnp.float32

kernel_name = "hymba_s5_nsa_moe_decode_step"


def layer_norm(x, eps=1e-5):
    xf = x.astype(F32)
    mu = xf.mean(-1, keepdims=True)
    var = jnp.mean(jnp.square(xf - mu), -1, keepdims=True)
    return (xf - mu) * lax.rsqrt(var + eps)


def modulate(x, shift, scale):
    return (layer_norm(x) * (1.0 + scale.astype(F32)) + shift.astype(F32)).astype(x.dtype)


def post_norm(x, gate, y, g, b):
    z = DN_ALPHA * x + gate * y
    return (layer_norm(z) * g.astype(F32) + b.astype(F32)).astype(x.dtype)


def adaln(c, w, b):
    m = (jax.nn.silu(c) @ w + b)[:, None, :]
    return jnp.split(m, 6, axis=-1)


def rel_bucket(dist):
    n = jnp.maximum(dist, 0)
    max_exact = NUM_BUCKETS // 2
    nf = jnp.maximum(n, 1).astype(F32)
    large = max_exact + (jnp.log(nf / max_exact) / math.log(REL_MAX_DIST / max_exact) * (NUM_BUCKETS - max_exact)).astype(jnp.int32)
    large = jnp.minimum(large, NUM_BUCKETS - 1)
    return jnp.where(n < max_exact, n, large)


def masked_softmax(s, mask):
    s = jnp.where(mask, s.astype(F32), NEG)
    m = jnp.max(s, -1, keepdims=True)
    p = jnp.where(mask, jnp.exp(s - m), 0.0)
    return p / jnp.maximum(p.sum(-1, keepdims=True), 1e-30)


def shared_bias(table, dist):
    bt = table[rel_bucket(dist)]
    return jnp.moveaxis(bt, -1, 0).reshape(N_KV_HEADS, GQA, *dist.shape).astype(F32)


def attend_shared(q, k, v, q_pos, k_pos, mask, table):
    s = jnp.einsum('bhgqd,bkhd->bhgqk', q, k).astype(F32) * HEAD_DIM ** -0.5
    s = s + shared_bias(table, q_pos[:, None] - k_pos[None, :])
    p = masked_softmax(s, mask)
    return jnp.einsum('bhgqk,bkhd->bhgqd', p.astype(v.dtype), v), p


def compress_kv(kv, phi_pe, phi_w1, phi_b1, phi_w2, phi_b2):
    B, L = kv.shape[:2]
    ch = kv.reshape(B, L // CMP_STRIDE, CMP_STRIDE, 2, N_KV_HEADS, HEAD_DIM)
    pe = jnp.transpose(phi_pe.reshape(2, 2, CMP_STRIDE, HEAD_DIM), (1, 2, 0, 3))[:, :, :, None, :]
    w1 = phi_w1.reshape(2, 2, CMP_STRIDE, HEAD_DIM, HEAD_DIM)
    first = jnp.einsum('bnjchd,cjde->bnche', ch + pe[0], w1[:, 0])
    second = jnp.einsum('bnjchd,cjde->bnche', ch + pe[1], w1[:, 1])
    hdn = jax.nn.gelu(first[:, :-1] + second[:, 1:] + phi_b1[:, None, :])
    return jnp.einsum('bnche,cef->bnchf', hdn, phi_w2) + phi_b2[:, None, :]


def attend_compressed(q, ckv, q_pos, table):
    nc = ckv.shape[1]
    c_end = jnp.arange(nc) * CMP_STRIDE + CMP_BLOCK - 1
    mask = c_end[None, :] <= q_pos[:, None]
    return attend_shared(q, ckv[:, :, 0], ckv[:, :, 1], q_pos, c_end, mask, table)


def select_blocks(p_cmp, q_pos, n_blocks):
    imp = p_cmp.sum(2)
    r = SEL_BLOCK // CMP_STRIDE
    padded = jnp.pad(imp, ((0, 0), (0, 0), (0, 0), (1, 2)))
    s = padded[..., :n_blocks * r].reshape(*imp.shape[:3], n_blocks, r).sum(-1) + padded[..., r:n_blocks * r + 1:r]
    blk = jnp.arange(n_blocks)[None, :]
    cur = (q_pos // SEL_BLOCK)[:, None]
    causal = blk <= cur
    forced = (blk == 0) | (blk == cur) | (blk == cur - 1)
    s = jnp.where(forced & causal, 1e4, jnp.where(causal, s, -1.0))
    _, idx = lax.top_k(s, min(N_SEL, n_blocks))
    return idx, idx <= cur


def gather_blocks_contig(kv, idx):
    B, L = kv.shape[:2]
    blocks = kv.reshape(B, L // SEL_BLOCK, SEL_BLOCK, 2, N_KV_HEADS, HEAD_DIM)
    b = jnp.arange(B)[:, None, None, None]
    h = jnp.arange(N_KV_HEADS)[None, :, None, None]
    return blocks[b, idx, :, :, h]


def gather_blocks_paged(pool, page_table, new_kv, idx):
    B = idx.shape[0]
    bpp = PAGE_SIZE // SEL_BLOCK
    n_past = page_table.shape[1] * bpp
    pages = pool.reshape(pool.shape[0], bpp, SEL_BLOCK, 2, N_KV_HEADS, HEAD_DIM)
    b = jnp.arange(B)[:, None, None, None]
    h = jnp.arange(N_KV_HEADS)[None, :, None, None]
    past_idx = jnp.minimum(idx, n_past - 1)
    phys = page_table[b, past_idx // bpp]
    past = pages[phys, past_idx % bpp, :, :, h]
    tn = new_kv.shape[1]
    n_new = -(-tn // SEL_BLOCK)
    new_blocks = jnp.pad(new_kv, ((0, 0), (0, n_new * SEL_BLOCK - tn), (0, 0), (0, 0), (0, 0))).reshape(B, n_new, SEL_BLOCK, 2, N_KV_HEADS, HEAD_DIM)
    new = new_blocks[b, jnp.clip(idx - n_past, 0, n_new - 1), :, :, h]
    return jnp.where((idx < n_past)[..., None, None, None], past, new)


def attend_selected(q, kv_sel, idx, valid, q_pos, table):
    B, H, Tq, ns = idx.shape
    k = kv_sel[..., 0, :].reshape(B, H, Tq, ns * SEL_BLOCK, HEAD_DIM)
    v = kv_sel[..., 1, :].reshape(B, H, Tq, ns * SEL_BLOCK, HEAD_DIM)
    k_pos = (idx[..., None] * SEL_BLOCK + jnp.arange(SEL_BLOCK)).reshape(B, H, Tq, ns * SEL_BLOCK)
    dist = q_pos[:, None] - k_pos
    mask = jnp.repeat(valid, SEL_BLOCK, axis=-1) & (dist >= 0)
    tbl = jnp.transpose(table.reshape(NUM_BUCKETS, N_KV_HEADS, GQA), (1, 0, 2))
    bias = tbl[jnp.arange(N_KV_HEADS)[None, :, None, None], rel_bucket(dist)]
    s = jnp.einsum('bhgqd,bhqkd->bhgqk', q, k).astype(F32) * HEAD_DIM ** -0.5 + jnp.moveaxis(bias, -1, 2).astype(F32)
    p = masked_softmax(s, mask[:, :, None])
    return jnp.einsum('bhgqk,bhqkd->bhgqd', p.astype(v.dtype), v)


def attend_window(q, wkv, q_pos, k_pos, table):
    d = q_pos[:, None] - k_pos[None, :]
    mask = (d >= 0) & (d <= WINDOW) & (k_pos[None, :] >= 0)
    o, _ = attend_shared(q, wkv[:, :, 0], wkv[:, :, 1], q_pos, k_pos, mask, table)
    return o


def nsa_core(q, gates, q_pos, ckv, gather_fn, n_blocks, wkv, w_pos, table):
    B, Tq = q.shape[:2]
    qh = jnp.transpose(q.reshape(B, Tq, N_KV_HEADS, GQA, HEAD_DIM), (0, 2, 3, 1, 4))
    o_cmp, p_cmp = attend_compressed(qh, ckv, q_pos, table)
    idx, valid = select_blocks(p_cmp, q_pos, n_blocks)
    o_sel = attend_selected(qh, gather_fn(idx), idx, valid, q_pos, table)
    o_win = attend_window(qh, wkv, q_pos, w_pos, table)
    o = jnp.stack([o_cmp, o_sel, o_win], -1)
    o = jnp.transpose(o, (0, 3, 1, 2, 4, 5)).reshape(B, Tq, N_HEADS, HEAD_DIM, 3)
    g = jax.nn.sigmoid(gates.astype(F32)).astype(o.dtype)
    return jnp.einsum('bqhdr,bqhr->bqhd', o, g).reshape(B, Tq, D_ATT)


def nsa_prompt(q, gates, kv_cmp, kv_sel, kv_win, phi_pe, phi_w1, phi_b1, phi_w2, phi_b2, table):
    B, T = q.shape[:2]
    ckv = compress_kv(kv_cmp, phi_pe, phi_w1, phi_b1, phi_w2, phi_b2)
    n_blocks = T // SEL_BLOCK
    qb = min(Q_BLOCK, T)
    wpad = jnp.pad(kv_win, ((0, 0), (WINDOW, 0), (0, 0), (0, 0), (0, 0)))

    def block(i):
        s = i * qb
        q_i = lax.dynamic_slice_in_dim(q, s, qb, axis=1)
        g_i = lax.dynamic_slice_in_dim(gates, s, qb, axis=1)
        w_i = lax.dynamic_slice_in_dim(wpad, s, WINDOW + qb, axis=1)
        q_pos = s + jnp.arange(qb)
        w_pos = s - WINDOW + jnp.arange(WINDOW + qb)
        return nsa_core(q_i, g_i, q_pos, ckv, lambda idx: gather_blocks_contig(kv_sel, idx), n_blocks, w_i, w_pos, table)

    out = lax.map(block, jnp.arange(T // qb))
    return jnp.moveaxis(out, 0, 1).reshape(B, T, D_ATT)


def nsa_sample(q, gates, kv_cmp, kv_sel, kv_win, pool_cmp, pool_sel, win_buf, page_table, phi_pe, phi_w1, phi_b1, phi_w2, phi_b2, table):
    B, tn = q.shape[:2]
    past_cmp = pool_cmp[page_table].reshape(B, PAST_LEN, 2, N_KV_HEADS, HEAD_DIM)
    L = PAST_LEN + tn
    Lp = -(-L // SEL_BLOCK) * SEL_BLOCK
    full = jnp.pad(jnp.concatenate([past_cmp, kv_cmp], 1), ((0, 0), (0, Lp - L), (0, 0), (0, 0), (0, 0)))
    ckv = compress_kv(full, phi_pe, phi_w1, phi_b1, phi_w2, phi_b2)
    q_pos = PAST_LEN + jnp.arange(tn)
    wkv = jnp.concatenate([win_buf, kv_win], 1)
    w_pos = PAST_LEN - win_buf.shape[1] + jnp.arange(wkv.shape[1])
    return nsa_core(q, gates, q_pos, ckv, lambda idx: gather_blocks_paged(pool_sel, page_table, kv_sel, idx), Lp // SEL_BLOCK, wkv, w_pos, table)


def ssm_branch(u, h0, lam_re, lam_im, log_dt, b_re, b_im, c_re, c_im, d_skip, w_glu, b_glu):
    B, T = u.shape[:2]
    uf = u.astype(F32).reshape(B, T, N_SSM_GROUPS, SSM_GROUP)
    lam = lax.complex(lam_re.astype(F32), lam_im.astype(F32))
    dt = jnp.exp(log_dt.astype(F32))[:, None]
    lam_bar = jnp.exp(lam * dt)
    bb = ((lam_bar - 1.0) / lam)[:, :, None] * lax.complex(b_re.astype(F32), b_im.astype(F32))
    bu = jnp.einsum('btgc,gpc->btgp', uf.astype(jnp.complex64), bb)
    bu = bu.at[:, 0].add(lam_bar * h0)
    a = jnp.broadcast_to(lam_bar, bu.shape)

    def comb(e1, e2):
        a1, b1 = e1
        a2, b2 = e2
        return a1 * a2, a2 * b1 + b2

    _, hs = lax.associative_scan(comb, (a, bu), axis=1)
    cc = lax.complex(c_re.astype(F32), c_im.astype(F32))
    y = jnp.einsum('btgp,gcp->btgc', hs, cc).real + d_skip.astype(F32).reshape(N_SSM_GROUPS, SSM_GROUP) * uf
    g = jax.nn.gelu(y.reshape(B, T, D_SSM))
    out = g * jax.nn.sigmoid(g @ w_glu.astype(F32) + b_glu.astype(F32))
    return out.astype(u.dtype), hs[:, -1]


def mixer_in(x, shift, scale, w_in):
    B, T = x.shape[:2]
    z = modulate(x, shift, scale) @ w_in
    cuts = [D_SSM, D_SSM + D_ATT, D_SSM + D_ATT + D_KV, D_SSM + D_ATT + 2 * D_KV, D_SSM + D_ATT + 3 * D_KV]
    u, q, kvc, kvs, kvw, g = jnp.split(z, cuts, axis=-1)
    kv_shape = (B, T, 2, N_KV_HEADS, HEAD_DIM)
    return (u, q.reshape(B, T, N_HEADS, HEAD_DIM), kvc.reshape(kv_shape), kvs.reshape(kv_shape),
            kvw.reshape(kv_shape), g.reshape(B, T, N_HEADS, 3))


def moe(h, w_router, b_router, w_gate_up, b_gate_up, w_down, b_down):
    shp = h.shape
    x = h.reshape(-1, D_MODEL)
    n = x.shape[0]
    logits = (x @ w_router).astype(F32) + b_router.astype(F32)
    top_v, top_e = lax.top_k(logits, TOP_K)
    top_w = jax.nn.softmax(top_v, axis=-1)
    e = top_e.reshape(-1)
    tok = jnp.repeat(jnp.arange(n, dtype=jnp.int32), TOP_K)
    order = jnp.argsort(e)
    e_s, tok_s, w_s = e[order], tok[order], top_w.reshape(-1)[order]
    blk = max(8, min(MOE_BLOCK_MAX, n * TOP_K // N_EXPERTS))
    counts = jnp.bincount(e, length=N_EXPERTS)
    pcounts = (counts + blk - 1) // blk * blk
    start = jnp.cumsum(counts) - counts
    pend = jnp.cumsum(pcounts)
    pstart = pend - pcounts
    dest = pstart[e_s] + jnp.arange(n * TOP_K) - start[e_s]
    n_blk = (n * TOP_K + N_EXPERTS * (blk - 1)) // blk
    rows = n_blk * blk
    row_tok = jnp.full((rows,), n, jnp.int32).at[dest].set(tok_s)
    row_w = jnp.zeros((rows,), F32).at[dest].set(w_s)
    blk_e = jnp.minimum(jnp.searchsorted(pend, jnp.arange(n_blk) * blk, side='right'), N_EXPERTS - 1)
    xb = jnp.concatenate([x, jnp.zeros((1, D_MODEL), x.dtype)])[row_tok].reshape(n_blk, blk, D_MODEL)

    def expert(args):
        xe, ei = args
        gu = xe @ w_gate_up[ei] + b_gate_up[ei]
        gate = jnp.minimum(gu[:, :D_FF], SWIGLU_LIMIT)
        up = jnp.clip(gu[:, D_FF:], -SWIGLU_LIMIT, SWIGLU_LIMIT)
        hh = (up + 1.0) * gate * jax.nn.sigmoid(SWIGLU_ALPHA * gate)
        return hh @ w_down[ei] + b_down[ei]

    yb = lax.map(expert, (xb, blk_e)).reshape(rows, D_MODEL)
    y = jnp.zeros((n + 1, D_MODEL), F32).at[row_tok].add(yb.astype(F32) * row_w[:, None])
    return y[:n].astype(h.dtype).reshape(shp)


def setup_inputs(seed: int = 0) -> dict:
    key = jax.random.key(seed)
    ks = iter(jax.random.split(key, 48))

    def nrm(shape, scale):
        return jax.random.normal(next(ks), shape, F32) * scale

    n_pages = PAST_LEN // PAGE_SIZE
    n_pool = (DEC_BATCH * n_pages * 5) // 4
    win_buf = min(WINDOW, PAST_LEN)
    kv_tail = (2, N_KV_HEADS, HEAD_DIM)
    G, P, C = N_SSM_GROUPS, SSM_STATE, SSM_GROUP
    inp = {}
    inp['x_prompt'] = nrm((BATCH, SEQ, D_MODEL), 1.0)
    inp['x_sample'] = nrm((DEC_BATCH, DEC_SEQ, D_MODEL), 1.0)
    inp['cache_cmp_kv'] = nrm((DEPTH, n_pool, PAGE_SIZE) + kv_tail, 1.0)
    inp['cache_sel_kv'] = nrm((DEPTH, n_pool, PAGE_SIZE) + kv_tail, 1.0)
    inp['state_win_kv'] = nrm((DEPTH, DEC_BATCH, win_buf) + kv_tail, 1.0)
    inp['state_ssm_re'] = nrm((DEPTH, DEC_BATCH, G, P), 1.0)
    inp['state_ssm_im'] = nrm((DEPTH, DEC_BATCH, G, P), 1.0)
    inp['page_table'] = jax.random.permutation(next(ks), n_pool)[:DEC_BATCH * n_pages].reshape(DEC_BATCH, n_pages).astype(jnp.int32)
    inp['c_prompt'] = nrm((BATCH, D_MODEL), 1.0)
    inp['c_sample'] = nrm((DEC_BATCH, D_MODEL), 1.0)
    inp['w_ada'] = nrm((DEPTH, D_MODEL, 6 * D_MODEL), D_MODEL ** -0.5)
    inp['b_ada'] = nrm((DEPTH, 6 * D_MODEL), 0.02)
    inp['w_in'] = nrm((DEPTH, D_MODEL, D_IN), D_MODEL ** -0.5)
    inp['lam_re'] = -0.5 + nrm((DEPTH, G, P), 0.01)
    inp['lam_im'] = math.pi * jnp.arange(P, dtype=F32) + nrm((DEPTH, G, P), 0.01)
    inp['log_dt'] = jax.random.uniform(next(ks), (DEPTH, G), F32, math.log(1e-3), math.log(1e-1))
    inp['b_re'] = nrm((DEPTH, G, P, C), (2 * C) ** -0.5)
    inp['b_im'] = nrm((DEPTH, G, P, C), (2 * C) ** -0.5)
    inp['c_re'] = nrm((DEPTH, G, C, P), P ** -0.5)
    inp['c_im'] = nrm((DEPTH, G, C, P), P ** -0.5)
    inp['d_skip'] = nrm((DEPTH, D_SSM), 1.0)
    inp['w_glu'] = nrm((DEPTH, D_SSM, D_SSM), D_SSM ** -0.5)
    inp['b_glu'] = nrm((DEPTH, D_SSM), 0.02)
    inp['phi_pe'] = nrm((DEPTH, 2, CMP_BLOCK, HEAD_DIM), 0.02)
    inp['phi_w1'] = nrm((DEPTH, 2, CMP_BLOCK, HEAD_DIM, HEAD_DIM), (CMP_BLOCK * HEAD_DIM) ** -0.5)
    inp['phi_b1'] = nrm((DEPTH, 2, HEAD_DIM), 0.02)
    inp['phi_w2'] = nrm((DEPTH, 2, HEAD_DIM, HEAD_DIM), HEAD_DIM ** -0.5)
    inp['phi_b2'] = nrm((DEPTH, 2, HEAD_DIM), 0.02)
    inp['rel_bias'] = nrm((NUM_BUCKETS, N_HEADS), 0.1)
    inp['w_out'] = nrm((DEPTH, D_SSM + D_ATT, D_MODEL), (D_SSM + D_ATT) ** -0.5 * DN_BETA)
    inp['ln1_g'] = 1.0 + nrm((DEPTH, D_MODEL), 0.02)
    inp['ln1_b'] = nrm((DEPTH, D_MODEL), 0.02)
    inp['w_router'] = nrm((DEPTH, D_MODEL, N_EXPERTS), D_MODEL ** -0.5)
    inp['b_router'] = nrm((DEPTH, N_EXPERTS), 0.01)
    inp['w_gate_up'] = nrm((DEPTH, N_EXPERTS, D_MODEL, 2 * D_FF), D_MODEL ** -0.5)
    inp['b_gate_up'] = nrm((DEPTH, N_EXPERTS, 2 * D_FF), 0.02)
    inp['w_down'] = nrm((DEPTH, N_EXPERTS, D_FF, D_MODEL), D_FF ** -0.5 * DN_BETA)
    inp['b_down'] = nrm((DEPTH, N_EXPERTS, D_MODEL), 0.02)
    inp['ln2_g'] = 1.0 + nrm((DEPTH, D_MODEL), 0.02)
    inp['ln2_b'] = nrm((DEPTH, D_MODEL), 0.02)
    return inp


def reference(x_prompt, x_sample, cache_cmp_kv, cache_sel_kv, state_win_kv, state_ssm_re, state_ssm_im, page_table,
              c_prompt, c_sample, w_ada, b_ada, w_in, lam_re, lam_im, log_dt, b_re, b_im, c_re, c_im, d_skip,
              w_glu, b_glu, phi_pe, phi_w1, phi_b1, phi_w2, phi_b2, rel_bias, w_out, ln1_g, ln1_b,
              w_router, b_router, w_gate_up, b_gate_up, w_down, b_down, ln2_g, ln2_b):
    xp, xs = x_prompt, x_sample
    cmp_p, cmp_s, sel_p, sel_s, win_p, win_s, re_p, im_p, re_s, im_s = ([] for _ in range(10))
    for l in range(DEPTH):
        ssm_w = (lam_re[l], lam_im[l], log_dt[l], b_re[l], b_im[l], c_re[l], c_im[l], d_skip[l], w_glu[l], b_glu[l])
        phi = (phi_pe[l], phi_w1[l], phi_b1[l], phi_w2[l], phi_b2[l])
        moe_w = (w_router[l], b_router[l], w_gate_up[l], b_gate_up[l], w_down[l], b_down[l])

        m = adaln(c_prompt, w_ada[l], b_ada[l])
        u, q, kvc, kvs, kvw, g = mixer_in(xp, m[0], m[1], w_in[l])
        h0 = jnp.zeros((xp.shape[0], N_SSM_GROUPS, SSM_STATE), jnp.complex64)
        ssm_y, h_last = ssm_branch(u, h0, *ssm_w)
        att = nsa_prompt(q, g, kvc, kvs, kvw, *phi, rel_bias)
        xp = post_norm(xp, m[2], jnp.concatenate([ssm_y, att], -1) @ w_out[l], ln1_g[l], ln1_b[l])
        xp = post_norm(xp, m[5], moe(modulate(xp, m[3], m[4]), *moe_w), ln2_g[l], ln2_b[l])
        cmp_p.append(kvc)
        sel_p.append(kvs)
        win_p.append(kvw[:, -min(WINDOW, kvw.shape[1]):])
        re_p.append(h_last.real.astype(state_ssm_re.dtype))
        im_p.append(h_last.imag.astype(state_ssm_im.dtype))

        m = adaln(c_sample, w_ada[l], b_ada[l])
        u, q, kvc, kvs, kvw, g = mixer_in(xs, m[0], m[1], w_in[l])
        h0 = lax.complex(state_ssm_re[l].astype(F32), state_ssm_im[l].astype(F32))
        ssm_y, h_last = ssm_branch(u, h0, *ssm_w)
        att = nsa_sample(q, g, kvc, kvs, kvw, cache_cmp_kv[l], cache_sel_kv[l], state_win_kv[l], page_table, *phi, rel_bias)
        xs = post_norm(xs, m[2], jnp.concatenate([ssm_y, att], -1) @ w_out[l], ln1_g[l], ln1_b[l])
        xs = post_norm(xs, m[5], moe(modulate(xs, m[3], m[4]), *moe_w), ln2_g[l], ln2_b[l])
        cmp_s.append(kvc)
        sel_s.append(kvs)
        win_s.append(jnp.concatenate([state_win_kv[l], kvw], 1)[:, -state_win_kv.shape[2]:])
        re_s.append(h_last.real.astype(state_ssm_re.dtype))
        im_s.append(h_last.imag.astype(state_ssm_im.dtype))

    return (xp, xs, jnp.stack(cmp_p), jnp.stack(cmp_s), jnp.stack(sel_p), jnp.stack(sel_s),
            jnp.stack(win_p), jnp.stack(win_s), jnp.stack(re_p), jnp.stack(im_p), jnp.stack(re_s), jnp.stack(im_s))
```

```python
import numpy as np
import concourse.bass as bass
import concourse.mybir as mybir
from contextlib import ExitStack

F32 = mybir.dt.float32
BF16 = mybir.dt.bfloat16
I32 = mybir.dt.int32
U32 = mybir.dt.uint32
AF = mybir.ActivationFunctionType
ALU = mybir.AluOpType
AX = mybir.AxisListType


class Buf:
    __slots__ = ("t", "name", "w", "r")

    def __init__(self, t, name):
        self.t = t
        self.name = name
        self.w = None
        self.r = []

    def __getitem__(self, k):
        return self.t[k]


class KB:
    ENG = ("pe", "dve", "act", "pool", "sp")

    def __init__(self, nc, n_dma_sems=40):
        self.nc = nc
        self.es = ExitStack()
        self.es.enter_context(nc.allow_non_contiguous_dma(reason="small strided loads"))
        self.eng = {"pe": nc.tensor, "dve": nc.vector, "act": nc.scalar, "pool": nc.gpsimd, "sp": nc.sync}
        self.sem = {e: self.es.enter_context(nc.semaphore("s_" + e)) for e in self.ENG}
        self.cnt = {e: 0 for e in self.ENG}
        self.dsem = [self.es.enter_context(nc.semaphore("d%d" % i)) for i in range(n_dma_sems)]
        self.dcnt = [0] * n_dma_sems
        self.dnext = 0
        self.seen = {e: {} for e in self.ENG}
        self.n_wait = 0
        self.n_inst = 0
        self.pend = None

    def sb(self, name, shape, dt=F32):
        self.uid = getattr(self, "uid", 0) + 1
        t = self.es.enter_context(self.nc.sbuf_tensor("sb%d_%s" % (self.uid, name), list(shape), dt))
        return Buf(t, name)

    def ps(self, name, shape, dt=F32):
        t = self.es.enter_context(self.nc.psum_tensor(name, list(shape), dt))
        return Buf(t, name)

    def dram(self, name, shape, dt=F32, kind="Internal"):
        t = self.nc.dram_tensor(name, list(shape), dt, kind=kind)
        return Buf(t.ap(), name)

    def scope(self):
        kb = self
        class _S:
            def __enter__(s2):
                s2.prev = kb.es
                kb.es = ExitStack()
                return kb
            def __exit__(s2, *a):
                kb.barrier()
                kb.es.close()
                kb.es = s2.prev
                return False
        return _S()

    def init_psum(self):
        self.psb = [self.ps("psb%d" % i, [128, 512]) for i in range(8)]
        self.psi = 0

    def nextps(self):
        b = self.psb[self.psi]
        self.psi = (self.psi + 1) % 8
        return b

    def _wait(self, e, tok):
        if tok is None:
            return
        sem, val, src = tok[0], tok[1], tok[2]
        key = id(sem)
        if self.seen[e].get(key, 0) >= val:
            return
        if self.pend is not None:
            cur = self.pend.get(key)
            if cur is None or cur[1] < val:
                self.pend[key] = (sem, val)
            return
        self.eng[e].wait_ge(sem, val)
        self.seen[e][key] = val
        self.n_wait += 1

    def _flush(self, e, inst_fn):
        items = list(self.pend.values())
        self.pend = None
        for sem, val in items[:-1]:
            self.eng[e].wait_ge(sem, val)
            self.seen[e][id(sem)] = val
            self.n_wait += 1
        inst = inst_fn()
        if items:
            sem, val = items[-1]
            inst._wait_ge(sem, val)
            self.seen[e][id(sem)] = val
        return inst

    def _deps(self, e, reads, writes, pe_acc=False):
        for b in reads:
            if b.w is not None:
                self._wait(e, b.w)
        for b in writes:
            if b.w is not None and not (pe_acc and b.w[2] == "pe" and e == "pe"):
                if not (b.w[2] == e and e != "dma"):
                    self._wait(e, b.w)
            for tok in b.r:
                if tok[2] != e:
                    self._wait(e, tok)

    def _commit(self, tok, reads, writes):
        for b in reads:
            b.r.append(tok)
            if len(b.r) > 24:
                b.r = b.r[-24:] if False else self._compact(b.r)
        for b in writes:
            b.w = tok
            b.r = []

    @staticmethod
    def _compact(rs):
        best = {}
        for tok in rs:
            k = id(tok[0])
            if k not in best or best[k][1] < tok[1]:
                best[k] = tok
        return list(best.values())

    def op(self, e, fn, reads=(), writes=(), pe_acc=False):
        self.pend = {}
        self._deps(e, reads, writes, pe_acc)
        inst = self._flush(e, fn)
        self.cnt[e] += 1
        inst.then_inc(self.sem[e], 1)
        tok = (self.sem[e], self.cnt[e], e)
        self._commit(tok, reads, writes)
        self.n_inst += 1
        return tok

    def dma(self, q, out, in_, reads=(), writes=(), grp=None, fn=None):
        self.pend = {}
        self._deps(q, reads, writes)
        if grp is not None and "i" in grp:
            i = grp["i"]
        else:
            i = self.dnext
            self.dnext = (self.dnext + 1) % len(self.dsem)
            if self.dcnt[i] > 0:
                self._wait(q, (self.dsem[i], self.dcnt[i], "dma"))
            if grp is not None:
                grp["i"] = i
        if fn is None:
            inst = self._flush(q, lambda: self.eng[q].dma_start(out=out, in_=in_))
        else:
            inst = self._flush(q, fn)
        self.dcnt[i] += 16
        inst.then_inc(self.dsem[i], 16)
        if grp is not None:
            tok = grp.setdefault("tok", [self.dsem[i], 0, "dma"])
            tok[1] = self.dcnt[i]
        else:
            tok = [self.dsem[i], self.dcnt[i], "dma"]
        self._commit(tok, reads, writes)
        self.n_inst += 1
        return tok

    def barrier(self):
        toks = [(self.sem[e], self.cnt[e], e) for e in self.ENG if self.cnt[e] > 0]
        toks += [(self.dsem[i], self.dcnt[i], "dma") for i in range(len(self.dsem)) if self.dcnt[i] > 0]
        for e in self.ENG:
            for tok in toks:
                if tok[2] != e:
                    self._wait(e, tok)

    def finish(self):
        self.barrier()

    def close(self):
        self.es.close()

import os
import math
from concourse.bass_utils import run_bass_kernel_spmd

D = 1024
T = 4096
NB = 32
NOWN = 16
NS = 4
D_IN = 1816
EPS = 1e-5
STAGE = int(os.environ.get("MK_STAGE", "9"))


DBG = bool(int(os.environ.get("MK_DBG", "0")))


def build_program(stage=STAGE, dbg=DBG):
    nc = bass.Bass("TRN2", target_bir_lowering=False)
    k = KB(nc)
    k.init_psum()
    V, A, G, PE = nc.vector, nc.scalar, nc.gpsimd, nc.tensor

    def din(name, shape, dt=F32):
        return k.dram(name, shape, dt, kind="ExternalInput")

    def dout(name, shape, dt=F32):
        return k.dram(name, shape, dt, kind="ExternalOutput")

    xp = din("xp", [T, D])
    xs = din("xs", [NS, D])
    cT_d = din("cT", [128, 8, 5])
    w_ada = din("w_ada", [D, 6 * D])
    b_adaT_d = din("b_adaT", [128, 48])
    b_ada_row = din("b_ada_row", [1, 6 * D])
    w_in = din("w_in", [D, D_IN])
    ident_d = din("ident", [128, 128])
    state_win = din("state_win", [NS, 512, 256])
    xo = din("xo", [NOWN * 128, D])
    parv_d = din("parv", [128, 1])
    lamc_d = din("lamc", [128, 3, 16])
    lamr_d = din("lamr", [3, 2048])
    btp_d = din("btp", [2, 128, 16, 128])
    ctp_d = din("ctp", [2, 128, 16, 32])
    dskc_d = din("dskc", [128, 4])
    w_glu_d = din("w_glu", [512, 512])
    b_glu_d = din("b_glu", [1, 512])
    h0c_d = din("h0c", [2, 128, 16, NS])
    YG = 1408
    CAP = 384
    NSLOT = 32 * CAP
    w_out_d = din("w_out", [D, D])
    lnrows_d = din("lnrows", [4, D])
    w_router_d = din("w_router", [D, 32])
    b_router_d = din("b_router", [1, 32])
    w_gu_d = din("w_gate_up", [32, D, 2048])
    b_guT_d = din("b_guT", [128, 32, 16])
    w_dn_d = din("w_down", [32, D, D])
    b_dn_d = din("b_down", [32, D])
    xs_rows = xs
    pool_cmp = din("pool_cmp", [5120 * 128, 256])
    pool_sel = din("pool_sel", [5120 * 128 * 2, 128])
    pt_d = din("pt", [1, NS * 128], I32)
    pidx_d = din("pidx", [128, 1])
    ohc_d = din("ohc", [33, 1024])
    ohs_d = din("ohs", [33, 9, 128])
    ohw_d = din("ohw", [33, 5, 128])
    gsum_d = din("gsum", [8, 2])
    gexp_d = din("gexp", [2, 8])
    selcs_d = din("selcs", [2, 2, 260])
    half2_d = din("half2", [2, 128])
    m01_d = din("m01", [8, 2])
    o_y_p = dout("o_y_p", [NOWN * 128, D])
    o_y_s = dout("o_y_s", [NS, D])
    rel_bias_d = din("rel_bias", [32, 8])
    oh_d = din("oh", [2, 33, YG])
    jflip_d = din("jflip", [128, 128])
    selc_d = din("selc", [3, NOWN, 128, 64])
    bd1_d = din("bd1", [128, 64, 128])
    bd2_d = din("bd2", [128, 2, 128])
    pel_d = din("pel", [128, 2, 2, 16])
    pb1_d = din("pb1", [128, 2])
    pb2_d = din("pb2", [128, 2])
    pb2r_d = din("pb2r", [1, 128])
    o_cmp_p = dout("o_cmp_p", [T, 256])
    o_sel_p = dout("o_sel_p", [T, 256])
    o_win_p = dout("o_win_p", [T, 256])
    o_cmp_s = dout("o_cmp_s", [NS, 256])
    o_sel_s = dout("o_sel_s", [NS, 256])
    o_win_s = dout("o_win_s", [NS, 512, 256])
    o_ssm_p = dout("o_ssm_p", [2, 128, 16])
    o_ssm_s = dout("o_ssm_s", [2, 128, 16, NS])
    zT_u = k.dram("zT_u", [4, 128, T])
    zT_kv = k.dram("zT_kv", [6, 128, T])
    zT_uo = k.dram("zT_uo", [4, 128, NOWN * 128])
    zT_q = k.dram("zT_q", [4, 128, NOWN * 128])
    gates_d = k.dram("gates_d", [NOWN * 128, 24])
    ssm_out_d = k.dram("ssm_out_d", [NOWN * 128, 512])
    zsT = k.sb("zsT", [128, 14, NS])
    zs = k.sb("zs", [NS, D_IN])
    ssm_out_s = k.sb("ssm_out_s", [NS, 512])
    att_d = k.dram("att_d", [NOWN * 128, 512])
    att_s = k.sb("att_s", [NS, 512])
    k.op("dve", lambda: V.memset(att_s[:], 0.0), writes=[att_s])
    x1_d = k.dram("x1_d", [NOWN * 128 + 128, D])
    selm_d = k.dram("selm_d", [NS, 8, 258])
    gts_d = k.dram("gts_d", [NS, 24])
    atts_d = k.dram("atts_d", [NS, 512])
    G_d = k.dram("G_d", [2, 8, 1408])

    ident = k.sb("ident", [128, 128])
    k.dma("sp", ident[:], ident_d[:], reads=[ident_d], writes=[ident])
    epsb = k.sb("epsb", [128, 1])
    k.op("dve", lambda: V.memset(epsb[:], EPS), writes=[epsb])
    mT = k.sb("mT", [128, 48, 5])
    sc1p = k.sb("sc1p", [128, 8, 5])
    sc2p = k.sb("sc2p", [128, 8, 5])

    def ln_rows(xt, P, st, mv, rstd, out):
        for j in range(2):
            k.op("dve", lambda j=j: V.bn_stats(st[0:P, j, :], xt[0:P, j * 512:(j + 1) * 512]), reads=[xt], writes=[st])
        k.op("dve", lambda: V.bn_aggr(mv[0:P, :], st[0:P].rearrange("p a b -> p (a b)")), reads=[st], writes=[mv])
        k.op("act", lambda: A.activation(rstd[0:P, :], mv[0:P, 1:2], AF.Sqrt, bias=epsb[0:P, 0:1], scale=1.0), reads=[mv, epsb], writes=[rstd])
        k.op("dve", lambda: V.reciprocal(rstd[0:P, :], rstd[0:P, :]), reads=[rstd], writes=[rstd])
        k.op("dve", lambda: V.tensor_scalar(out[0:P, :], xt[0:P, :], mv[0:P, 0:1], rstd[0:P, 0:1], op0=ALU.subtract, op1=ALU.mult),
             reads=[xt, mv, rstd], writes=[out])

    with k.scope():
        cT = k.sb("cTs", [128, 8, 5])
        sc = k.sb("sc", [128, 8, 5])
        b_adaT = k.sb("b_adaTs", [128, 48])
        k.dma("sp", cT[:], cT_d[:], reads=[cT_d], writes=[cT])
        k.dma("sp", b_adaT[:], b_adaT_d[:], reads=[b_adaT_d], writes=[b_adaT])
        k.op("act", lambda: A.activation(sc[:], cT[:], AF.Silu), reads=[cT], writes=[sc])
        wa = [k.sb("wa%d" % i, [128, 8, 768]) for i in range(2)]
        w_ada_v = w_ada.t.rearrange("(k p) c -> p k c", p=128)
        for tg in range(8):
            wb = wa[tg % 2]
            g = {}
            for kk in range(8):
                k.dma("sp" if kk % 2 == 0 else "act", wb[:, kk, :], w_ada_v[:, kk, tg * 768:(tg + 1) * 768], reads=[w_ada], writes=[wb], grp=g)
            ps = k.nextps()
            for t6 in range(6):
                for kk in range(8):
                    k.op("pe", lambda t6=t6, kk=kk: PE.matmul(ps[:, t6 * 8:t6 * 8 + 5], wb[:, kk, t6 * 128:(t6 + 1) * 128], sc[:, kk, :],
                                                             start=(kk == 0), stop=(kk == 7)),
                         reads=[wb, sc], writes=[ps], pe_acc=True)
            k.op("dve", lambda tg=tg, ps=ps: V.tensor_tensor(
                mT[:, tg * 6:(tg + 1) * 6, :], ps[:, 0:48].rearrange("p (a b) -> p a b", b=8)[:, :, 0:5],
                b_adaT[:, tg * 6:(tg + 1) * 6].unsqueeze(2).to_broadcast([128, 6, 5]), op=ALU.add),
                 reads=[ps, b_adaT], writes=[mT])
        k.op("dve", lambda: V.tensor_scalar(sc1p[:], mT[:, 8:16, :], 1.0, None, op0=ALU.add), reads=[mT], writes=[sc1p])
        k.op("dve", lambda: V.tensor_scalar(sc2p[:], mT[:, 32:40, :], 1.0, None, op0=ALU.add), reads=[mT], writes=[sc2p])

    with k.scope():
        w_in_sb = k.sb("w_in_sb", [128, 8, D_IN])
        w_in_v = w_in.t.rearrange("(k p) c -> p k c", p=128)
        g = {}
        for kk in range(8):
            k.dma("sp" if kk % 2 == 0 else "act", w_in_sb[:, kk, :], w_in_v[:, kk, :], reads=[w_in], writes=[w_in_sb], grp=g)
        xt = [k.sb("xt%d" % i, [128, D]) for i in range(2)]
        xn = [k.sb("xn%d" % i, [128, D]) for i in range(2)]
        st = k.sb("st", [128, 2, 6])
        mv = k.sb("mv", [128, 2])
        rstd = k.sb("rstd", [128, 1])
        xmT = [k.sb("xmT%d" % i, [128, 8, 512]) for i in range(2)]
        stg = [k.sb("stg%d" % i, [128, 512]) for i in range(14)]
        kvst = [k.sb("kvst%d" % i, [128, 768]) for i in range(2)]

        def front_group(x_d, gi, jvec, fm_tiles, tm_kv):
            xm = xmT[gi % 2]
            for tt in range(4):
                r0 = gi * 512 + tt * 128
                xb, xnb = xt[tt % 2], xn[tt % 2]
                k.dma("sp", xb[:], x_d[r0:r0 + 128, :], reads=[x_d], writes=[xb])
                ln_rows(xb, 128, st, mv, rstd, xnb)
                for h in range(2):
                    ps = k.nextps()
                    for q in range(4):
                        kk = h * 4 + q
                        k.op("pe", lambda kk=kk, q=q, ps=ps: PE.transpose(ps[:, q * 128:(q + 1) * 128], xnb[:, kk * 128:(kk + 1) * 128], ident[:]),
                             reads=[xnb, ident], writes=[ps], pe_acc=True)
                    for q in range(4):
                        kk = h * 4 + q
                        k.op("act", lambda kk=kk, q=q, ps=ps: A.activation(
                            xm[:, kk, tt * 128:(tt + 1) * 128], ps[:, q * 128:(q + 1) * 128], AF.Identity,
                            bias=mT[:, kk, jvec:jvec + 1], scale=sc1p[:, kk, jvec:jvec + 1]),
                             reads=[ps, mT, sc1p], writes=[xm])
            for si, (c0, dst, di) in enumerate(fm_tiles):
                ps = k.nextps()
                for kk in range(8):
                    k.op("pe", lambda kk=kk, ps=ps, c0=c0: PE.matmul(ps[:, :], w_in_sb[:, kk, c0:c0 + 128], xm[:, kk, :], start=(kk == 0), stop=(kk == 7)),
                         reads=[w_in_sb, xm], writes=[ps], pe_acc=True)
                sb_ = stg[si]
                if si % 2 == 0:
                    k.op("dve", lambda ps=ps, sb_=sb_: V.tensor_copy(sb_[:], ps[:]), reads=[ps], writes=[sb_])
                else:
                    k.op("act", lambda ps=ps, sb_=sb_: A.copy(sb_[:], ps[:]), reads=[ps], writes=[sb_])
                k.dma("pool", dst[di, :, gi * 512:(gi + 1) * 512], sb_[:], reads=[sb_], writes=[dst])
            if tm_kv is not None:
                kv_stage_idx, outs = tm_kv
                for tt in range(4):
                    kb_ = kvst[tt % 2]
                    for h in range(2):
                        ps = k.nextps()
                        for q in range(3):
                            sb_ = stg[kv_stage_idx[h * 3 + q]]
                            k.op("pe", lambda q=q, ps=ps, sb_=sb_: PE.transpose(ps[:, q * 128:(q + 1) * 128], sb_[:, tt * 128:(tt + 1) * 128], ident[:]),
                                 reads=[sb_, ident], writes=[ps], pe_acc=True)
                        if h == 0:
                            k.op("dve", lambda ps=ps, kb_=kb_: V.tensor_copy(kb_[:, 0:384], ps[:, 0:384]), reads=[ps], writes=[kb_])
                        else:
                            k.op("act", lambda ps=ps, kb_=kb_: A.copy(kb_[:, 384:768], ps[:, 0:384]), reads=[ps], writes=[kb_])
                    r0 = gi * 512 + tt * 128
                    for oi, od in enumerate(outs):
                        k.dma("pool", od[r0:r0 + 128, :], kb_[:, oi * 256:(oi + 1) * 256], reads=[kb_], writes=[od])

        fmA = [(128 * i, zT_u, i) for i in range(4)] + [(1024 + 128 * i, zT_kv, i) for i in range(6)]
        for gi in range(T // 512):
            front_group(xp, gi, 0, fmA, ([4, 5, 6, 7, 8, 9], [o_cmp_p, o_sel_p, o_win_p]))

        fmB = [(128 * i, zT_uo, i) for i in range(4)] + [(512 + 128 * i, zT_q, i) for i in range(4)]
        gst = k.sb("gst", [128, 4, 24])
        for gi in range(NOWN * 128 // 512):
            front_group(xo, gi, 0, fmB, None)
            xm = xmT[gi % 2]
            ps = k.nextps()
            for tt in range(4):
                for kk in range(8):
                    k.op("pe", lambda kk=kk, tt=tt, ps=ps: PE.matmul(ps[:, tt * 32:tt * 32 + 24], xm[:, kk, tt * 128:(tt + 1) * 128], w_in_sb[:, kk, 1792:1816],
                                                                     start=(kk == 0), stop=(kk == 7)),
                         reads=[xm, w_in_sb], writes=[ps], pe_acc=True)
            k.op("act", lambda ps=ps: A.activation(gst[:], ps[:, 0:128].rearrange("p (a b) -> p a b", b=32)[:, :, 0:24], AF.Sigmoid), reads=[ps], writes=[gst])
            for tt in range(4):
                r0 = gi * 512 + tt * 128
                k.dma("pool", gates_d[r0:r0 + 128, :], gst[:, tt, :], reads=[gst], writes=[gates_d])

        xst = k.sb("xst", [NS, D])
        xsn = k.sb("xsn", [NS, D])
        xsT = k.sb("xsT", [128, 8, NS])
        k.dma("sp", xst[:], xs[:], reads=[xs], writes=[xst])
        ln_rows(xst, NS, st, mv, rstd, xsn)
        ps = k.nextps()
        for kk in range(8):
            k.op("pe", lambda kk=kk: PE.transpose(ps[:, kk * NS:(kk + 1) * NS], xsn[0:NS, kk * 128:(kk + 1) * 128], ident[0:NS, 0:NS]),
                 reads=[xsn, ident], writes=[ps], pe_acc=True)
        k.op("dve", lambda: V.tensor_tensor(xsT[:], ps[:, 0:8 * NS].rearrange("p (a b) -> p a b", b=NS), sc1p[:, :, 1:5], op=ALU.mult),
             reads=[ps, sc1p], writes=[xsT])
        k.op("dve", lambda: V.tensor_tensor(xsT[:], xsT[:], mT[:, 0:8, 1:5], op=ALU.add), reads=[xsT, mT], writes=[xsT])
        for cg in range(4):
            c0 = cg * 512
            cn = min(512, D_IN - c0)
            ps = k.nextps()
            for kk in range(8):
                k.op("pe", lambda kk=kk, ps=ps, c0=c0, cn=cn: PE.matmul(ps[0:NS, 0:cn], xsT[:, kk, :], w_in_sb[:, kk, c0:c0 + cn], start=(kk == 0), stop=(kk == 7)),
                     reads=[xsT, w_in_sb], writes=[ps], pe_acc=True)
            k.op("dve", lambda ps=ps, c0=c0, cn=cn: V.tensor_copy(zs[:, c0:c0 + cn], ps[0:NS, 0:cn]), reads=[ps], writes=[zs])
        ps = k.nextps()
        for ct in range(14):
            for kk in range(8):
                k.op("pe", lambda kk=kk, ct=ct, ps=ps: PE.matmul(ps[:, ct * NS:(ct + 1) * NS], w_in_sb[:, kk, ct * 128:(ct + 1) * 128], xsT[:, kk, :],
                                                                 start=(kk == 0), stop=(kk == 7)),
                     reads=[xsT, w_in_sb], writes=[ps], pe_acc=True)
        k.op("dve", lambda ps=ps: V.tensor_copy(zsT[:], ps[:, 0:14 * NS].rearrange("p (a b) -> p a b", b=NS)), reads=[ps], writes=[zsT])
        k.dma("pool", o_cmp_s[:, :], zs[:, 1024:1280], reads=[zs], writes=[o_cmp_s])
        k.dma("pool", o_sel_s[:, :], zs[:, 1280:1536], reads=[zs], writes=[o_sel_s])
        k.dma("pool", o_win_s[:, 511, :], zs[:, 1536:1792], reads=[zs], writes=[o_win_s])
        for j in range(NS):
            k.dma("sp", o_win_s[j, 0:511, :], state_win[j, 1:512, :], reads=[state_win], writes=[o_win_s])


    def gelu_tanh(out, xin, tmp, P, reads_extra=()):
        k.op("dve", lambda: V.tensor_tensor(tmp[0:P], xin[0:P], xin[0:P], op=ALU.mult), reads=[xin], writes=[tmp])
        k.op("dve", lambda: V.tensor_scalar(tmp[0:P], tmp[0:P], 0.044715, 1.0, op0=ALU.mult, op1=ALU.add), reads=[tmp], writes=[tmp])
        k.op("dve", lambda: V.tensor_tensor(tmp[0:P], tmp[0:P], xin[0:P], op=ALU.mult), reads=[tmp, xin], writes=[tmp])
        k.op("act", lambda: A.activation(tmp[0:P], tmp[0:P], AF.Sigmoid, scale=1.5957691216057308), reads=[tmp], writes=[tmp])
        k.op("dve", lambda: V.tensor_tensor(out[0:P], tmp[0:P], xin[0:P], op=ALU.mult), reads=[tmp, xin], writes=[out])

    if stage >= 2:
      with k.scope():
        TWO_PI = 2.0 * np.pi

        def ssm_consts(src, F, tag):
            o = {n: k.sb(tag + n, [128, F]) for n in ("lr", "li", "ir", "ii", "wr", "wi", "t1", "t2", "t3")}
            ti = k.sb(tag + "ti", [128, F], I32)
            dt_, ang, mag = o["t1"], o["t2"], o["t3"]
            k.op("act", lambda: A.activation(dt_[:], src[:, 2, :], AF.Exp), reads=[src], writes=[dt_])
            k.op("dve", lambda: V.tensor_tensor(mag[:], src[:, 0, :], dt_[:], op=ALU.mult), reads=[src, dt_], writes=[mag])
            k.op("dve", lambda: V.tensor_tensor(ang[:], src[:, 1, :], dt_[:], op=ALU.mult), reads=[src, dt_], writes=[ang])

            def sin_of(out, offs):
                r, f = o["wr"], o["wi"]
                k.op("dve", lambda: V.tensor_scalar(r[:], ang[:], 1.0 / TWO_PI, offs / TWO_PI, op0=ALU.mult, op1=ALU.add), reads=[ang], writes=[r])
                k.op("dve", lambda: V.tensor_copy(ti[:], r[:]), reads=[r], writes=[ti])
                k.op("dve", lambda: V.tensor_copy(f[:], ti[:]), reads=[ti], writes=[f])
                k.op("dve", lambda: V.tensor_tensor(r[:], r[:], f[:], op=ALU.subtract), reads=[r, f], writes=[r])
                k.op("dve", lambda: V.scalar_tensor_tensor(f[:], r[:], 0.5, r[:], op0=ALU.is_gt, op1=ALU.subtract), reads=[r], writes=[f])
                k.op("dve", lambda: V.scalar_tensor_tensor(r[:], f[:], 0.5, f[:], op0=ALU.is_gt, op1=ALU.subtract), reads=[f], writes=[r])
                k.op("act", lambda: A.activation(out[:], r[:], AF.Sin, scale=TWO_PI), reads=[r], writes=[out])
            sin_of(o["li"], 0.0)
            sin_of(o["lr"], np.pi / 2)
            k.op("act", lambda: A.activation(o["ii"][:], mag[:], AF.Exp, scale=-1.0), reads=[mag], writes=[o["ii"]])
            k.op("act", lambda: A.activation(mag[:], mag[:], AF.Exp), reads=[mag], writes=[mag])
            k.op("dve", lambda: V.tensor_tensor(o["ir"][:], o["lr"][:], o["ii"][:], op=ALU.mult), reads=[o["lr"], o["ii"]], writes=[o["ir"]])
            k.op("dve", lambda: V.scalar_tensor_tensor(o["ii"][:], o["li"][:], -1.0, o["ii"][:], op0=ALU.mult, op1=ALU.mult), reads=[o["li"], o["ii"]], writes=[o["ii"]])
            k.op("dve", lambda: V.tensor_tensor(o["lr"][:], o["lr"][:], mag[:], op=ALU.mult), reads=[o["lr"], mag], writes=[o["lr"]])
            k.op("dve", lambda: V.tensor_tensor(o["li"][:], o["li"][:], mag[:], op=ALU.mult), reads=[o["li"], mag], writes=[o["li"]])
            a, b_, den = o["t1"], o["t2"], o["t3"]
            k.op("dve", lambda: V.tensor_scalar(a[:], o["lr"][:], -1.0, None, op0=ALU.add), reads=[o["lr"]], writes=[a])
            k.op("dve", lambda: V.tensor_tensor(den[:], src[:, 0, :], src[:, 0, :], op=ALU.mult), reads=[src], writes=[den])
            k.op("dve", lambda: V.tensor_tensor(b_[:], src[:, 1, :], src[:, 1, :], op=ALU.mult), reads=[src], writes=[b_])
            k.op("dve", lambda: V.tensor_tensor(den[:], den[:], b_[:], op=ALU.add), reads=[den, b_], writes=[den])
            k.op("dve", lambda: V.reciprocal(den[:], den[:]), reads=[den], writes=[den])
            k.op("dve", lambda: V.tensor_tensor(o["wr"][:], a[:], src[:, 0, :], op=ALU.mult), reads=[a, src], writes=[o["wr"]])
            k.op("dve", lambda: V.tensor_tensor(b_[:], o["li"][:], src[:, 1, :], op=ALU.mult), reads=[o["li"], src], writes=[b_])
            k.op("dve", lambda: V.tensor_tensor(o["wr"][:], o["wr"][:], b_[:], op=ALU.add), reads=[o["wr"], b_], writes=[o["wr"]])
            k.op("dve", lambda: V.tensor_tensor(o["wr"][:], o["wr"][:], den[:], op=ALU.mult), reads=[o["wr"], den], writes=[o["wr"]])
            k.op("dve", lambda: V.tensor_tensor(o["wi"][:], o["li"][:], src[:, 0, :], op=ALU.mult), reads=[o["li"], src], writes=[o["wi"]])
            k.op("dve", lambda: V.tensor_tensor(b_[:], a[:], src[:, 1, :], op=ALU.mult), reads=[a, src], writes=[b_])
            k.op("dve", lambda: V.tensor_tensor(o["wi"][:], o["wi"][:], b_[:], op=ALU.subtract), reads=[o["wi"], b_], writes=[o["wi"]])
            k.op("dve", lambda: V.tensor_tensor(o["wi"][:], o["wi"][:], den[:], op=ALU.mult), reads=[o["wi"], den], writes=[o["wi"]])
            return o

        lamc = k.sb("lamc", [128, 3, 16])
        k.dma("sp", lamc[:], lamc_d[:], reads=[lamc_d], writes=[lamc])
        cc = ssm_consts(lamc, 16, "c_")
        bbT = [k.sb("bbT%d" % i, [128, 16, 128]) for i in range(2)]
        with k.scope():
            lamr = k.sb("lamr", [128, 3, 2048])
            for i in range(3):
                k.dma("sp", lamr[:, i, :], lamr_d[i:i + 1, :].partition_broadcast(128), reads=[lamr_d], writes=[lamr])
            cr = ssm_consts(lamr, 2048, "r_")
            btp = [k.sb("btp%d" % i, [128, 2048]) for i in range(2)]
            for i in range(2):
                k.dma("act", btp[i][:], btp_d[i].rearrange("p s t -> p (s t)"), reads=[btp_d], writes=[btp[i]])
            t1 = cr["t1"]
            bre, bim = (bbT[0][:].rearrange("p s t -> p (s t)"), bbT[1][:].rearrange("p s t -> p (s t)"))
            k.op("dve", lambda: V.tensor_tensor(bre, btp[0][:], cr["wr"][:], op=ALU.mult), reads=[btp[0], cr["wr"]], writes=[bbT[0]])
            k.op("dve", lambda: V.tensor_tensor(t1[:], btp[1][:], cr["wi"][:], op=ALU.mult), reads=[btp[1], cr["wi"]], writes=[t1])
            k.op("dve", lambda: V.tensor_tensor(bre, bre, t1[:], op=ALU.subtract), reads=[bbT[0], t1], writes=[bbT[0]])
            k.op("dve", lambda: V.tensor_tensor(bim, btp[0][:], cr["wi"][:], op=ALU.mult), reads=[btp[0], cr["wi"]], writes=[bbT[1]])
            k.op("dve", lambda: V.tensor_tensor(t1[:], btp[1][:], cr["wr"][:], op=ALU.mult), reads=[btp[1], cr["wr"]], writes=[t1])
            k.op("dve", lambda: V.tensor_tensor(bim, bim, t1[:], op=ALU.add), reads=[bbT[1], t1], writes=[bbT[1]])
        ctp = [k.sb("ctp%d" % i, [128, 16, 32]) for i in range(2)]
        for i in range(2):
            k.dma("sp", ctp[i][:], ctp_d[i], reads=[ctp_d], writes=[ctp[i]])
        k.op("dve", lambda: V.tensor_scalar(ctp[1][:], ctp[1][:], -1.0, None, op0=ALU.mult), reads=[ctp[1]], writes=[ctp[1]])
        dskc = k.sb("dskc", [128, 4])
        k.dma("sp", dskc[:], dskc_d[:], reads=[dskc_d], writes=[dskc])
        diagD = k.sb("diagD", [128, 4, 128])
        for ut in range(4):
            k.op("dve", lambda ut=ut: V.tensor_scalar(diagD[:, ut, :], ident[:], dskc[:, ut:ut + 1], None, op0=ALU.mult), reads=[ident, dskc], writes=[diagD])
        w_glu = k.sb("w_glu", [128, 4, 512])
        k.dma("act", w_glu[:], w_glu_d.t.rearrange("(k p) c -> p k c", p=128), reads=[w_glu_d], writes=[w_glu])
        b_glu = k.sb("b_glu", [1, 512])
        k.dma("act", b_glu[:], b_glu_d[:], reads=[b_glu_d], writes=[b_glu])
        ones_row = k.sb("ones_row", [1, 128])
        k.op("dve", lambda: V.memset(ones_row[:], 1.0), writes=[ones_row])
        ones_b = k.sb("ones_b", [128, 128])
        k.op("dve", lambda: V.memset(ones_b[:], 1.0), writes=[ones_b])
        ones_row_b = ones_b
        parv = k.sb("parv", [128, 1])
        k.dma("sp", parv[:], parv_d[:], reads=[parv_d], writes=[parv])

        Pp = [k.sb("Pp%d" % i, [128, 16, 128]) for i in range(2)]
        Pm = [k.sb("Pm%d" % i, [128, 16, 128]) for i in range(2)]
        ptmp = k.sb("ptmp", [128, 2, 16, 64])
        L128 = [k.sb("L128_%d" % i, [128, 16]) for i in range(2)]
        L127 = [k.sb("L127_%d" % i, [128, 16]) for i in range(2)]

        def cmul_small(o_re, o_im, a_re, a_im, b_re, b_im, bufs_r, bufs_w, t_re, t_im, tb):
            k.op("dve", lambda: V.tensor_tensor(t_re, a_re, b_re, op=ALU.mult), reads=bufs_r, writes=[tb])
            k.op("dve", lambda: V.tensor_tensor(t_im, a_im, b_im, op=ALU.mult), reads=bufs_r, writes=[tb])
            k.op("dve", lambda: V.tensor_tensor(t_re, t_re, t_im, op=ALU.subtract), reads=[tb], writes=[tb])
            k.op("dve", lambda: V.tensor_tensor(t_im, a_re, b_im, op=ALU.mult), reads=bufs_r, writes=[tb])
            k.op("dve", lambda: V.tensor_tensor(o_im, a_im, b_re, op=ALU.mult), reads=bufs_r + [tb], writes=bufs_w)
            k.op("dve", lambda: V.tensor_tensor(o_im, o_im, t_im, op=ALU.add), reads=bufs_w + [tb], writes=bufs_w)
            k.op("dve", lambda: V.tensor_copy(o_re, t_re), reads=[tb], writes=bufs_w)

        def build_pow(P, l_re, l_im, lbuf, tag):
            cur = [k.sb(tag + "cur%d" % i, [128, 16]) for i in range(2)]
            nxt = [k.sb(tag + "nxt%d" % i, [128, 16]) for i in range(2)]
            tmpb = k.sb(tag + "tmpb", [128, 2, 16])
            k.op("dve", lambda: V.memset(P[0][:, :, 0:1], 1.0), writes=[P[0]])
            k.op("dve", lambda: V.memset(P[1][:, :, 0:1], 0.0), writes=[P[1]])
            k.op("dve", lambda: V.tensor_copy(cur[0][:], l_re), reads=[lbuf[0]], writes=[cur[0]])
            k.op("dve", lambda: V.tensor_copy(cur[1][:], l_im), reads=[lbuf[1]], writes=[cur[1]])
            m = 1
            while m < 128:
                br = cur[0][:].unsqueeze(2).to_broadcast([128, 16, m])
                bi = cur[1][:].unsqueeze(2).to_broadcast([128, 16, m])
                cmul_small(P[0][:, :, m:2 * m], P[1][:, :, m:2 * m], P[0][:, :, 0:m], P[1][:, :, 0:m], br, bi,
                           [P[0], P[1], cur[0], cur[1]], [P[0], P[1]], ptmp[:, 0, :, 0:m], ptmp[:, 1, :, 0:m], ptmp)
                cmul_small(nxt[0][:], nxt[1][:], cur[0][:], cur[1][:], cur[0][:], cur[1][:], [cur[0], cur[1]], [nxt[0], nxt[1]],
                           tmpb[:, 0, :], tmpb[:, 1, :], tmpb)
                cur, nxt = nxt, cur
                m *= 2
            return cur

        l128 = build_pow(Pp, cc["lr"][:], cc["li"][:], [cc["lr"], cc["li"]], "pp_")
        build_pow(Pm, cc["ir"][:], cc["ii"][:], [cc["ir"], cc["ii"]], "pm_")
        for i in range(2):
            k.op("dve", lambda i=i: V.tensor_copy(L128[i][:], l128[i][:]), reads=[l128[i]], writes=[L128[i]])
            k.op("dve", lambda i=i: V.tensor_copy(L127[i][:], Pp[i][:, :, 127]), reads=[Pp[i]], writes=[L127[i]])

        E = [k.sb("E%d" % i, [128, 16, NB]) for i in range(2)]
        uTg = [k.sb("uTg%d" % i, [128, 4, 512]) for i in range(2)]
        bus = [[k.sb("bus%d_%d" % (j, i), [128, 512]) for i in range(2)] for j in range(2)]
        pr = [k.sb("pr%d" % i, [128, 512]) for i in range(4)]

        def bu_group(src_d, gi, s, par_):
            ug = uTg[gi % 2]
            for i in range(2):
                ps = k.nextps()
                k.op("pe", lambda i=i, ps=ps: PE.matmul(ps[:, :], bbT[i][:, s, :], ug[:, s // 4, :], start=True, stop=True),
                     reads=[bbT[i], ug], writes=[ps], pe_acc=True)
                k.op("act", lambda i=i, ps=ps: A.copy(bus[par_][i][:], ps[:]), reads=[ps], writes=[bus[par_][i]])
            return bus[par_]

        def v4(ap):
            return ap.rearrange("p (a b) -> p a b", b=128)

        def bc4(ap):
            return ap.unsqueeze(1).to_broadcast([128, 4, 128])

        for gi in range(T // 512):
            ug = uTg[gi % 2]
            g = {}
            for ut in range(4):
                k.dma("sp", ug[:, ut, :], zT_u[ut, :, gi * 512:(gi + 1) * 512], reads=[zT_u], writes=[ug], grp=g)
            for s in range(16):
                b = bu_group(zT_u, gi, s, s % 2)
                pmr, pmi = bc4(Pm[0][:, s, :]), bc4(Pm[1][:, s, :])
                k.op("dve", lambda: V.tensor_tensor(v4(pr[0][:]), v4(b[0][:]), pmr, op=ALU.mult), reads=[b[0], Pm[0]], writes=[pr[0]])
                k.op("dve", lambda: V.tensor_tensor(v4(pr[1][:]), v4(b[1][:]), pmi, op=ALU.mult), reads=[b[1], Pm[1]], writes=[pr[1]])
                k.op("dve", lambda: V.tensor_tensor(pr[0][:], pr[0][:], pr[1][:], op=ALU.subtract), reads=[pr[0], pr[1]], writes=[pr[0]])
                k.op("dve", lambda: V.tensor_reduce(E[0][:, s, gi * 4:(gi + 1) * 4], v4(pr[0][:]), axis=AX.X, op=ALU.add), reads=[pr[0]], writes=[E[0]])
                k.op("dve", lambda: V.tensor_tensor(v4(pr[2][:]), v4(b[0][:]), pmi, op=ALU.mult), reads=[b[0], Pm[1]], writes=[pr[2]])
                k.op("dve", lambda: V.tensor_tensor(v4(pr[3][:]), v4(b[1][:]), pmr, op=ALU.mult), reads=[b[1], Pm[0]], writes=[pr[3]])
                k.op("dve", lambda: V.tensor_tensor(pr[2][:], pr[2][:], pr[3][:], op=ALU.add), reads=[pr[2], pr[3]], writes=[pr[2]])
                k.op("dve", lambda: V.tensor_reduce(E[1][:, s, gi * 4:(gi + 1) * 4], v4(pr[2][:]), axis=AX.X, op=ALU.add), reads=[pr[2]], writes=[E[1]])

        Hst = [k.sb("Hst%d" % i, [128, 16, NB + 1]) for i in range(2)]
        hb = k.sb("hb", [128, 4, 16])
        for i in range(2):
            k.op("dve", lambda i=i: V.memset(Hst[i][:, :, 0:1], 0.0), writes=[Hst[i]])
        for kb_ in range(NB):
            hr, hi = Hst[0][:, :, kb_], Hst[1][:, :, kb_]
            er, ei = E[0][:, :, kb_], E[1][:, :, kb_]
            k.op("dve", lambda: V.tensor_tensor(hb[:, 0, :], L127[0][:], er, op=ALU.mult), reads=[L127[0], E[0]], writes=[hb])
            k.op("dve", lambda: V.tensor_tensor(hb[:, 1, :], L127[1][:], ei, op=ALU.mult), reads=[L127[1], E[1]], writes=[hb])
            k.op("dve", lambda: V.tensor_tensor(hb[:, 0, :], hb[:, 0, :], hb[:, 1, :], op=ALU.subtract), reads=[hb], writes=[hb])
            k.op("dve", lambda: V.tensor_tensor(hb[:, 1, :], L127[0][:], ei, op=ALU.mult), reads=[L127[0], E[1]], writes=[hb])
            k.op("dve", lambda: V.tensor_tensor(hb[:, 2, :], L127[1][:], er, op=ALU.mult), reads=[L127[1], E[0]], writes=[hb])
            k.op("dve", lambda: V.tensor_tensor(hb[:, 1, :], hb[:, 1, :], hb[:, 2, :], op=ALU.add), reads=[hb], writes=[hb])
            k.op("dve", lambda: V.tensor_tensor(hb[:, 2, :], L128[0][:], hr, op=ALU.mult), reads=[L128[0], Hst[0]], writes=[hb])
            k.op("dve", lambda: V.tensor_tensor(hb[:, 3, :], L128[1][:], hi, op=ALU.mult), reads=[L128[1], Hst[1]], writes=[hb])
            k.op("dve", lambda: V.tensor_tensor(hb[:, 2, :], hb[:, 2, :], hb[:, 3, :], op=ALU.subtract), reads=[hb], writes=[hb])
            k.op("dve", lambda: V.tensor_tensor(Hst[0][:, :, kb_ + 1], hb[:, 2, :], hb[:, 0, :], op=ALU.add), reads=[hb], writes=[Hst[0]])
            k.op("dve", lambda: V.tensor_tensor(hb[:, 2, :], L128[0][:], hi, op=ALU.mult), reads=[L128[0], Hst[1]], writes=[hb])
            k.op("dve", lambda: V.tensor_tensor(hb[:, 3, :], L128[1][:], hr, op=ALU.mult), reads=[L128[1], Hst[0]], writes=[hb])
            k.op("dve", lambda: V.tensor_tensor(hb[:, 2, :], hb[:, 2, :], hb[:, 3, :], op=ALU.add), reads=[hb], writes=[hb])
            k.op("dve", lambda: V.tensor_tensor(Hst[1][:, :, kb_ + 1], hb[:, 2, :], hb[:, 1, :], op=ALU.add), reads=[hb], writes=[Hst[1]])
        for i in range(2):
            k.dma("pool", o_ssm_p[i], Hst[i][:, :, NB], reads=[Hst[i]], writes=[o_ssm_p])

        Hown = [k.sb("Hown%d" % i, [128, 16, NOWN]) for i in range(2)]
        LH = [k.sb("LH%d" % i, [128, 16, NOWN]) for i in range(2)]
        for i in range(2):
            he, ho = Hst[i][:, :, 0:NB:2], Hst[i][:, :, 1:NB:2]
            k.op("dve", lambda i=i, he=he, ho=ho: V.tensor_tensor(Hown[i][:], ho, he, op=ALU.subtract), reads=[Hst[i]], writes=[Hown[i]])
            k.op("dve", lambda i=i: V.tensor_scalar(Hown[i][:], Hown[i][:], parv[:, 0:1], None, op0=ALU.mult), reads=[Hown[i], parv], writes=[Hown[i]])
            k.op("dve", lambda i=i, he=he: V.tensor_tensor(Hown[i][:], Hown[i][:], he, op=ALU.add), reads=[Hown[i], Hst[i]], writes=[Hown[i]])
        lrb = cc["lr"][:].unsqueeze(2).to_broadcast([128, 16, NOWN])
        lib = cc["li"][:].unsqueeze(2).to_broadcast([128, 16, NOWN])
        tmpLH = k.sb("tmpLH", [128, 2, 16, NOWN])
        cmul_small(LH[0][:], LH[1][:], Hown[0][:], Hown[1][:], lrb, lib, [Hown[0], Hown[1], cc["lr"], cc["li"]], [LH[0], LH[1]],
                   tmpLH[:, 0], tmpLH[:, 1], tmpLH)

        hbuf = [[k.sb("hbuf%d_%d" % (j, i), [128, 512]) for i in range(2)] for j in range(2)]
        gx = [k.sb("gx%d" % i, [128, 512]) for i in range(2)]
        ysb = k.sb("ysb", [128, 512])
        gel = k.sb("gel", [128, 512])
        gT = k.sb("gT", [128, 4, 128])
        sso = k.sb("sso", [128, 512])

        def glu_tail(psY, P, out_buf, out_writer):
            k.op("act", lambda: A.copy(ysb[0:P, :], psY[0:P, :]), reads=[psY], writes=[ysb])
            gelu_tanh(gel, ysb, sso, P)
            ps = k.nextps()
            for kk in range(4):
                k.op("pe", lambda kk=kk, ps=ps: PE.transpose(ps[:, kk * P:(kk + 1) * P], gel[0:P, kk * 128:(kk + 1) * 128], ident[0:P, 0:P]),
                     reads=[gel, ident], writes=[ps], pe_acc=True)
            k.op("dve", lambda ps=ps: V.tensor_copy(gT[:, :, 0:P], ps[:, 0:4 * P].rearrange("p (a b) -> p a b", b=P)), reads=[ps], writes=[gT])
            ps2 = k.nextps()
            for kk in range(4):
                k.op("pe", lambda kk=kk, ps2=ps2: PE.matmul(ps2[0:P, :], gT[:, kk, 0:P], w_glu[:, kk, :], start=(kk == 0), stop=False),
                     reads=[gT, w_glu], writes=[ps2], pe_acc=True)
            k.op("pe", lambda ps2=ps2: PE.matmul(ps2[0:P, :], ones_row[0:1, 0:P], b_glu[0:1, :], start=False, stop=True),
                 reads=[ones_row, b_glu], writes=[ps2], pe_acc=True)
            k.op("act", lambda ps2=ps2: A.activation(sso[0:P, :], ps2[0:P, :], AF.Sigmoid), reads=[ps2], writes=[sso])
            k.op("dve", lambda: V.tensor_tensor(out_buf[0:P, :], sso[0:P, :], gel[0:P, :], op=ALU.mult), reads=[sso, gel], writes=[out_buf])

        if stage >= 3:
          psY = [k.psb[i] for i in range(4)]
          k.psi = 4
          def nextps_hi():
              b = k.psb[4 + (k.psi % 4)]
              k.psi += 1
              return b
          old_nextps = k.nextps
          for gi in range(NOWN * 128 // 512):
            k.nextps = nextps_hi
            ug = uTg[gi % 2]
            g = {}
            for ut in range(4):
                k.dma("sp", ug[:, ut, :], zT_uo[ut, :, gi * 512:(gi + 1) * 512], reads=[zT_uo], writes=[ug], grp=g)
            for ut in range(4):
                for blk in range(4):
                    k.op("pe", lambda ut=ut, blk=blk: PE.matmul(psY[blk][:, ut * 128:(ut + 1) * 128], ug[:, ut, blk * 128:(blk + 1) * 128], diagD[:, ut, :],
                                                                start=(ut == 0), stop=False, skip_group_check=True),
                         reads=[ug, diagD], writes=[psY[blk]], pe_acc=True)
            for s in range(16):
                b = bu_group(zT_uo, gi, s, s % 2)
                hb_ = hbuf[s % 2]
                pmr, pmi = bc4(Pm[0][:, s, :]), bc4(Pm[1][:, s, :])
                ppr, ppi = bc4(Pp[0][:, s, :]), bc4(Pp[1][:, s, :])
                k.op("dve", lambda: V.tensor_tensor(v4(pr[0][:]), v4(b[0][:]), pmr, op=ALU.mult), reads=[b[0], Pm[0]], writes=[pr[0]])
                k.op("dve", lambda: V.tensor_tensor(v4(pr[1][:]), v4(b[1][:]), pmi, op=ALU.mult), reads=[b[1], Pm[1]], writes=[pr[1]])
                k.op("dve", lambda: V.tensor_tensor(pr[0][:], pr[0][:], pr[1][:], op=ALU.subtract), reads=[pr[0], pr[1]], writes=[pr[0]])
                k.op("dve", lambda: V.tensor_tensor(v4(pr[2][:]), v4(b[0][:]), pmi, op=ALU.mult), reads=[b[0], Pm[1]], writes=[pr[2]])
                k.op("dve", lambda: V.tensor_tensor(v4(pr[3][:]), v4(b[1][:]), pmr, op=ALU.mult), reads=[b[1], Pm[0]], writes=[pr[3]])
                k.op("dve", lambda: V.tensor_tensor(pr[2][:], pr[2][:], pr[3][:], op=ALU.add), reads=[pr[2], pr[3]], writes=[pr[2]])
                for blk in range(4):
                    n = gi * 4 + blk
                    sl = slice(blk * 128, (blk + 1) * 128)
                    k.op("dve", lambda sl=sl, n=n: V.tensor_tensor_scan(gx[0][:, sl], ones_row_b[:, 0:128], pr[0][:, sl], LH[0][:, s, n:n + 1], op0=ALU.mult, op1=ALU.add),
                         reads=[pr[0], LH[0], ones_b], writes=[gx[0]])
                    k.op("dve", lambda sl=sl, n=n: V.tensor_tensor_scan(gx[1][:, sl], ones_row_b[:, 0:128], pr[2][:, sl], LH[1][:, s, n:n + 1], op0=ALU.mult, op1=ALU.add),
                         reads=[pr[2], LH[1], ones_b], writes=[gx[1]])
                k.op("dve", lambda: V.tensor_tensor(v4(pr[0][:]), v4(gx[0][:]), ppr, op=ALU.mult), reads=[gx[0], Pp[0]], writes=[pr[0]])
                k.op("dve", lambda: V.tensor_tensor(v4(pr[1][:]), v4(gx[1][:]), ppi, op=ALU.mult), reads=[gx[1], Pp[1]], writes=[pr[1]])
                k.op("dve", lambda: V.tensor_tensor(hb_[0][:], pr[0][:], pr[1][:], op=ALU.subtract), reads=[pr[0], pr[1]], writes=[hb_[0]])
                k.op("dve", lambda: V.tensor_tensor(v4(pr[2][:]), v4(gx[0][:]), ppi, op=ALU.mult), reads=[gx[0], Pp[1]], writes=[pr[2]])
                k.op("dve", lambda: V.tensor_tensor(v4(pr[3][:]), v4(gx[1][:]), ppr, op=ALU.mult), reads=[gx[1], Pp[0]], writes=[pr[3]])
                k.op("dve", lambda: V.tensor_tensor(hb_[1][:], pr[2][:], pr[3][:], op=ALU.add), reads=[pr[2], pr[3]], writes=[hb_[1]])
                for blk in range(4):
                    for i in range(2):
                        k.op("pe", lambda blk=blk, i=i: PE.matmul(psY[blk][:, s * 32:(s + 1) * 32], hb_[i][:, blk * 128:(blk + 1) * 128], ctp[i][:, s, :],
                                                                  start=False, stop=(i == 1), skip_group_check=True),
                             reads=[hb_[i], ctp[i]], writes=[psY[blk]], pe_acc=True)
            for blk in range(4):
                n = gi * 4 + blk
                glu_tail(psY[blk], 128, sso, None)
                k.dma("pool", ssm_out_d[n * 128:(n + 1) * 128, :], sso[:], reads=[sso], writes=[ssm_out_d])
          k.nextps = old_nextps
          k.psi = 0

        h0c = [k.sb("h0c%d" % i, [128, 16, NS]) for i in range(2)]
        hs = [k.sb("hs%d" % i, [128, 16, NS]) for i in range(2)]
        for i in range(2):
            k.dma("sp", h0c[i][:], h0c_d[i], reads=[h0c_d], writes=[h0c[i]])
        psb_ = [k.nextps(), k.nextps()]
        for i in range(2):
            for s in range(16):
                k.op("pe", lambda i=i, s=s: PE.matmul(psb_[i][:, s * NS:(s + 1) * NS], bbT[i][:, s, :], zsT[:, s // 4, :], start=True, stop=True),
                     reads=[bbT[i], zsT], writes=[psb_[i]], pe_acc=True)
        lrs = cc["lr"][:].unsqueeze(2).to_broadcast([128, 16, NS])
        lis = cc["li"][:].unsqueeze(2).to_broadcast([128, 16, NS])
        tmpS = k.sb("tmpS", [128, 2, 16, NS])
        cmul_small(hs[0][:], hs[1][:], h0c[0][:], h0c[1][:], lrs, lis, [h0c[0], h0c[1], cc["lr"], cc["li"]], [hs[0], hs[1]],
                   tmpS[:, 0], tmpS[:, 1], tmpS)
        for i in range(2):
            k.op("dve", lambda i=i: V.tensor_tensor(hs[i][:], hs[i][:], psb_[i][:, 0:16 * NS].rearrange("p (a b) -> p a b", b=NS), op=ALU.add),
                 reads=[hs[i], psb_[i]], writes=[hs[i]])
            k.dma("pool", o_ssm_s[i], hs[i][:], reads=[hs[i]], writes=[o_ssm_s])
        psy = k.nextps()
        for ut in range(4):
            k.op("pe", lambda ut=ut: PE.matmul(psy[0:NS, ut * 128:(ut + 1) * 128], zsT[:, ut, :], diagD[:, ut, :], start=(ut == 0), stop=False, skip_group_check=True),
                 reads=[zsT, diagD], writes=[psy], pe_acc=True)
        for s in range(16):
            for i in range(2):
                k.op("pe", lambda s=s, i=i: PE.matmul(psy[0:NS, s * 32:(s + 1) * 32], hs[i][:, s, :], ctp[i][:, s, :], start=False, stop=(i == 1), skip_group_check=True),
                     reads=[hs[i], ctp[i]], writes=[psy], pe_acc=True)
        glu_tail(psy, NS, ssm_out_s, None)

    NEG = -1.0e30
    if stage >= 4:
      with k.scope():
        KT_sel = k.sb("KT_sel", [128, T]); KT_win = k.sb("KT_win", [128, T])
        V_sel = k.sb("V_sel", [128, NB, 128]); V_win = k.sb("V_win", [128, NB, 128])
        k.dma("sp", KT_sel[:], zT_kv[2], reads=[zT_kv], writes=[KT_sel])
        k.dma("act", KT_win[:], zT_kv[4], reads=[zT_kv], writes=[KT_win])
        k.dma("sp", V_sel[:], o_sel_p.t.rearrange("(n p) c -> p n c", p=128)[:, :, 128:256], reads=[o_sel_p], writes=[V_sel])
        k.dma("act", V_win[:], o_win_p.t.rearrange("(n p) c -> p n c", p=128)[:, :, 128:256], reads=[o_win_p], writes=[V_win])
        CKT = k.sb("CKT", [128, 256])
        CV = k.sb("CV", [128, 2, 128])
        jflip = k.sb("jflip", [128, 128])
        k.dma("sp", jflip[:], jflip_d[:], reads=[jflip_d], writes=[jflip])
        onesr = k.sb("onesr", [1, 128])
        k.op("dve", lambda: V.memset(onesr[:], 1.0), writes=[onesr])

        with k.scope():
            bd1 = k.sb("bd1", [128, 64, 128]); bd2 = k.sb("bd2", [128, 2, 128])
            pel = k.sb("pel", [128, 2, 2, 16]); pb1 = k.sb("pb1", [128, 2]); pb2 = k.sb("pb2", [128, 2]); pb2r = k.sb("pb2r", [1, 128])
            k.dma("sp", bd1[:], bd1_d[:], reads=[bd1_d], writes=[bd1])
            for (sb_, d_) in ((bd2, bd2_d), (pel, pel_d), (pb1, pb1_d), (pb2, pb2_d), (pb2r, pb2r_d)):
                k.dma("act", sb_[:], d_[:], reads=[d_], writes=[sb_])
            XTc = [k.sb("XTc%d" % c, [128, T]) for c in range(2)]
            for c in range(2):
                k.dma("sp", XTc[c][:], zT_kv[c], reads=[zT_kv], writes=[XTc[c]])
            hdn = [k.sb("hdn%d" % c, [128, 256]) for c in range(2)]
            htmp = k.sb("htmp", [128, 256]); hpre = k.sb("hpre", [128, 256])
            pbias = k.sb("pbias", [128, 2])
            for c in range(2):
                psb_ = k.nextps()
                for half in range(2):
                    for j in range(16):
                        k.op("pe", lambda c=c, half=half, j=j: PE.matmul(psb_[:, 0:1], bd1[:, (c * 2 + half) * 16 + j, :], pel[:, c, half, j:j + 1],
                                                                         start=(half == 0 and j == 0), stop=(half == 1 and j == 15)),
                             reads=[bd1, pel], writes=[psb_], pe_acc=True)
                k.op("dve", lambda c=c: V.tensor_tensor(pbias[:, c:c + 1], psb_[:, 0:1], pb1[:, c:c + 1], op=ALU.add), reads=[psb_, pb1], writes=[pbias])
                ps = k.nextps()
                xv = XTc[c][:].rearrange("p (n j) -> p n j", j=16)
                for half in range(2):
                    for j in range(16):
                        k.op("pe", lambda c=c, half=half, j=j, ps=ps: PE.matmul(ps[:, 0:255], bd1[:, (c * 2 + half) * 16 + j, :], xv[:, half:half + 255, j],
                                                                                start=(half == 0 and j == 0), stop=(half == 1 and j == 15)),
                             reads=[bd1, XTc[c]], writes=[ps], pe_acc=True)
                k.op("dve", lambda c=c: V.memset(hpre[:], 0.0), writes=[hpre])
                k.op("act", lambda c=c, ps=ps: A.activation(hpre[:, 0:255], ps[:, 0:255], AF.Identity, bias=pbias[:, c:c + 1], scale=1.0), reads=[ps, pbias], writes=[hpre])
                gelu_tanh(hdn[c], hpre, htmp, 128)
            ps = k.nextps()
            k.op("pe", lambda: PE.matmul(ps[:, 0:256], bd2[:, 0, :], hdn[0][:], start=True, stop=True), reads=[bd2, hdn[0]], writes=[ps], pe_acc=True)
            k.op("act", lambda: A.activation(CKT[:], ps[:, 0:256], AF.Identity, bias=pb2[:, 0:1], scale=1.0), reads=[ps, pb2], writes=[CKT])
            for i in range(2):
                ps = k.nextps()
                k.op("pe", lambda i=i, ps=ps: PE.matmul(ps[:, 0:128], hdn[1][:, i * 128:(i + 1) * 128], bd2[:, 1, :], start=True, stop=False),
                     reads=[bd2, hdn[1]], writes=[ps], pe_acc=True)
                k.op("pe", lambda i=i, ps=ps: PE.matmul(ps[:, 0:128], onesr[0:1, :], pb2r[0:1, :], start=False, stop=True),
                     reads=[onesr, pb2r], writes=[ps], pe_acc=True)
                k.op("dve", lambda i=i, ps=ps: V.tensor_copy(CV[:, i, :], ps[:, 0:128]), reads=[ps], writes=[CV])

        Tsel = k.sb("Tsel", [128, 10, 8, 128])
        Twin = k.sb("Twin", [128, 2, 8, 128])
        Wc = k.sb("Wc", [128, 8, 34 * 8])
        k.op("dve", lambda: V.memset(Wc[:, :, 33 * 8:34 * 8], NEG), writes=[Wc])
        with k.scope():
            tb = k.sb("tb", [33, 8])
            k.op("dve", lambda: V.memset(tb[:], NEG), writes=[tb])
            k.dma("sp", tb[0:32, :], rel_bias_d[:], reads=[rel_bias_d], writes=[tb])
            oh = k.sb("oh", [33, 2, 1408])
            for w in range(2):
                k.dma("act", oh[:, w, :], oh_d[w], reads=[oh_d], writes=[oh])
            Gs = k.sb("Gs", [8, 2, 1408])
            for w in range(2):
                for c0 in range(0, 1408, 512):
                    cn = min(512, 1408 - c0)
                    ps = k.nextps()
                    k.op("pe", lambda w=w, c0=c0, cn=cn, ps=ps: PE.matmul(ps[0:8, 0:cn], tb[:, :], oh[:, w, c0:c0 + cn], start=True, stop=True),
                         reads=[tb, oh], writes=[ps], pe_acc=True)
                    k.op("dve", lambda w=w, c0=c0, cn=cn, ps=ps: V.tensor_copy(Gs[:, w, c0:c0 + cn], ps[0:8, 0:cn]), reads=[ps], writes=[Gs])
                k.dma("pool", G_d[w], Gs[:, w, :], reads=[Gs], writes=[G_d])
            Hk = [k.sb("Hk%d" % i, [128, 8, 128]) for i in range(2)]
            Y0 = 1151
            jobs = [(0, dp, Tsel, dp + 1) for dp in range(-1, 9)] + [(1, 3, Twin, 0), (1, 4, Twin, 1)]
            for ji, (w, dp, dstT, di) in enumerate(jobs):
                hk = Hk[ji % 2]
                off = Y0 - 128 * dp - 127
                src = bass.AP(G_d.t.tensor, w * 8 * 1408 + off, [[1, 128], [1408, 8], [1, 128]])
                k.dma("sp", hk[:], src, reads=[G_d], writes=[hk])
                for hh in range(2):
                    ps = k.nextps()
                    k.op("pe", lambda hh=hh, ps=ps, hk=hk: PE.matmul(ps[:, :], jflip[:], hk[:, hh * 4:(hh + 1) * 4, :].rearrange("p a b -> p (a b)"), start=True, stop=True),
                         reads=[jflip, hk], writes=[ps], pe_acc=True)
                    k.op("act" if hh else "dve",
                         (lambda hh=hh, ps=ps, dstT=dstT, di=di: A.copy(dstT[:, di, hh * 4:(hh + 1) * 4, :].rearrange("p a b -> p (a b)"), ps[:, :])) if hh else
                         (lambda hh=hh, ps=ps, dstT=dstT, di=di: V.tensor_copy(dstT[:, di, hh * 4:(hh + 1) * 4, :].rearrange("p a b -> p (a b)"), ps[:, :])),
                         reads=[ps], writes=[dstT])
            k.op("dve", lambda: V.tensor_copy(Wc[:, :, 0:192].rearrange("p h (e c) -> p h e c", c=8),
                                              Tsel[:, 9, :, 15:128:16].unsqueeze(2).to_broadcast([128, 8, 24, 8])), reads=[Tsel], writes=[Wc])
            for e in range(24, 33):
                dp = 31 - e
                k.op("dve", lambda e=e, dp=dp: V.tensor_copy(Wc[:, :, e * 8:(e + 1) * 8], Tsel[:, dp + 1, :, 15:128:16]), reads=[Tsel], writes=[Wc])

        qT = [k.sb("qT%d" % i, [128, 4, 128]) for i in range(2)]
        gat = [k.sb("gat%d" % i, [128, 24]) for i in range(2)]
        selc = [k.sb("selc%d" % i, [128, 3, 64]) for i in range(2)]
        S2 = [k.sb("S%d" % i, [128, T]) for i in range(2)]
        Sw2 = [k.sb("Sw%d" % i, [128, 768]) for i in range(2)]
        Pc2 = [k.sb("Pc%d" % i, [128, 256]) for i in range(2)]
        imp = k.sb("imp", [128, 264])
        sblk = k.sb("sblk", [128, 64]); sb2 = k.sb("sb2", [128, 64]); negm = k.sb("negm", [128, 64])
        m8 = k.sb("m8", [128, 8])
        sm2 = [k.sb("smalls%d" % i, [128, 16]) for i in range(2)]
        sm = sm2[0]; S = S2[0]; Sw = Sw2[0]; Pc = Pc2[0]
        PT = [k.sb("PT%d" % i, [128, 512]) for i in range(2)]
        attb = [k.sb("attb%d" % i, [128, 512]) for i in range(2)]
        psO = [k.psb[0], k.psb[1]]; psS = [k.psb[2], k.psb[3]]; psT = [k.psb[4], k.psb[5]]
        cnt = {"s": 0, "t": 0, "o": 0}

        def softmax_rows(Sap, ncols, col_max, col_neg, col_sum, col_rinv, reads):
            k.op("dve", lambda: V.tensor_reduce(sm[:, col_max:col_max + 1], Sap, axis=AX.X, op=ALU.max), reads=reads, writes=[sm])
            k.op("dve", lambda: V.tensor_scalar(sm[:, col_neg:col_neg + 1], sm[:, col_max:col_max + 1], -1.0e4, -1.0, op0=ALU.max, op1=ALU.mult), reads=[sm], writes=[sm])
            k.op("dve", lambda: V.memset(sm[:, col_sum:col_sum + 1], 0.0), writes=[sm])
            k.op("act", lambda: A.activation(Sap, Sap, AF.Exp, bias=sm[:, col_neg:col_neg + 1], scale=1.0, accum_out=sm[:, col_sum:col_sum + 1]),
                 reads=reads + [sm], writes=reads + [sm])
            k.op("dve", lambda: V.tensor_scalar(sm[:, col_rinv:col_rinv + 1], sm[:, col_sum:col_sum + 1], 1.0e-30, None, op0=ALU.max), reads=[sm], writes=[sm])
            k.op("dve", lambda: V.reciprocal(sm[:, col_rinv:col_rinv + 1], sm[:, col_rinv:col_rinv + 1]), reads=[sm], writes=[sm])

        def pv_accum(Pbuf, ntiles, vfn, po, ocol):
            for c0 in range(0, ntiles, 4):
                cn = min(4, ntiles - c0)
                pt = psT[cnt["t"] % 2]; ptb = PT[cnt["t"] % 2]; cnt["t"] += 1
                for i in range(cn):
                    k.op("pe", lambda i=i, pt=pt: PE.transpose(pt[:, i * 128:(i + 1) * 128], Pbuf[:, (c0 + i) * 128:(c0 + i + 1) * 128], ident[:]),
                         reads=[Pbuf, ident], writes=[pt], pe_acc=True)
                if cnt["t"] % 2:
                    k.op("act", lambda pt=pt, ptb=ptb, cn=cn: A.copy(ptb[:, 0:cn * 128], pt[:, 0:cn * 128]), reads=[pt], writes=[ptb])
                else:
                    k.op("dve", lambda pt=pt, ptb=ptb, cn=cn: V.tensor_copy(ptb[:, 0:cn * 128], pt[:, 0:cn * 128]), reads=[pt], writes=[ptb])
                for i in range(cn):
                    jt = c0 + i
                    k.op("pe", lambda i=i, jt=jt, ptb=ptb: PE.matmul(po[:, ocol:ocol + 64], ptb[:, i * 128:(i + 1) * 128], vfn(jt),
                                                                     start=(jt == 0), stop=(jt == ntiles - 1)),
                         reads=[ptb] + vfn.bufs, writes=[po], pe_acc=True)

        for n in range(NOWN):
            qb, gb, scb, ab = qT[n % 2], gat[n % 2], selc[n % 2], attb[n % 2]
            g = {}
            for t in range(4):
                k.dma("sp", qb[:, t, :], zT_q[t, :, n * 128:(n + 1) * 128], reads=[zT_q], writes=[qb], grp=g)
            k.dma("sp", gb[:], gates_d[n * 128:(n + 1) * 128, :], reads=[gates_d], writes=[gb])
            for i in range(3):
                k.dma("act", scb[:, i, :], selc_d[i, n], reads=[selc_d], writes=[scb])
            NT = 2 * n + 2
            NKc = 8 * NT
            woff = (31 - 2 * n) * 8 + 1
            for h in range(2):
                hp = slice(64 * h, 64 * h + 64)
                k.op("dve", lambda: V.memset(imp[:], 0.0), writes=[imp])
                pos = []
                for gq in range(4):
                    hh = 4 * h + gq
                    sm = sm2[gq % 2]; Pc = Pc2[gq % 2]
                    po = psO[gq // 2]; ob = (gq % 2) * 192
                    pos.append((po, ob))
                    ps = psS[cnt["s"] % 2]; cnt["s"] += 1
                    k.op("pe", lambda ps=ps, gq=gq: PE.matmul(ps[:, 0:NKc], qb[hp, gq, :], CKT[hp, 0:NKc], start=True, stop=True),
                         reads=[qb, CKT], writes=[ps], pe_acc=True)
                    k.op("dve", lambda ps=ps, hh=hh: V.scalar_tensor_tensor(Pc[:, 0:NKc], ps[:, 0:NKc], 0.125, Wc[:, hh, woff:woff + NKc], op0=ALU.mult, op1=ALU.add),
                         reads=[ps, Wc], writes=[Pc])
                    softmax_rows(Pc[:, 0:NKc], NKc, 0, 1, 2, 3, [Pc])
                    k.op("dve", lambda: V.tensor_scalar(Pc[:, 0:NKc], Pc[:, 0:NKc], sm[:, 3:4], None, op0=ALU.mult), reads=[Pc, sm], writes=[Pc])
                    k.op("dve", lambda: V.tensor_tensor(imp[:, 1:1 + NKc], imp[:, 1:1 + NKc], Pc[:, 0:NKc], op=ALU.add), reads=[imp, Pc], writes=[imp])
                    nct = (NKc + 127) // 128
                    if NKc < nct * 128:
                        k.op("dve", lambda: V.memset(Pc[:, NKc:nct * 128], 0.0), writes=[Pc])
                    vf = lambda jt: CV[:, jt, hp]
                    vf.bufs = [CV]
                    pv_accum(Pc, nct, vf, po, ob)
                k.op("dve", lambda: V.tensor_reduce(sblk[:], imp[:, 0:256].rearrange("p (j m) -> p j m", m=4), axis=AX.X, op=ALU.add), reads=[imp], writes=[sblk])
                k.op("dve", lambda: V.tensor_tensor(sblk[:], sblk[:], imp[:, 4:260:4], op=ALU.add), reads=[sblk, imp], writes=[sblk])
                k.op("dve", lambda: V.tensor_tensor(sblk[:], sblk[:], scb[:, 0, :], op=ALU.mult), reads=[sblk, scb], writes=[sblk])
                k.op("dve", lambda: V.tensor_tensor(sblk[:], sblk[:], scb[:, 1, :], op=ALU.add), reads=[sblk, scb], writes=[sblk])
                k.op("dve", lambda: V.max(m8[:], sblk[:]), reads=[sblk], writes=[m8])
                k.op("dve", lambda: V.match_replace(sb2[:], m8[:], sblk[:], NEG), reads=[m8, sblk], writes=[sb2])
                k.op("dve", lambda: V.max(m8[:], sb2[:]), reads=[sb2], writes=[m8])
                k.op("dve", lambda: V.scalar_tensor_tensor(sb2[:], sblk[:], m8[:, 7:8], scb[:, 2, :], op0=ALU.is_ge, op1=ALU.mult), reads=[sblk, m8, scb], writes=[sb2])
                k.op("dve", lambda: V.tensor_scalar(negm[:], sb2[:], 1.0, 1.0e30, op0=ALU.subtract, op1=ALU.mult), reads=[sb2], writes=[negm])
                for gq in range(4):
                    hh = 4 * h + gq
                    sm = sm2[gq % 2]; S = S2[gq % 2]; Sw = Sw2[gq % 2]
                    po, ob = pos[gq]
                    for c0 in range(0, NT, 4):
                        cn = min(4, NT - c0)
                        ps = psS[cnt["s"] % 2]; cnt["s"] += 1
                        k.op("pe", lambda ps=ps, gq=gq, c0=c0, cn=cn: PE.matmul(ps[:, 0:cn * 128], qb[hp, gq, :], KT_sel[hp, c0 * 128:(c0 + cn) * 128], start=True, stop=True),
                             reads=[qb, KT_sel], writes=[ps], pe_acc=True)
                        for i in range(cn):
                            jt = c0 + i
                            di = min(2 * n - jt, 8) + 1
                            k.op("dve", lambda ps=ps, i=i, jt=jt, di=di, hh=hh: V.scalar_tensor_tensor(
                                S[:, jt * 128:(jt + 1) * 128], ps[:, i * 128:(i + 1) * 128], 0.125, Tsel[:, di, hh, :], op0=ALU.mult, op1=ALU.add),
                                 reads=[ps, Tsel], writes=[S])
                    k.op("dve", lambda: V.tensor_tensor(S[:, 0:NT * 128].rearrange("p (b c) -> p b c", c=64), S[:, 0:NT * 128].rearrange("p (b c) -> p b c", c=64),
                                                        negm[:, 0:2 * NT].unsqueeze(2).to_broadcast([128, 2 * NT, 64]), op=ALU.add), reads=[S, negm], writes=[S])
                    softmax_rows(S[:, 0:NT * 128], NT * 128, 4, 5, 6, 7, [S])
                    vf = lambda jt: V_sel[:, jt, hp]
                    vf.bufs = [V_sel]
                    pv_accum(S, NT, vf, po, ob + 64)
                    wt = [jt for jt in range(2 * n - 4, 2 * n + 2) if jt >= 0]
                    nw = len(wt)
                    for c0 in range(0, nw, 4):
                        cn = min(4, nw - c0)
                        ps = psS[cnt["s"] % 2]; cnt["s"] += 1
                        k.op("pe", lambda ps=ps, gq=gq, c0=c0, cn=cn: PE.matmul(ps[:, 0:cn * 128], qb[hp, gq, :], KT_win[hp, wt[c0] * 128:(wt[c0] + cn) * 128], start=True, stop=True),
                             reads=[qb, KT_win], writes=[ps], pe_acc=True)
                        for i in range(cn):
                            jt = wt[c0 + i]
                            dp = 2 * n - jt
                            bt = Twin[:, dp - 3, hh, :] if dp >= 3 else Tsel[:, dp + 1, hh, :]
                            k.op("dve", lambda ps=ps, i=i, c0=c0, bt=bt: V.scalar_tensor_tensor(
                                Sw[:, (c0 + i) * 128:(c0 + i + 1) * 128], ps[:, i * 128:(i + 1) * 128], 0.125, bt, op0=ALU.mult, op1=ALU.add),
                                 reads=[ps, Tsel, Twin], writes=[Sw])
                    softmax_rows(Sw[:, 0:nw * 128], nw * 128, 8, 9, 10, 11, [Sw])
                    vf = lambda jt: V_win[:, wt[jt], hp]
                    vf.bufs = [V_win]
                    pv_accum(Sw, nw, vf, po, ob + 128)
                    k.op("dve", lambda hh=hh: V.tensor_tensor(sm[:, 12:13], sm[:, 7:8], gb[:, hh * 3 + 1:hh * 3 + 2], op=ALU.mult), reads=[sm, gb], writes=[sm])
                    k.op("dve", lambda hh=hh: V.tensor_tensor(sm[:, 13:14], sm[:, 11:12], gb[:, hh * 3 + 2:hh * 3 + 3], op=ALU.mult), reads=[sm, gb], writes=[sm])
                    oc = ab[:, hh * 64:(hh + 1) * 64]
                    k.op("dve", lambda hh=hh, po=po, oc=oc, ob=ob: V.tensor_scalar(oc, po[:, ob:ob + 64], gb[:, hh * 3:hh * 3 + 1], None, op0=ALU.mult), reads=[po, gb], writes=[ab])
                    k.op("dve", lambda po=po, oc=oc: V.scalar_tensor_tensor(oc, po[:, ob + 64:ob + 128], sm[:, 12:13], oc, op0=ALU.mult, op1=ALU.add), reads=[po, sm, ab], writes=[ab])
                    k.op("dve", lambda po=po, oc=oc: V.scalar_tensor_tensor(oc, po[:, ob + 128:ob + 192], sm[:, 13:14], oc, op0=ALU.mult, op1=ALU.add), reads=[po, sm, ab], writes=[ab])
            k.dma("pool", att_d[n * 128:(n + 1) * 128, :], ab[:], reads=[ab], writes=[att_d])

    if stage >= 6:
      with k.scope():
        def ld(name, d_, shape, dt=F32, q="sp"):
            b = k.sb(name, shape, dt)
            k.dma(q, b[:], d_[:], reads=[d_], writes=[b])
            return b
        bd1 = ld("s_bd1", bd1_d, [128, 64, 128]); bd2 = ld("s_bd2", bd2_d, [128, 2, 128], q="act")
        pel = ld("s_pel", pel_d, [128, 2, 2, 16], q="act"); pb1 = ld("s_pb1", pb1_d, [128, 2], q="act")
        pb2 = ld("s_pb2", pb2_d, [128, 2], q="act"); pb2r = ld("s_pb2r", pb2r_d, [1, 128], q="act")
        ohc = ld("s_ohc", ohc_d, [33, 1024]); ohs = ld("s_ohs", ohs_d, [33, 9, 128], q="act"); ohw = ld("s_ohw", ohw_d, [33, 5, 128], q="act")
        gsum = ld("s_gsum", gsum_d, [8, 2]); gexp = ld("s_gexp", gexp_d, [2, 8]); selcs = ld("s_selcs", selcs_d, [2, 2, 260])
        half2 = ld("s_half2", half2_d, [2, 128]); m01 = ld("s_m01", m01_d, [8, 2]); pidx = ld("s_pidx", pidx_d, [128, 1])
        tb = k.sb("s_tb", [33, 8])
        k.op("dve", lambda: V.memset(tb[:], NEG), writes=[tb])
        k.dma("sp", tb[0:32, :], rel_bias_d[:], reads=[rel_bias_d], writes=[tb])
        onesr = k.sb("s_onesr", [1, 128]); ones8 = k.sb("s_ones8", [8, 128]); onesc = k.sb("s_onesc", [128, 1])
        for b_ in (onesr, ones8, onesc):
            k.op("dve", lambda b_=b_: V.memset(b_[:], 1.0), writes=[b_])
        pti = k.sb("s_pti", [128, NS * 128], I32); ptf = k.sb("s_ptf", [128, NS * 128]); idx = k.sb("s_idx", [128, NS * 128], I32)
        k.dma("sp", pti[:], pt_d[0:1, :].partition_broadcast(128), reads=[pt_d], writes=[pti])
        k.op("dve", lambda: V.tensor_copy(ptf[:], pti[:]), reads=[pti], writes=[ptf])
        k.op("dve", lambda: V.tensor_scalar(ptf[:], ptf[:], 128.0, pidx[:, 0:1], op0=ALU.mult, op1=ALU.add), reads=[ptf, pidx], writes=[ptf])
        k.op("dve", lambda: V.tensor_copy(idx[:], ptf[:]), reads=[ptf], writes=[idx])
        idxK = k.sb("s_idxK", [128, NS * 128], I32); idxV = k.sb("s_idxV", [128, NS * 128], I32)
        k.op("dve", lambda: V.tensor_scalar(ptf[:], ptf[:], 2.0, None, op0=ALU.mult), reads=[ptf], writes=[ptf])
        k.op("dve", lambda: V.tensor_copy(idxK[:], ptf[:]), reads=[ptf], writes=[idxK])
        k.op("dve", lambda: V.tensor_scalar(ptf[:], ptf[:], 1.0, None, op0=ALU.add), reads=[ptf], writes=[ptf])
        k.op("dve", lambda: V.tensor_copy(idxV[:], ptf[:]), reads=[ptf], writes=[idxV])
        pbias = k.sb("s_pbias", [128, 2])
        for c in range(2):
            psb_ = k.nextps()
            for half in range(2):
                for j_ in range(16):
                    k.op("pe", lambda c=c, half=half, j_=j_: PE.matmul(psb_[:, 0:1], bd1[:, (c * 2 + half) * 16 + j_, :], pel[:, c, half, j_:j_ + 1],
                                                                      start=(half == 0 and j_ == 0), stop=(half == 1 and j_ == 15)),
                         reads=[bd1, pel], writes=[psb_], pe_acc=True)
            k.op("dve", lambda c=c: V.tensor_tensor(pbias[:, c:c + 1], psb_[:, 0:1], pb1[:, c:c + 1], op=ALU.add), reads=[psb_, pb1], writes=[pbias])
        Qbd = k.sb("s_Qbd", [128, NS, 8])
        k.op("dve", lambda: V.memset(Qbd[:], 0.0), writes=[Qbd])
        k.op("dve", lambda: V.tensor_scalar(Qbd[0:64, :, 0:4], zsT[0:64, 4:8, :].rearrange("p g j -> p j g"), 0.125, None, op0=ALU.mult), reads=[zsT], writes=[Qbd])
        k.op("dve", lambda: V.tensor_scalar(Qbd[64:128, :, 4:8], zsT[64:128, 4:8, :].rearrange("p g j -> p j g"), 0.125, None, op0=ALU.mult), reads=[zsT], writes=[Qbd])
        gsb = k.sb("s_gsb", [NS, 24]); g8 = k.sb("s_g8", [8, NS, 3])
        k.op("act", lambda: A.activation(gsb[:], zs[:, 1792:1816], AF.Sigmoid), reads=[zs], writes=[gsb])
        k.dma("pool", gts_d[:, :], gsb[:], reads=[gsb], writes=[gts_d])
        k.dma("pool", g8[:], gts_d.t.rearrange("j (h r) -> h j r", r=3), reads=[gts_d], writes=[g8])

        gt_ = [k.sb("s_gt%d" % i, [128, 256]) for i in range(3)]
        CKTs = k.sb("s_CKT", [128, 1024]); CVs = k.sb("s_CV", [128, 8, 128])
        Sc = k.sb("s_Sc", [8, 1024]); PcT = k.sb("s_PcT", [128, 8, 8])
        impP = k.sb("s_impP", [2, 1040]); sbk = k.sb("s_sbk", [2, 260]); sbk2 = k.sb("s_sbk2", [2, 260]); m8s = k.sb("s_m8", [2, 8])
        sel8 = k.sb("s_sel8", [8, 258]); B2 = k.sb("s_B2", [2, 129, 8]); nmT = k.sb("s_nmT", [128, 129, 8])
        ST = k.sb("s_ST", [128, 129, 8]); KTu = k.sb("s_KTu", [128, 512])
        knew = k.sb("s_knew", [128, 128])
        hold = {}
        sms = k.sb("s_sms", [128, 24]); pm = k.sb("s_pm", [128, 8]); dg = k.sb("s_dg", [8, 8]); nb = k.sb("s_nb", [128, 8])
        o8 = k.sb("s_o8", [8, 3, 64]); ot = k.sb("s_ot", [8, 128]); attj = k.sb("s_attj", [8, 64])
        k.op("dve", lambda: V.memset(knew[:], 0.0), writes=[knew])

        def sel_half(dst, src128, reads):
            k.op("dve", lambda: V.tensor_scalar(dst, src128[:, 0:64], m01[:, 0:1], None, op0=ALU.mult), reads=reads + [m01], writes=[o8])
            k.op("dve", lambda: V.scalar_tensor_tensor(dst, src128[:, 64:128], m01[:, 1:2], dst, op0=ALU.mult, op1=ALU.add), reads=reads + [m01, o8], writes=[o8])

        def attend_T(NP, oidx):
            Sv = ST[:, 0:NP, :]
            k.op("dve", lambda: V.tensor_reduce(pm[:], Sv.rearrange("p n h -> p h n"), axis=AX.X, op=ALU.max), reads=[ST], writes=[pm])
            ps = k.nextps()
            k.op("pe", lambda: PE.transpose(ps[0:8, 0:128], pm[:, :], ident[:]), reads=[pm, ident], writes=[ps], pe_acc=True)
            k.op("dve", lambda: V.tensor_reduce(sms[0:8, 0:1], ps[0:8, 0:128], axis=AX.X, op=ALU.max), reads=[ps], writes=[sms])
            k.op("dve", lambda: V.tensor_scalar(sms[0:8, 1:2], sms[0:8, 0:1], -1.0e4, -1.0, op0=ALU.max, op1=ALU.mult), reads=[sms], writes=[sms])
            k.op("dve", lambda: V.tensor_scalar(dg[:], ident[0:8, 0:8], sms[0:8, 1:2], None, op0=ALU.mult), reads=[ident, sms], writes=[dg])
            ps2 = k.nextps()
            k.op("pe", lambda: PE.matmul(ps2[:, 0:8], ones8[:, :], dg[:, :], start=True, stop=True), reads=[ones8, dg], writes=[ps2], pe_acc=True)
            k.op("dve", lambda: V.tensor_copy(nb[:], ps2[:, 0:8]), reads=[ps2], writes=[nb])
            k.op("dve", lambda: V.tensor_tensor(Sv, Sv, nb[:].unsqueeze(1).to_broadcast([128, NP, 8]), op=ALU.add), reads=[ST, nb], writes=[ST])
            k.op("act", lambda: A.activation(Sv, Sv, AF.Exp), reads=[ST], writes=[ST])
            k.op("dve", lambda: V.tensor_reduce(pm[:], Sv.rearrange("p n h -> p h n"), axis=AX.X, op=ALU.add), reads=[ST], writes=[pm])
            ps3 = k.nextps()
            k.op("pe", lambda: PE.matmul(ps3[0:8, 0:1], pm[:, :], onesc[:, :], start=True, stop=True), reads=[pm, onesc], writes=[ps3], pe_acc=True)
            k.op("dve", lambda: V.tensor_scalar(sms[0:8, 2:3], ps3[0:8, 0:1], 1.0e-30, None, op0=ALU.max), reads=[ps3], writes=[sms])
            k.op("dve", lambda: V.reciprocal(sms[0:8, 2:3], sms[0:8, 2:3]), reads=[sms], writes=[sms])
            ps4 = k.nextps()
            for pg in range(NP):
                k.op("pe", lambda pg=pg: PE.matmul(ps4[0:8, 0:128], ST[:, pg, :], hold["V_all"][:, pg, :], start=(pg == 0), stop=(pg == NP - 1)),
                     reads=[ST, hold["V_all"]], writes=[ps4], pe_acc=True)
            k.op("dve", lambda: V.tensor_scalar(ot[:], ps4[0:8, 0:128], sms[0:8, 2:3], None, op0=ALU.mult), reads=[ps4, sms], writes=[ot])
            sel_half(o8[:, oidx, :], ot, [ot])

        def score_pages(j, kt_tiles, pg0, oh_idx_fn):
            npg = len(kt_tiles)
            for c0 in range(0, npg, 64):
                cn = min(64, npg - c0)
                ps = k.nextps()
                for i in range(cn):
                    k.op("pe", lambda i=i, ps=ps: PE.matmul(ps[:, i * 8:(i + 1) * 8], kt_tiles[c0 + i][0], Qbd[:, j, :], start=True, stop=False),
                         reads=[kt_tiles[c0 + i][1], Qbd], writes=[ps], pe_acc=True)
                    oa, ob_ = oh_idx_fn(pg0 + c0 + i)
                    k.op("pe", lambda i=i, ps=ps, oa=oa: PE.matmul(ps[:, i * 8:(i + 1) * 8], oa, tb[:, :], start=False, stop=True),
                         reads=[ob_, tb], writes=[ps], pe_acc=True)
                k.op("dve", lambda ps=ps, c0=c0, cn=cn: V.tensor_copy(ST[:, pg0 + c0:pg0 + c0 + cn, :], ps[:, 0:cn * 8].rearrange("p (n h) -> p n h", h=8)),
                     reads=[ps], writes=[ST])

        for j in range(NS):
          with k.scope():
            XT = [k.sb("s_XT%d" % c, [128, 4096]) for c in range(2)]
            fs = [[k.sb("s_fs%d_%d" % (c, hf), [128, 1024]) for hf in range(2)] for c in range(2)]
            hdn = [k.sb("s_hdn%d" % c, [128, 1024]) for c in range(2)]
            htmp = k.sb("s_htmp", [128, 1024])
            for u in range(4):
                for pg4 in range(0, 32, 2):
                    psk = k.nextps(); psv = k.nextps()
                    for i in range(2):
                        pg = u * 32 + pg4 + i
                        gtile = gt_[pg % 3]
                        k.dma("pool", None, None, reads=[pool_cmp, idx], writes=[gtile],
                              fn=lambda pg=pg, gtile=gtile: G.indirect_dma_start(out=gtile[:], out_offset=None, in_=pool_cmp[:, :],
                                                                                 in_offset=bass.IndirectOffsetOnAxis(ap=idx[:, j * 128 + pg:j * 128 + pg + 1], axis=0)))
                        k.op("pe", lambda i=i, gtile=gtile: PE.transpose(psk[:, i * 128:(i + 1) * 128], gtile[:, 0:128], ident[:]), reads=[gtile, ident], writes=[psk], pe_acc=True)
                        k.op("pe", lambda i=i, gtile=gtile: PE.transpose(psv[:, i * 128:(i + 1) * 128], gtile[:, 128:256], ident[:]), reads=[gtile, ident], writes=[psv], pe_acc=True)
                    o0 = pg4 * 128
                    k.op("dve", lambda psk=psk, o0=o0: V.tensor_copy(XT[0][:, o0:o0 + 256], psk[:, 0:256]), reads=[psk], writes=[XT[0]])
                    k.op("act", lambda psv=psv, o0=o0: A.copy(XT[1][:, o0:o0 + 256], psv[:, 0:256]), reads=[psv], writes=[XT[1]])
                for c in range(2):
                    xv = XT[c][:].rearrange("p (n j) -> p n j", j=16)
                    for half in range(2):
                        ps = k.nextps()
                        for j_ in range(16):
                            k.op("pe", lambda c=c, half=half, j_=j_, ps=ps: PE.matmul(ps[:, 0:256], bd1[:, (c * 2 + half) * 16 + j_, :], xv[:, :, j_], start=(j_ == 0), stop=(j_ == 15)),
                                 reads=[bd1, XT[c]], writes=[ps], pe_acc=True)
                        if half:
                            k.op("act", lambda ps=ps, c=c, half=half: A.copy(fs[c][half][:, u * 256:(u + 1) * 256], ps[:, 0:256]), reads=[ps], writes=[fs[c][half]])
                        else:
                            k.op("dve", lambda ps=ps, c=c, half=half: V.tensor_copy(fs[c][half][:, u * 256:(u + 1) * 256], ps[:, 0:256]), reads=[ps], writes=[fs[c][half]])
            for c in range(2):
                k.op("dve", lambda c=c: V.memset(fs[c][0][:, 1023:1024], 0.0), writes=[fs[c][0]])
                k.op("dve", lambda c=c: V.tensor_tensor(fs[c][0][:, 0:1023], fs[c][0][:, 0:1023], fs[c][1][:, 1:1024], op=ALU.add), reads=[fs[c][0], fs[c][1]], writes=[fs[c][0]])
                k.op("act", lambda c=c: A.activation(fs[c][0][:, 0:1023], fs[c][0][:, 0:1023], AF.Identity, bias=pbias[:, c:c + 1], scale=1.0), reads=[fs[c][0], pbias], writes=[fs[c][0]])
                gelu_tanh(hdn[c], fs[c][0], htmp, 128)
            for hf in range(2):
                ps = k.nextps()
                k.op("pe", lambda hf=hf, ps=ps: PE.matmul(ps[:, :], bd2[:, 0, :], hdn[0][:, hf * 512:(hf + 1) * 512], start=True, stop=True), reads=[bd2, hdn[0]], writes=[ps], pe_acc=True)
                k.op("act", lambda hf=hf, ps=ps: A.activation(CKTs[:, hf * 512:(hf + 1) * 512], ps[:, :], AF.Identity, bias=pb2[:, 0:1], scale=1.0), reads=[ps, pb2], writes=[CKTs])
            for i in range(8):
                ps = k.nextps()
                k.op("pe", lambda i=i, ps=ps: PE.matmul(ps[:, 0:128], hdn[1][:, i * 128:(i + 1) * 128], bd2[:, 1, :], start=True, stop=False), reads=[bd2, hdn[1]], writes=[ps], pe_acc=True)
                k.op("pe", lambda i=i, ps=ps: PE.matmul(ps[:, 0:128], onesr[0:1, :], pb2r[0:1, :], start=False, stop=True), reads=[onesr, pb2r], writes=[ps], pe_acc=True)
                k.op("dve", lambda i=i, ps=ps: V.tensor_copy(CVs[:, i, :], ps[:, 0:128]), reads=[ps], writes=[CVs])
            for hf in range(2):
                ps = k.nextps()
                k.op("pe", lambda hf=hf, ps=ps: PE.matmul(ps[0:8, :], Qbd[:, j, :], CKTs[:, hf * 512:(hf + 1) * 512], start=True, stop=False), reads=[Qbd, CKTs], writes=[ps], pe_acc=True)
                k.op("pe", lambda hf=hf, ps=ps: PE.matmul(ps[0:8, :], tb[:, :], ohc[:, hf * 512:(hf + 1) * 512], start=False, stop=True), reads=[tb, ohc], writes=[ps], pe_acc=True)
                k.op("dve", lambda hf=hf, ps=ps: V.tensor_copy(Sc[:, hf * 512:(hf + 1) * 512], ps[0:8, :]), reads=[ps], writes=[Sc])
            k.op("dve", lambda: V.tensor_reduce(sms[0:8, 4:5], Sc[:], axis=AX.X, op=ALU.max), reads=[Sc], writes=[sms])
            k.op("dve", lambda: V.tensor_scalar(sms[0:8, 5:6], sms[0:8, 4:5], -1.0e4, -1.0, op0=ALU.max, op1=ALU.mult), reads=[sms], writes=[sms])
            k.op("dve", lambda: V.memset(sms[0:8, 6:7], 0.0), writes=[sms])
            k.op("act", lambda: A.activation(Sc[:], Sc[:], AF.Exp, bias=sms[0:8, 5:6], scale=1.0, accum_out=sms[0:8, 6:7]), reads=[Sc, sms], writes=[Sc, sms])
            k.op("dve", lambda: V.tensor_scalar(sms[0:8, 7:8], sms[0:8, 6:7], 1.0e-30, None, op0=ALU.max), reads=[sms], writes=[sms])
            k.op("dve", lambda: V.reciprocal(sms[0:8, 7:8], sms[0:8, 7:8]), reads=[sms], writes=[sms])
            k.op("dve", lambda: V.tensor_scalar(Sc[:], Sc[:], sms[0:8, 7:8], None, op0=ALU.mult), reads=[Sc, sms], writes=[Sc])
            k.op("dve", lambda: V.memset(impP[:], 0.0), writes=[impP])
            for hf in range(2):
                ps = k.nextps()
                k.op("pe", lambda hf=hf, ps=ps: PE.matmul(ps[0:2, :], gsum[:, :], Sc[:, hf * 512:(hf + 1) * 512], start=True, stop=True), reads=[gsum, Sc], writes=[ps], pe_acc=True)
                k.op("dve", lambda hf=hf, ps=ps: V.tensor_copy(impP[:, 1 + hf * 512:1 + (hf + 1) * 512], ps[0:2, :]), reads=[ps], writes=[impP])
            ps = k.nextps()
            for i in range(8):
                k.op("pe", lambda i=i: PE.transpose(ps[:, i * 8:(i + 1) * 8], Sc[:, i * 128:(i + 1) * 128], ident[0:8, 0:8]), reads=[Sc, ident], writes=[ps], pe_acc=True)
            k.op("dve", lambda: V.tensor_copy(PcT[:], ps[:, 0:64].rearrange("p (a b) -> p a b", b=8)), reads=[ps], writes=[PcT])
            ps = k.nextps()
            for i in range(8):
                k.op("pe", lambda i=i: PE.matmul(ps[0:8, 0:128], PcT[:, i, :], CVs[:, i, :], start=(i == 0), stop=(i == 7)), reads=[PcT, CVs], writes=[ps], pe_acc=True)
            k.op("dve", lambda: V.tensor_copy(ot[:], ps[0:8, 0:128]), reads=[ps], writes=[ot])
            sel_half(o8[:, 0, :], ot, [ot])
            k.op("dve", lambda: V.tensor_reduce(sbk[:, 0:257], impP[:, 0:1028].rearrange("p (j m) -> p j m", m=4), axis=AX.X, op=ALU.add), reads=[impP], writes=[sbk])
            k.op("dve", lambda: V.memset(sbk[:, 257:260], 0.0), writes=[sbk])
            k.op("dve", lambda: V.tensor_tensor(sbk[:, 0:257], sbk[:, 0:257], impP[:, 4:1032:4], op=ALU.add), reads=[sbk, impP], writes=[sbk])
            k.op("dve", lambda: V.tensor_tensor(sbk[:], sbk[:], selcs[:, 0, :], op=ALU.mult), reads=[sbk, selcs], writes=[sbk])
            k.op("dve", lambda: V.tensor_tensor(sbk[:], sbk[:], selcs[:, 1, :], op=ALU.add), reads=[sbk, selcs], writes=[sbk])
            k.op("dve", lambda: V.max(m8s[:], sbk[:]), reads=[sbk], writes=[m8s])
            k.op("dve", lambda: V.match_replace(sbk2[:], m8s[:], sbk[:], NEG), reads=[m8s, sbk], writes=[sbk2])
            k.op("dve", lambda: V.max(m8s[:], sbk2[:]), reads=[sbk2], writes=[m8s])
            k.op("dve", lambda: V.tensor_scalar(sbk2[:], sbk[:], m8s[:, 7:8], None, op0=ALU.is_ge), reads=[sbk, m8s], writes=[sbk2])
            ps = k.nextps()
            k.op("pe", lambda: PE.matmul(ps[0:8, 0:258], gexp[:, :], sbk2[:, 0:258], start=True, stop=True), reads=[gexp, sbk2], writes=[ps], pe_acc=True)
            k.op("dve", lambda: V.tensor_copy(sel8[:], ps[0:8, 0:258]), reads=[ps], writes=[sel8])
            k.dma("pool", selm_d[j], sel8[:], reads=[sel8], writes=[selm_d])
            for kk2 in range(2):
                srcap = bass.AP(selm_d.t.tensor, j * 8 * 258 + kk2, [[1, 1], [2, 129], [258, 8]])
                k.dma("pool", B2[kk2:kk2 + 1, :, :], srcap, reads=[selm_d], writes=[B2])
            B2f = B2[:].rearrange("k n h -> k (n h)")
            for c0 in range(0, 1032, 512):
                cn = min(512, 1032 - c0)
                ps = k.nextps()
                k.op("pe", lambda c0=c0, cn=cn, ps=ps: PE.matmul(ps[:, 0:cn], half2[:, :], B2f[:, c0:c0 + cn], start=True, stop=True), reads=[half2, B2], writes=[ps], pe_acc=True)
                k.op("dve", lambda c0=c0, cn=cn, ps=ps: V.tensor_scalar(nmT[:].rearrange("p n h -> p (n h)")[:, c0:c0 + cn], ps[:, 0:cn], 1.0, 1.0e30, op0=ALU.subtract, op1=ALU.mult),
                     reads=[ps], writes=[nmT])
          with k.scope():
            V_all = k.sb("s_Vall", [128, 129, 128])
            hold["V_all"] = V_all
            for pg4 in range(0, 128, 4):
                psk = k.nextps()
                for i in range(4):
                    pg = pg4 + i
                    gtile = gt_[pg % 3]
                    k.dma("pool", None, None, reads=[pool_sel, idxK], writes=[gtile],
                          fn=lambda pg=pg, gtile=gtile: G.indirect_dma_start(out=gtile[:, 0:128], out_offset=None, in_=pool_sel[:, :],
                                                                             in_offset=bass.IndirectOffsetOnAxis(ap=idxK[:, j * 128 + pg:j * 128 + pg + 1], axis=0)))
                    k.dma("pool", None, None, reads=[pool_sel, idxV], writes=[V_all],
                          fn=lambda pg=pg: G.indirect_dma_start(out=V_all[:, pg, :], out_offset=None, in_=pool_sel[:, :],
                                                                in_offset=bass.IndirectOffsetOnAxis(ap=idxV[:, j * 128 + pg:j * 128 + pg + 1], axis=0)))
                    k.op("pe", lambda i=i, gtile=gtile: PE.transpose(psk[:, i * 128:(i + 1) * 128], gtile[:, 0:128], ident[:]), reads=[gtile, ident], writes=[psk], pe_acc=True)
                k.op("act", lambda psk=psk: A.copy(KTu[:], psk[:, :]), reads=[psk], writes=[KTu])
                score_pages(j, [(KTu[:, i * 128:(i + 1) * 128], KTu) for i in range(4)], pg4,
                            lambda pg: ((ohs[:, 0, :], ohs) if pg <= 120 else (ohs[:, pg - 120, :], ohs)))
            k.op("dve", lambda: V.tensor_copy(knew[:, 0:1], zsT[:, 10, j:j + 1]), reads=[zsT], writes=[knew])
            score_pages(j, [(knew[:, :], knew)], 128, lambda pg: (ohs[:, 8, :], ohs))
            k.op("dve", lambda: V.memset(V_all[:, 128, :], 0.0), writes=[V_all])
            k.dma("sp", V_all[0:1, 128, :], o_sel_s[j:j + 1, 128:256], reads=[o_sel_s], writes=[V_all])
            k.op("dve", lambda: V.tensor_tensor(ST[:], ST[:], nmT[:], op=ALU.add), reads=[ST, nmT], writes=[ST])
            attend_T(129, 1)
            k.dma("sp", V_all[:, 0:4, :], state_win[j].rearrange("(n p) c -> p n c", p=128)[:, :, 128:256], reads=[state_win], writes=[V_all])
            wk = k.sb("s_wk%d" % j, [128, 4, 128])
            k.dma("act", wk[:], state_win[j].rearrange("(n p) c -> p n c", p=128)[:, :, 0:128], reads=[state_win], writes=[wk])
            psk = k.nextps()
            for i in range(4):
                k.op("pe", lambda i=i: PE.transpose(psk[:, i * 128:(i + 1) * 128], wk[:, i, :], ident[:]), reads=[wk, ident], writes=[psk], pe_acc=True)
            k.op("act", lambda: A.copy(KTu[:], psk[:, :]), reads=[psk], writes=[KTu])
            score_pages(j, [(KTu[:, i * 128:(i + 1) * 128], KTu) for i in range(4)], 0, lambda pg: (ohw[:, pg, :], ohw))
            k.op("dve", lambda: V.tensor_copy(knew[:, 0:1], zsT[:, 12, j:j + 1]), reads=[zsT], writes=[knew])
            score_pages(j, [(knew[:, :], knew)], 4, lambda pg: (ohw[:, 4, :], ohw))
            k.op("dve", lambda: V.memset(V_all[:, 4, :], 0.0), writes=[V_all])
            k.dma("sp", V_all[0:1, 4, :], o_win_s[j:j + 1, 511, 128:256], reads=[o_win_s], writes=[V_all])
            attend_T(5, 2)
            k.op("dve", lambda: V.tensor_scalar(attj[:], o8[:, 0, :], g8[:, j, 0:1], None, op0=ALU.mult), reads=[o8, g8], writes=[attj])
            k.op("dve", lambda: V.scalar_tensor_tensor(attj[:], o8[:, 1, :], g8[:, j, 1:2], attj[:], op0=ALU.mult, op1=ALU.add), reads=[o8, g8, attj], writes=[attj])
            k.op("dve", lambda: V.scalar_tensor_tensor(attj[:], o8[:, 2, :], g8[:, j, 2:3], attj[:], op0=ALU.mult, op1=ALU.add), reads=[o8, g8, attj], writes=[attj])
            k.dma("pool", atts_d[j].rearrange("(h f) -> h f", f=64), attj[:], reads=[attj], writes=[atts_d])
        k.dma("sp", att_s[:], atts_d[:, :], reads=[atts_d], writes=[att_s])

    DN_ALPHA = float(2.0 ** 0.25)
    NBLK = NOWN + 1
    NTOK = NBLK * 128
    if stage >= 5:
      xm2T_d = k.dram("xm2T_d", [8, 128, NTOK], BF16)
      moe_d = k.dram("moe_d", [NTOK, D])
      with k.scope():
        Gall = k.sb("Gall", [128, NBLK, 32])
        st = k.sb("st5", [128, 2, 6]); mv = k.sb("mv5", [128, 2]); rstd = k.sb("rstd5", [128, 1])

        def build_rowv(names):
            srcs = {"g1": (mT, 16), "g2": (mT, 40), "sc2": (sc2p, 0), "sh2": (mT, 24)}
            rowv = {}
            bcol = k.sb("bcol", [128, 128])
            for nm in names:
                srcb, t0 = srcs[nm]
                rowv[nm + "p"] = k.sb("rowv_" + nm + "p", [128, D]); rowv[nm + "s"] = k.sb("rowv_" + nm + "s", [NS, D])
                for half in range(2):
                    psp = k.nextps(); pss = k.nextps()
                    for q in range(4):
                        tq = half * 4 + q
                        k.op("dve", lambda tq=tq, srcb=srcb, t0=t0: V.tensor_copy(bcol[:], srcb[:, t0 + tq, 0:1].to_broadcast([128, 128])), reads=[srcb], writes=[bcol])
                        k.op("pe", lambda q=q, psp=psp: PE.matmul(psp[:, q * 128:(q + 1) * 128], bcol[:], ident[:], start=True, stop=True),
                             reads=[bcol, ident], writes=[psp], pe_acc=True)
                        k.op("pe", lambda q=q, pss=pss, tq=tq, srcb=srcb, t0=t0: PE.matmul(pss[0:NS, q * 128:(q + 1) * 128], srcb[:, t0 + tq, 1:5], ident[:], start=True, stop=True),
                             reads=[srcb, ident], writes=[pss], pe_acc=True)
                    k.op("act", lambda nm=nm, half=half, psp=psp: A.copy(rowv[nm + "p"][:, half * 512:(half + 1) * 512], psp[:, :]), reads=[psp], writes=[rowv[nm + "p"]])
                    k.op("dve", lambda nm=nm, half=half, pss=pss: V.tensor_copy(rowv[nm + "s"][0:NS, half * 512:(half + 1) * 512], pss[0:NS, :]), reads=[pss], writes=[rowv[nm + "s"]])
            return rowv

        with k.scope():
            rowv = build_rowv(["g1", "sc2", "sh2"])
            lnr = k.sb("lnr", [128, 2, D])
            for i in range(2):
                k.dma("sp", lnr[:, i, :], lnrows_d[i:i + 1, :].partition_broadcast(128), reads=[lnrows_d], writes=[lnr])
            w_router = k.sb("w_router", [128, 8, 32])
            k.dma("sp", w_router[:], w_router_d.t.rearrange("(k p) c -> p k c", p=128), reads=[w_router_d], writes=[w_router])
            b_router = k.sb("b_router", [128, 32])
            k.dma("sp", b_router[:], b_router_d[0:1, :].partition_broadcast(128), reads=[b_router_d], writes=[b_router])
            w_out_sb = k.sb("w_out_sb", [128, 8, D])
            g = {}
            for kk in range(8):
                k.dma("sp" if kk % 2 == 0 else "act", w_out_sb[:, kk, :], w_out_d.t.rearrange("(k p) c -> p k c", p=128)[:, kk, :], reads=[w_out_d], writes=[w_out_sb], grp=g)
            cat = [k.sb("cat%d" % i, [128, D]) for i in range(2)]
            catT = k.sb("catT", [128, 8, 128])
            xtb = [k.sb("xt5_%d" % i, [128, D]) for i in range(2)]
            zb = k.sb("zb", [128, D]); x1b = k.sb("x1b", [128, D]); xnb = k.sb("xnb5", [128, D]); xm2 = k.sb("xm2", [128, D])
            xm2T = [k.sb("xm2T%d" % i, [128, 8, 128]) for i in range(2)]
            xm2Tb = [k.sb("xm2Tb%d" % i, [128, 8, 128], BF16) for i in range(2)]
            lg = k.sb("lg", [128, 32]); m8r = k.sb("m8r", [128, 8]); ew = k.sb("ew", [128, 8]); wk4 = k.sb("wk4", [128, 4])
            ohk = k.sb("ohk", [128, 4, 32])
            xm2T_v = xm2T_d.t.rearrange("k p t -> p k t")

            for blk in range(NBLK):
                samp = (blk == NOWN)
                P = NS if samp else 128
                sfx = "s" if samp else "p"
                cb, xb, xmT = cat[blk % 2], xtb[blk % 2], xm2T[blk % 2]
                if samp:
                    k.op("dve", lambda: V.tensor_copy(cb[0:P, 0:512], ssm_out_s[0:P, :]), reads=[ssm_out_s], writes=[cb])
                    k.op("dve", lambda: V.tensor_copy(cb[0:P, 512:1024], att_s[0:P, :]), reads=[att_s], writes=[cb])
                    k.dma("act", xb[0:P, :], xs_rows[0:P, :], reads=[xs_rows], writes=[xb])
                else:
                    k.dma("sp", cb[:, 0:512], ssm_out_d[blk * 128:(blk + 1) * 128, :], reads=[ssm_out_d], writes=[cb])
                    k.dma("sp", cb[:, 512:1024], att_d[blk * 128:(blk + 1) * 128, :], reads=[att_d], writes=[cb])
                    k.dma("act", xb[:, :], xo[blk * 128:(blk + 1) * 128, :], reads=[xo], writes=[xb])
                for hlf in range(2):
                    ps = k.nextps()
                    for q in range(4):
                        kk = hlf * 4 + q
                        k.op("pe", lambda kk=kk, q=q, ps=ps: PE.transpose(ps[:, q * P:(q + 1) * P], cb[0:P, kk * 128:(kk + 1) * 128], ident[0:P, 0:P]),
                             reads=[cb, ident], writes=[ps], pe_acc=True)
                    k.op("act" if hlf else "dve",
                         (lambda ps=ps, hlf=hlf: A.copy(catT[:, hlf * 4:(hlf + 1) * 4, 0:P], ps[:, 0:4 * P].rearrange("p (a b) -> p a b", b=P))) if hlf else
                         (lambda ps=ps, hlf=hlf: V.tensor_copy(catT[:, hlf * 4:(hlf + 1) * 4, 0:P], ps[:, 0:4 * P].rearrange("p (a b) -> p a b", b=P))),
                         reads=[ps], writes=[catT])
                for hlf in range(2):
                    ps = k.nextps()
                    for kk in range(8):
                        k.op("pe", lambda kk=kk, ps=ps, hlf=hlf: PE.matmul(ps[0:P, :], catT[:, kk, 0:P], w_out_sb[:, kk, hlf * 512:(hlf + 1) * 512], start=(kk == 0), stop=(kk == 7)),
                             reads=[catT, w_out_sb], writes=[ps], pe_acc=True)
                    cs = slice(hlf * 512, (hlf + 1) * 512)
                    k.op("dve", lambda ps=ps, cs=cs: V.tensor_tensor(zb[0:P, cs], ps[0:P, :], rowv["g1" + sfx][0:P, cs], op=ALU.mult), reads=[ps, rowv["g1" + sfx]], writes=[zb])
                k.op("dve", lambda: V.scalar_tensor_tensor(zb[0:P, :], xb[0:P, :], DN_ALPHA, zb[0:P, :], op0=ALU.mult, op1=ALU.add), reads=[xb, zb], writes=[zb])
                ln_rows(zb, P, st, mv, rstd, x1b)
                k.op("dve", lambda: V.tensor_tensor(x1b[0:P, :], x1b[0:P, :], lnr[0:P, 0, :], op=ALU.mult), reads=[x1b, lnr], writes=[x1b])
                k.op("dve", lambda: V.tensor_tensor(x1b[0:P, :], x1b[0:P, :], lnr[0:P, 1, :], op=ALU.add), reads=[x1b, lnr], writes=[x1b])
                k.dma("pool", x1_d[blk * 128:blk * 128 + P, :], x1b[0:P, :], reads=[x1b], writes=[x1_d])
                ln_rows(x1b, P, st, mv, rstd, xnb)
                k.op("dve", lambda: V.tensor_tensor(xm2[0:P, :], xnb[0:P, :], rowv["sc2" + sfx][0:P, :], op=ALU.mult), reads=[xnb, rowv["sc2" + sfx]], writes=[xm2])
                k.op("dve", lambda: V.tensor_tensor(xm2[0:P, :], xm2[0:P, :], rowv["sh2" + sfx][0:P, :], op=ALU.add), reads=[xm2, rowv["sh2" + sfx]], writes=[xm2])
                for hlf in range(2):
                    ps = k.nextps()
                    for q in range(4):
                        kk = hlf * 4 + q
                        k.op("pe", lambda kk=kk, q=q, ps=ps: PE.transpose(ps[:, q * P:(q + 1) * P], xm2[0:P, kk * 128:(kk + 1) * 128], ident[0:P, 0:P]),
                             reads=[xm2, ident], writes=[ps], pe_acc=True)
                    k.op("act", lambda ps=ps, hlf=hlf: A.copy(xmT[:, hlf * 4:(hlf + 1) * 4, 0:P], ps[:, 0:4 * P].rearrange("p (a b) -> p a b", b=P)), reads=[ps], writes=[xmT])
                xmTb = xm2Tb[blk % 2]
                k.op("act", lambda: A.copy(xmTb[:, :, 0:P], xmT[:, :, 0:P]), reads=[xmT], writes=[xmTb])
                k.dma("pool", xm2T_v[:, :, blk * 128:blk * 128 + P], xmTb[:, :, 0:P], reads=[xmTb], writes=[xm2T_d])
                ps = k.nextps()
                for kk in range(8):
                    k.op("pe", lambda kk=kk, ps=ps: PE.matmul(ps[0:P, 0:32], xmT[:, kk, 0:P], w_router[:, kk, :], start=(kk == 0), stop=(kk == 7)),
                         reads=[xmT, w_router], writes=[ps], pe_acc=True)
                k.op("dve", lambda ps=ps: V.tensor_tensor(lg[0:P, :], ps[0:P, 0:32], b_router[0:P, :], op=ALU.add), reads=[ps, b_router], writes=[lg])
                k.op("dve", lambda: V.max(m8r[0:P, :], lg[0:P, :]), reads=[lg], writes=[m8r])
                k.op("dve", lambda: V.tensor_scalar(ew[0:P, 4:5], m8r[0:P, 0:1], -1.0, None, op0=ALU.mult), reads=[m8r], writes=[ew])
                k.op("dve", lambda: V.memset(ew[0:P, 5:6], 0.0), writes=[ew])
                k.op("act", lambda: A.activation(ew[0:P, 0:4], m8r[0:P, 0:4], AF.Exp, bias=ew[0:P, 4:5], scale=1.0, accum_out=ew[0:P, 5:6]), reads=[m8r, ew], writes=[ew])
                k.op("dve", lambda: V.reciprocal(ew[0:P, 6:7], ew[0:P, 5:6]), reads=[ew], writes=[ew])
                k.op("dve", lambda: V.tensor_scalar(wk4[0:P, :], ew[0:P, 0:4], ew[0:P, 6:7], None, op0=ALU.mult), reads=[ew], writes=[wk4])
                for kq in range(4):
                    k.op("dve", lambda kq=kq: V.tensor_scalar(ohk[0:P, kq, :], lg[0:P, :], m8r[0:P, kq:kq + 1], None, op0=ALU.is_equal), reads=[lg, m8r], writes=[ohk])
                k.op("dve", lambda: V.tensor_scalar(Gall[0:P, blk, :], ohk[0:P, 0, :], wk4[0:P, 0:1], None, op0=ALU.mult), reads=[ohk, wk4], writes=[Gall])
                for kq in range(1, 4):
                    k.op("dve", lambda kq=kq: V.scalar_tensor_tensor(Gall[0:P, blk, :], ohk[0:P, kq, :], wk4[0:P, kq:kq + 1], Gall[0:P, blk, :], op0=ALU.mult, op1=ALU.add),
                         reads=[ohk, wk4, Gall], writes=[Gall])

        with k.scope():
            b_guT = k.sb("b_guT", [128, 32, 16])
            k.dma("sp", b_guT[:], b_guT_d[:], reads=[b_guT_d], writes=[b_guT])
            onesr5 = k.sb("onesr5", [1, 128])
            k.op("dve", lambda: V.memset(onesr5[:], 1.0), writes=[onesr5])
            HT = 1028
            xmh = k.sb("xmh", [128, 8, HT], BF16); yacc = k.sb("yacc", [128, 9, D]); hhT = k.sb("hhT", [128, 8, HT], BF16)
            wgs = [k.sb("wgs%d" % i, [128, 8, 256]) for i in range(3)]
            wgu = [k.sb("wgu%d" % i, [128, 8, 256], BF16) for i in range(3)]
            wds = [k.sb("wds%d" % i, [128, D]) for i in range(2)]
            wdn2 = [k.sb("wdn%d" % i, [128, 8, D], BF16) for i in range(2)]; bdn = [k.sb("bdn%d" % i, [1, D]) for i in range(2)]
            gt = [k.sb("gt%d" % i, [128, 512]) for i in range(2)]; ut_ = [k.sb("ut%d" % i, [128, 512]) for i in range(2)]
            sg = [k.sb("sg%d" % i, [128, 512]) for i in range(2)]
            pc = 0; ac = 0
            for hf_ in range(2):
                blk0 = 8 * hf_
                ntok = 1024 if hf_ == 0 else 1028
                chunks = [(0, 512), (512, 512)] + ([(1024, 4)] if hf_ == 1 else [])
                tiles = [(i * 128, 128) for i in range(8)] + ([(1024, 4)] if hf_ == 1 else [])
                k.dma("sp", xmh[:, :, 0:ntok], xm2T_v[:, :, blk0 * 128:blk0 * 128 + ntok], reads=[xm2T_d], writes=[xmh])
                k.op("dve", lambda: V.memset(yacc[:], 0.0), writes=[yacc])
                for e in range(32):
                    bd = bdn[e % 2]
                    wdn = wdn2[e % 2]
                    g = {}
                    for f in range(8):
                        ws_ = wds[f % 2]
                        k.dma("act", ws_[:], w_dn_d[e, f * 128:(f + 1) * 128, :], reads=[w_dn_d], writes=[ws_])
                        k.op("act", lambda f=f, ws_=ws_: A.copy(wdn[:, f, :], ws_[:]), reads=[ws_], writes=[wdn])
                    k.dma("act", bd[:], b_dn_d[e:e + 1, :], reads=[b_dn_d], writes=[bd])
                    wv = w_gu_d[e].rearrange("(k p) c -> p k c", p=128)
                    for f in range(8):
                        wb = wgu[pc % 3]; wst = wgs[pc % 3]; pc += 1
                        g = {}
                        k.dma("sp", wst[:, :, 0:128], wv[:, :, f * 128:(f + 1) * 128], reads=[w_gu_d], writes=[wst], grp=g)
                        k.dma("sp", wst[:, :, 128:256], wv[:, :, 1024 + f * 128:1024 + (f + 1) * 128], reads=[w_gu_d], writes=[wst], grp=g)
                        k.op("act", lambda wb=wb, wst=wst: A.copy(wb[:].rearrange("p a b -> p (a b)"), wst[:].rearrange("p a b -> p (a b)")), reads=[wst], writes=[wb])
                        for (c0, cn) in chunks:
                            gt_, u_, s_ = gt[ac % 2], ut_[ac % 2], sg[ac % 2]; ac += 1
                            psg = k.nextps(); psu = k.nextps()
                            for kk in range(8):
                                k.op("pe", lambda kk=kk, psg=psg, wb=wb, c0=c0, cn=cn: PE.matmul(psg[:, 0:cn], wb[:, kk, 0:128], xmh[:, kk, c0:c0 + cn], start=(kk == 0), stop=(kk == 7)),
                                     reads=[wb, xmh], writes=[psg], pe_acc=True)
                            for kk in range(8):
                                k.op("pe", lambda kk=kk, psu=psu, wb=wb, c0=c0, cn=cn: PE.matmul(psu[:, 0:cn], wb[:, kk, 128:256], xmh[:, kk, c0:c0 + cn], start=(kk == 0), stop=(kk == 7)),
                                     reads=[wb, xmh], writes=[psu], pe_acc=True)
                            k.op("dve", lambda psg=psg, f=f, cn=cn, gt_=gt_: V.tensor_scalar(gt_[:, 0:cn], psg[:, 0:cn], b_guT[:, e, f:f + 1], 7.0, op0=ALU.add, op1=ALU.min), reads=[psg, b_guT], writes=[gt_])
                            k.op("dve", lambda psu=psu, f=f, cn=cn, u_=u_: V.tensor_scalar(u_[:, 0:cn], psu[:, 0:cn], b_guT[:, e, 8 + f:9 + f], 7.0, op0=ALU.add, op1=ALU.min), reads=[psu, b_guT], writes=[u_])
                            k.op("dve", lambda cn=cn, u_=u_: V.tensor_scalar(u_[:, 0:cn], u_[:, 0:cn], -7.0, 1.0, op0=ALU.max, op1=ALU.add), reads=[u_], writes=[u_])
                            k.op("act", lambda cn=cn, gt_=gt_, s_=s_: A.activation(s_[:, 0:cn], gt_[:, 0:cn], AF.Silu, scale=1.702), reads=[gt_], writes=[s_])
                            k.op("dve", lambda f=f, c0=c0, cn=cn, u_=u_, s_=s_: V.scalar_tensor_tensor(hhT[:, f, c0:c0 + cn], s_[:, 0:cn], 1.0 / 1.702, u_[:, 0:cn], op0=ALU.mult, op1=ALU.mult),
                                 reads=[s_, u_], writes=[hhT])
                    for ti, (t0, tn) in enumerate(tiles):
                        for hlf in range(2):
                            ps = k.nextps()
                            for f in range(8):
                                k.op("pe", lambda f=f, ps=ps, t0=t0, tn=tn, hlf=hlf: PE.matmul(ps[0:tn, :], hhT[:, f, t0:t0 + tn], wdn[:, f, hlf * 512:(hlf + 1) * 512], start=(f == 0), stop=False),
                                     reads=[hhT, wdn], writes=[ps], pe_acc=True)
                            k.op("pe", lambda ps=ps, hlf=hlf, tn=tn: PE.matmul(ps[0:tn, :], onesr5[0:1, 0:tn], bd[0:1, hlf * 512:(hlf + 1) * 512], start=False, stop=True),
                                 reads=[onesr5, bd], writes=[ps], pe_acc=True)
                            k.op("dve", lambda ps=ps, hlf=hlf, tn=tn, ti=ti: V.scalar_tensor_tensor(
                                yacc[0:tn, ti, hlf * 512:(hlf + 1) * 512], ps[0:tn, :], Gall[0:tn, blk0 + ti, e:e + 1], yacc[0:tn, ti, hlf * 512:(hlf + 1) * 512], op0=ALU.mult, op1=ALU.add),
                                 reads=[ps, Gall, yacc], writes=[yacc])
                for ti, (t0, tn) in enumerate(tiles):
                    r0 = (blk0 + ti) * 128
                    k.dma("pool", moe_d[r0:r0 + tn, :], yacc[0:tn, ti, :], reads=[yacc], writes=[moe_d])

        with k.scope():
            rowv = build_rowv(["g2"])
            lnr = k.sb("lnr2", [128, 2, D])
            for i in range(2):
                k.dma("sp", lnr[:, i, :], lnrows_d[2 + i:3 + i, :].partition_broadcast(128), reads=[lnrows_d], writes=[lnr])
            accs = [k.sb("acc%d" % i, [128, D]) for i in range(2)]; x1rs = [k.sb("x1r%d" % i, [128, D]) for i in range(2)]; yo = k.sb("yo", [128, D])
            for blk in range(NBLK):
                samp = (blk == NOWN)
                P = NS if samp else 128
                sfx = "s" if samp else "p"
                acc, x1r = accs[blk % 2], x1rs[blk % 2]
                k.dma("sp", acc[0:P, :], moe_d[blk * 128:blk * 128 + P, :], reads=[moe_d], writes=[acc])
                k.dma("act", x1r[0:P, :], x1_d[blk * 128:blk * 128 + P, :], reads=[x1_d], writes=[x1r])
                k.op("dve", lambda: V.tensor_tensor(acc[0:P, :], acc[0:P, :], rowv["g2" + sfx][0:P, :], op=ALU.mult), reads=[acc, rowv["g2" + sfx]], writes=[acc])
                k.op("dve", lambda: V.scalar_tensor_tensor(acc[0:P, :], x1r[0:P, :], DN_ALPHA, acc[0:P, :], op0=ALU.mult, op1=ALU.add), reads=[x1r, acc], writes=[acc])
                ln_rows(acc, P, st, mv, rstd, yo)
                k.op("dve", lambda: V.tensor_tensor(yo[0:P, :], yo[0:P, :], lnr[0:P, 0, :], op=ALU.mult), reads=[yo, lnr], writes=[yo])
                k.op("dve", lambda: V.tensor_tensor(yo[0:P, :], yo[0:P, :], lnr[0:P, 1, :], op=ALU.add), reads=[yo, lnr], writes=[yo])
                if samp:
                    k.dma("pool", o_y_s[:, :], yo[0:P, :], reads=[yo], writes=[o_y_s])
                else:
                    k.dma("pool", o_y_p[blk * 128:(blk + 1) * 128, :], yo[:, :], reads=[yo], writes=[o_y_p])

    if dbg:
        for nm, src, shp in (("dbg_att", att_d, [NOWN * 128, 512]), ("dbg_ssm", ssm_out_d, [NOWN * 128, 512]), ("dbg_x1", x1_d, [NOWN * 128 + 128, D]), ("dbg_att_s", atts_d, [NS, 512]), ("dbg_moe", moe_d, [NTOK, D])):
            o = dout(nm, shp)
            k.dma("sp", o[:, :], src[:, :], reads=[src], writes=[o])
    k.finish()
    return nc, k


def _q_perm():
    perm = []
    for t in range(4):
        perm += list(range(512 + 64 * t, 512 + 64 * t + 64))
        perm += list(range(512 + 64 * (4 + t), 512 + 64 * (4 + t) + 64))
    return np.array(perm)


_CACHE = {}


def _bucket_table(nmax):
    try:
        import jax, jax.numpy as jnp
        with jax.default_device(jax.devices("cpu")[0]):
            n = jnp.arange(nmax + 1)
            nf = jnp.maximum(n, 1).astype(jnp.float32)
            lg = 16 + (jnp.log(nf / 16) / math.log(1024 / 16) * 16).astype(jnp.int32)
            out = np.asarray(jnp.where(n < 16, n, jnp.minimum(lg, 31)))
        return out.astype(np.int64)
    except Exception:
        n = np.arange(nmax + 1)
        nf = np.maximum(n, 1).astype(np.float32)
        lg = 16 + (np.log(nf / np.float32(16)) / np.float32(math.log(64.0)) * np.float32(16)).astype(np.int32)
        return np.where(n < 16, n, np.minimum(lg, 31)).astype(np.int64)


def _sample_consts():
    f32 = np.float32
    bt = _bucket_table(20000)
    def onehot(dists):
        dists = np.asarray(dists)
        o = np.zeros((33,) + dists.shape, f32)
        b = np.where(dists < 0, 32, bt[np.clip(dists, 0, 20000)])
        np.put_along_axis(o, b[None], 1.0, axis=0)
        return o
    n = np.arange(1024)
    ohc = onehot(np.where(n <= 1022, 16384 - (16 * n + 31), -1))
    tok = np.arange(128)
    ohs = np.zeros((33, 9, 128), f32)
    ohs[:, 0, :] = onehot(np.full(128, 5000))
    for i in range(1, 8):
        ohs[:, i, :] = onehot(16384 - ((120 + i) * 128 + tok))
    ohs[:, 8, :] = onehot(np.where(tok == 0, 0, -1))
    ohw = np.zeros((33, 5, 128), f32)
    for i in range(4):
        ohw[:, i, :] = onehot(512 - (i * 128 + tok))
    ohw[:, 4, :] = onehot(np.where(tok == 0, 0, -1))
    gsum = np.zeros((8, 2), f32); gsum[0:4, 0] = 1; gsum[4:8, 1] = 1
    selcs = np.zeros((2, 2, 260), f32)
    selcs[:, 0, :257] = 1.0
    selcs[:, 1, 257:] = -1.0
    for fb in (0, 255, 256):
        selcs[:, 0, fb] = 0.0; selcs[:, 1, fb] = 1.0e4
    half2 = np.zeros((2, 128), f32); half2[0, :64] = 1; half2[1, 64:] = 1
    m01 = np.zeros((8, 2), f32); m01[0:4, 0] = 1; m01[4:8, 1] = 1
    return dict(pidx=np.arange(128, dtype=f32).reshape(128, 1), ohc=np.ascontiguousarray(ohc), ohs=ohs, ohw=ohw, gsum=gsum,
                gexp=np.ascontiguousarray(gsum.T), selcs=selcs, half2=half2, m01=m01)


def _nsa_consts(par):
    f32 = np.float32
    YG, Y0 = 1408, 1151
    bt = _bucket_table(4096)
    y = np.arange(YG)
    d = Y0 - y + 128 * par
    oh = np.zeros((2, 33, YG), f32)
    for w in range(2):
        bad = (d < 0) | ((d > 512) if w == 1 else False)
        b = np.where(bad, 32, bt[np.clip(d, 0, 4096)])
        oh[w, b, y] = 1.0
    selc = np.zeros((3, NOWN, 128, 64), f32)
    for n in range(NOWN):
        qpos = 128 * (2 * n + par) + np.arange(128)
        cur = (qpos // 64)[:, None]
        blk = np.arange(64)[None, :]
        cm = (blk <= cur)
        fm = ((blk == 0) | (blk == cur) | (blk == cur - 1)) & cm
        selc[0, n] = (cm & ~fm)
        selc[1, n] = 1.0e4 * fm - ((~fm) & (~cm))
        selc[2, n] = cm
    return np.ascontiguousarray(oh), np.ascontiguousarray(selc)


def kernel(**inp):
    f32 = np.float32
    A_ = lambda a: np.ascontiguousarray(np.asarray(a), dtype=None)
    x_prompt = np.asarray(inp["x_prompt"], f32)
    x_sample = np.asarray(inp["x_sample"], f32)
    w_in_full = np.asarray(inp["w_in"], f32)[0]
    cols = np.arange(D_IN)
    cols[512:1024] = _q_perm()
    w_in_p = np.ascontiguousarray(w_in_full[:, cols])
    w_ada = np.ascontiguousarray(np.asarray(inp["w_ada"], f32)[0])
    b_ada = np.asarray(inp["b_ada"], f32)[0]
    b_adaT = np.ascontiguousarray(b_ada.reshape(48, 128).T)
    b_ada_row = np.ascontiguousarray(b_ada.reshape(1, -1))
    c_prompt = np.asarray(inp["c_prompt"], f32)
    c_sample = np.asarray(inp["c_sample"], f32)
    state_win = np.asarray(inp["state_win_kv"], f32)[0].reshape(32, 512, 256)
    ident = np.eye(128, dtype=f32)
    lam_re = np.asarray(inp["lam_re"], f32)[0]; lam_im = np.asarray(inp["lam_im"], f32)[0]
    log_dt = np.asarray(inp["log_dt"], f32)[0]
    ldt_full = np.repeat(log_dt[:, None], 64, 1)
    def col_layout(a):
        return np.ascontiguousarray(a.reshape(16, 2, 64).transpose(1, 2, 0).reshape(128, 16))
    lamc = np.ascontiguousarray(np.stack([col_layout(lam_re), col_layout(lam_im), col_layout(ldt_full)], 1))
    lamr = np.ascontiguousarray(np.stack([lam_re.reshape(-1), lam_im.reshape(-1), ldt_full.reshape(-1)], 0))
    btp = np.zeros((2, 128, 16, 128), f32)
    ctp = np.zeros((2, 128, 16, 32), f32)
    for i, (bsrc, csrc) in enumerate(((inp["b_re"], inp["c_re"]), (inp["b_im"], inp["c_im"]))):
        bsrc = np.asarray(bsrc, f32)[0]; csrc = np.asarray(csrc, f32)[0]
        for g_ in range(32):
            s_, gl = g_ // 2, g_ % 2
            ut = s_ // 4
            r0 = (g_ - 8 * ut) * 16
            btp[i, r0:r0 + 16, s_, gl * 64:(gl + 1) * 64] = bsrc[g_].T
            ctp[i, gl * 64:(gl + 1) * 64, s_, gl * 16:(gl + 1) * 16] = csrc[g_].T
    dskc = np.ascontiguousarray(np.asarray(inp["d_skip"], f32)[0].reshape(4, 128).T)
    w_glu = np.ascontiguousarray(np.asarray(inp["w_glu"], f32)[0])
    b_glu = np.ascontiguousarray(np.asarray(inp["b_glu"], f32)[0].reshape(1, 512))
    st_re = np.asarray(inp["state_ssm_re"], f32)[0]; st_im = np.asarray(inp["state_ssm_im"], f32)[0]
    def h0_layout(a4):
        return a4.reshape(4, 16, 2, 64).transpose(2, 3, 1, 0).reshape(128, 16, 4)

    rel_bias = np.ascontiguousarray(np.asarray(inp["rel_bias"], f32))
    jflip = np.ascontiguousarray(np.eye(128, dtype=f32)[::-1])
    w1 = np.asarray(inp["phi_w1"], f32)[0]
    w2 = np.asarray(inp["phi_w2"], f32)[0]
    ppe = np.asarray(inp["phi_pe"], f32)[0]
    pb1v = np.asarray(inp["phi_b1"], f32)[0]; pb2v = np.asarray(inp["phi_b2"], f32)[0]
    bd1 = np.zeros((128, 64, 128), f32); bd2 = np.zeros((128, 2, 128), f32)
    for h_ in range(2):
        bd1[h_ * 64:(h_ + 1) * 64, :, h_ * 64:(h_ + 1) * 64] = w1.reshape(64, 64, 64).transpose(1, 0, 2)
        bd2[h_ * 64:(h_ + 1) * 64, :, h_ * 64:(h_ + 1) * 64] = w2.transpose(1, 0, 2)
    pel = np.ascontiguousarray(np.tile(ppe.reshape(2, 2, 16, 64).transpose(3, 0, 1, 2), (2, 1, 1, 1)))
    pb1 = np.ascontiguousarray(np.tile(pb1v.T, (2, 1))); pb2 = np.ascontiguousarray(np.tile(pb2v.T, (2, 1)))
    pb2r = np.ascontiguousarray(np.tile(pb2v[1], 2).reshape(1, 128))
    nsa_c = [_nsa_consts(p_) for p_ in range(2)]
    smp_c = _sample_consts()
    pool_cmp = np.asarray(inp["cache_cmp_kv"], f32)[0].reshape(5120 * 128, 256)
    pool_sel = np.asarray(inp["cache_sel_kv"], f32)[0].reshape(5120 * 128 * 2, 128)
    page_table = np.asarray(inp["page_table"]).astype(np.int32)
    w_out = np.ascontiguousarray(np.asarray(inp["w_out"], f32)[0])
    lnrows = np.ascontiguousarray(np.stack([np.asarray(inp[n_], f32)[0] for n_ in ("ln1_g", "ln1_b", "ln2_g", "ln2_b")], 0))
    w_router = np.ascontiguousarray(np.asarray(inp["w_router"], f32)[0])
    b_router = np.ascontiguousarray(np.asarray(inp["b_router"], f32)[0].reshape(1, 32))
    w_gu = np.ascontiguousarray(np.asarray(inp["w_gate_up"], f32)[0])
    b_guT = np.ascontiguousarray(np.asarray(inp["b_gate_up"], f32)[0].reshape(32, 16, 128).transpose(2, 0, 1))
    w_dn = np.ascontiguousarray(np.asarray(inp["w_down"], f32)[0])
    b_dn = np.ascontiguousarray(np.asarray(inp["b_down"], f32)[0])
    triu = np.ascontiguousarray(np.triu(np.ones((128, 128), f32), 1))
    eoff = (np.arange(32, dtype=f32) * 384.0).reshape(1, 32)

    if "prog" not in _CACHE:
        _CACHE["prog"] = build_program()
    nc, kb = _CACHE["prog"]

    in_maps = []
    for c in range(8):
        b = c // 2
        cv = np.concatenate([c_prompt[b:b + 1], c_sample[4 * c:4 * c + 4]], 0)
        cT = np.ascontiguousarray(cv.T.reshape(8, 128, 5).transpose(1, 0, 2))
        in_maps.append(dict(
            xp=np.ascontiguousarray(x_prompt[b]),
            xs=np.ascontiguousarray(x_sample[4 * c:4 * c + 4, 0, :]),
            cT=cT, w_ada=w_ada, b_adaT=b_adaT, b_ada_row=b_ada_row, w_in=w_in_p, ident=ident,
            state_win=np.ascontiguousarray(state_win[4 * c:4 * c + 4]),
            xo=np.ascontiguousarray(x_prompt[b].reshape(16, 2, 128, D)[:, c % 2].reshape(2048, D)),
            parv=np.full((128, 1), float(c % 2), f32),
            lamc=lamc, lamr=lamr, btp=btp, ctp=ctp, dskc=dskc, w_glu=w_glu, b_glu=b_glu,
            h0c=np.ascontiguousarray(np.stack([h0_layout(st_re[4 * c:4 * c + 4]), h0_layout(st_im[4 * c:4 * c + 4])], 0)),
            rel_bias=rel_bias, oh=nsa_c[c % 2][0], jflip=jflip, selc=nsa_c[c % 2][1],
            bd1=bd1, bd2=bd2, pel=pel, pb1=pb1, pb2=pb2, pb2r=pb2r,
            w_out=w_out, lnrows=lnrows, w_router=w_router, b_router=b_router, w_gate_up=w_gu, b_guT=b_guT,
            w_down=w_dn, b_down=b_dn,
            pool_cmp=pool_cmp, pool_sel=pool_sel, pt=np.ascontiguousarray(page_table[4 * c:4 * c + 4].reshape(1, 512)),
            **smp_c,
        ))
    res = run_bass_kernel_spmd(nc, in_maps, core_ids=list(range(8)))
    R = res.results
    _CACHE["last"] = R

    def g(c, name):
        return np.asarray(R[c][name], f32)

    kvs = (1, 4, T, 2, 2, 64)
    y_prompt = np.zeros((4, 16, 2, 128, D), f32)
    for c in range(8):
        y_prompt[c // 2, :, c % 2] = g(c, "o_y_p").reshape(16, 128, D)
    y_prompt = y_prompt.reshape(4, T, D)
    y_sample = np.concatenate([g(c, "o_y_s") for c in range(8)]).reshape(32, 1, D)
    cmp_p = np.stack([g(2 * b, "o_cmp_p") for b in range(4)]).reshape(kvs)
    sel_p = np.stack([g(2 * b, "o_sel_p") for b in range(4)]).reshape(kvs)
    win_p = np.stack([g(2 * b, "o_win_p")[T - 512:] for b in range(4)]).reshape(1, 4, 512, 2, 2, 64)
    cmp_s = np.concatenate([g(c, "o_cmp_s") for c in range(8)]).reshape(1, 32, 1, 2, 2, 64)
    sel_s = np.concatenate([g(c, "o_sel_s") for c in range(8)]).reshape(1, 32, 1, 2, 2, 64)
    win_s = np.concatenate([g(c, "o_win_s") for c in range(8)]).reshape(1, 32, 512, 2, 2, 64)
    def from_col(a):
        return a.reshape(2, 64, 16).transpose(2, 0, 1).reshape(32, 64)
    re_p = np.stack([from_col(g(2 * b, "o_ssm_p")[0]) for b in range(4)])[None]
    im_p = np.stack([from_col(g(2 * b, "o_ssm_p")[1]) for b in range(4)])[None]
    def from_col_s(a):
        return a.reshape(2, 64, 16, 4).transpose(3, 2, 0, 1).reshape(4, 32, 64)
    re_s = np.concatenate([from_col_s(g(c, "o_ssm_s")[0]) for c in range(8)])[None]
    im_s = np.concatenate([from_col_s(g(c, "o_ssm_s")[1]) for c in range(8)])[None]
    return (y_prompt, y_sample, cmp_p, cmp_s, sel_p, sel_s, win_p, win_s, re_p, im_p, re_s, im_s)
```

```python
import numpy as np
import concourse.bass as bass
import concourse.mybir as mybir
from contextlib import ExitStack

F32 = mybir.dt.float32
BF16 = mybir.dt.bfloat16
I32 = mybir.dt.int32
U32 = mybir.dt.uint32
AF = mybir.ActivationFunctionType
ALU = mybir.AluOpType
AX = mybir.AxisListType


class Buf:
    __slots__ = ("t", "name", "w", "r")

    def __init__(self, t, name):
        self.t = t
        self.name = name
        self.w = None
        self.r = []

    def __getitem__(self, k):
        return self.t[k]


class KB:
    ENG = ("pe", "dve", "act", "pool", "sp")

    def __init__(self, nc, n_dma_sems=40):
        self.nc = nc
        self.es = ExitStack()
        self.es.enter_context(nc.allow_non_contiguous_dma(reason="small strided loads"))
        self.eng = {"pe": nc.tensor, "dve": nc.vector, "act": nc.scalar, "pool": nc.gpsimd, "sp": nc.sync}
        self.sem = {e: self.es.enter_context(nc.semaphore("s_" + e)) for e in self.ENG}
        self.cnt = {e: 0 for e in self.ENG}
        self.dsem = [self.es.enter_context(nc.semaphore("d%d" % i)) for i in range(n_dma_sems)]
        self.dcnt = [0] * n_dma_sems
        self.dnext = 0
        self.seen = {e: {} for e in self.ENG}
        self.n_wait = 0
        self.n_inst = 0
        self.pend = None

    def sb(self, name, shape, dt=F32):
        self.uid = getattr(self, "uid", 0) + 1
        t = self.es.enter_context(self.nc.sbuf_tensor("sb%d_%s" % (self.uid, name), list(shape), dt))
        return Buf(t, name)

    def ps(self, name, shape, dt=F32):
        t = self.es.enter_context(self.nc.psum_tensor(name, list(shape), dt))
        return Buf(t, name)

    def dram(self, name, shape, dt=F32, kind="Internal"):
        t = self.nc.dram_tensor(name, list(shape), dt, kind=kind)
        return Buf(t.ap(), name)

    def scope(self):
        kb = self
        class _S:
            def __enter__(s2):
                s2.prev = kb.es
                kb.es = ExitStack()
                return kb
            def __exit__(s2, *a):
                kb.barrier()
                kb.es.close()
                kb.es = s2.prev
                return False
        return _S()

    def init_psum(self):
        self.psb = [self.ps("psb%d" % i, [128, 512]) for i in range(8)]
        self.psi = 0

    def nextps(self):
        b = self.psb[self.psi]
        self.psi = (self.psi + 1) % 8
        return b

    def _wait(self, e, tok):
        if tok is None:
            return
        sem, val, src = tok[0], tok[1], tok[2]
        key = id(sem)
        if self.seen[e].get(key, 0) >= val:
            return
        if self.pend is not None:
            cur = self.pend.get(key)
            if cur is None or cur[1] < val:
                self.pend[key] = (sem, val)
            return
        self.eng[e].wait_ge(sem, val)
        self.seen[e][key] = val
        self.n_wait += 1

    def _flush(self, e, inst_fn):
        items = list(self.pend.values())
        self.pend = None
        for sem, val in items[:-1]:
            self.eng[e].wait_ge(sem, val)
            self.seen[e][id(sem)] = val
            self.n_wait += 1
        inst = inst_fn()
        if items:
            sem, val = items[-1]
            inst._wait_ge(sem, val)
            self.seen[e][id(sem)] = val
        return inst

    def _deps(self, e, reads, writes, pe_acc=False):
        for b in reads:
            if b.w is not None:
                self._wait(e, b.w)
        for b in writes:
            if b.w is not None and not (pe_acc and b.w[2] == "pe" and e == "pe"):
                if not (b.w[2] == e and e != "dma"):
                    self._wait(e, b.w)
            for tok in b.r:
                if tok[2] != e:
                    self._wait(e, tok)

    def _commit(self, tok, reads, writes):
        for b in reads:
            b.r.append(tok)
            if len(b.r) > 24:
                b.r = b.r[-24:] if False else self._compact(b.r)
        for b in writes:
            b.w = tok
            b.r = []

    @staticmethod
    def _compact(rs):
        best = {}
        for tok in rs:
            k = id(tok[0])
            if k not in best or best[k][1] < tok[1]:
                best[k] = tok
        return list(best.values())

    def op(self, e, fn, reads=(), writes=(), pe_acc=False):
        self.pend = {}
        self._deps(e, reads, writes, pe_acc)
        inst = self._flush(e, fn)
        self.cnt[e] += 1
        inst.then_inc(self.sem[e], 1)
        tok = (self.sem[e], self.cnt[e], e)
        self._commit(tok, reads, writes)
        self.n_inst += 1
        return tok

    def dma(self, q, out, in_, reads=(), writes=(), grp=None, fn=None):
        self.pend = {}
        self._deps(q, reads, writes)
        if grp is not None and "i" in grp:
            i = grp["i"]
        else:
            i = self.dnext
            self.dnext = (self.dnext + 1) % len(self.dsem)
            if self.dcnt[i] > 0:
                self._wait(q, (self.dsem[i], self.dcnt[i], "dma"))
            if grp is not None:
                grp["i"] = i
        if fn is None:
            inst = self._flush(q, lambda: self.eng[q].dma_start(out=out, in_=in_))
        else:
            inst = self._flush(q, fn)
        self.dcnt[i] += 16
        inst.then_inc(self.dsem[i], 16)
        if grp is not None:
            tok = grp.setdefault("tok", [self.dsem[i], 0, "dma"])
            tok[1] = self.dcnt[i]
        else:
            tok = [self.dsem[i], self.dcnt[i], "dma"]
        self._commit(tok, reads, writes)
        self.n_inst += 1
        return tok

    def barrier(self):
        toks = [(self.sem[e], self.cnt[e], e) for e in self.ENG if self.cnt[e] > 0]
        toks += [(self.dsem[i], self.dcnt[i], "dma") for i in range(len(self.dsem)) if self.dcnt[i] > 0]
        for e in self.ENG:
            for tok in toks:
                if tok[2] != e:
                    self._wait(e, tok)

    def finish(self):
        self.barrier()

    def close(self):
        self.es.close()

import os
import math
from concourse.bass_utils import run_bass_kernel_spmd

D = 1024
T = 4096
NB = 32
NOWN = 16
NS = 4
D_IN = 1816
EPS = 1e-5
STAGE = int(os.environ.get("MK_STAGE", "9"))


DBG = bool(int(os.environ.get("MK_DBG", "0")))


def build_program(stage=STAGE, dbg=DBG):
    nc = bass.Bass("TRN2", target_bir_lowering=False)
    k = KB(nc)
    k.init_psum()
    V, A, G, PE = nc.vector, nc.scalar, nc.gpsimd, nc.tensor

    def din(name, shape, dt=F32):
        return k.dram(name, shape, dt, kind="ExternalInput")

    def dout(name, shape, dt=F32):
        return k.dram(name, shape, dt, kind="ExternalOutput")

    xp = din("xp", [T, D])
    xs = din("xs", [NS, D])
    cT_d = din("cT", [128, 8, 5])
    w_ada = din("w_ada", [D, 6 * D])
    b_adaT_d = din("b_adaT", [128, 48])
    b_ada_row = din("b_ada_row", [1, 6 * D])
    w_in = din("w_in", [D, D_IN])
    ident_d = din("ident", [128, 128])
    state_win = din("state_win", [NS, 512, 256])
    xo = din("xo", [NOWN * 128, D])
    parv_d = din("parv", [128, 1])
    lamc_d = din("lamc", [128, 3, 16])
    lamr_d = din("lamr", [3, 2048])
    btp_d = din("btp", [2, 128, 16, 128])
    ctp_d = din("ctp", [2, 128, 16, 32])
    dskc_d = din("dskc", [128, 4])
    w_glu_d = din("w_glu", [512, 512])
    b_glu_d = din("b_glu", [1, 512])
    h0c_d = din("h0c", [2, 128, 16, NS])
    YG = 1408
    CAP = 384
    NSLOT = 32 * CAP
    w_out_d = din("w_out", [D, D])
    lnrows_d = din("lnrows", [4, D])
    w_router_d = din("w_router", [D, 32])
    b_router_d = din("b_router", [1, 32])
    w_gu_d = din("w_gate_up", [32, D, 2048])
    b_guT_d = din("b_guT", [128, 32, 16])
    w_dn_d = din("w_down", [32, D, D])
    b_dn_d = din("b_down", [32, D])
    xs_rows = xs
    pool_cmp = din("pool_cmp", [5120 * 128, 256])
    pool_sel = din("pool_sel", [5120 * 128 * 2, 128])
    pt_d = din("pt", [1, NS * 128], I32)
    pidx_d = din("pidx", [128, 1])
    ohc_d = din("ohc", [33, 1024])
    ohs_d = din("ohs", [33, 9, 128])
    ohw_d = din("ohw", [33, 5, 128])
    gsum_d = din("gsum", [8, 2])
    gexp_d = din("gexp", [2, 8])
    selcs_d = din("selcs", [2, 2, 260])
    half2_d = din("half2", [2, 128])
    m01_d = din("m01", [8, 2])
    o_y_p = dout("o_y_p", [NOWN * 128, D])
    o_y_s = dout("o_y_s", [NS, D])
    rel_bias_d = din("rel_bias", [32, 8])
    oh_d = din("oh", [2, 33, YG])
    jflip_d = din("jflip", [128, 128])
    selc_d = din("selc", [3, NOWN, 128, 64])
    bd1_d = din("bd1", [128, 64, 128])
    bd2_d = din("bd2", [128, 2, 128])
    pel_d = din("pel", [128, 2, 2, 16])
    pb1_d = din("pb1", [128, 2])
    pb2_d = din("pb2", [128, 2])
    pb2r_d = din("pb2r", [1, 128])
    o_cmp_p = dout("o_cmp_p", [T, 256])
    o_sel_p = dout("o_sel_p", [T, 256])
    o_win_p = dout("o_win_p", [T, 256])
    o_cmp_s = dout("o_cmp_s", [NS, 256])
    o_sel_s = dout("o_sel_s", [NS, 256])
    o_win_s = dout("o_win_s", [NS, 512, 256])
    o_ssm_p = dout("o_ssm_p", [2, 128, 16])
    o_ssm_s = dout("o_ssm_s", [2, 128, 16, NS])
    zT_u = k.dram("zT_u", [4, 128, T])
    zT_kv = k.dram("zT_kv", [6, 128, T])
    zT_uo = k.dram("zT_uo", [4, 128, NOWN * 128])
    zT_q = k.dram("zT_q", [4, 128, NOWN * 128])
    gates_d = k.dram("gates_d", [NOWN * 128, 24])
    ssm_out_d = k.dram("ssm_out_d", [NOWN * 128, 512])
    zsT = k.sb("zsT", [128, 14, NS])
    zs = k.sb("zs", [NS, D_IN])
    ssm_out_s = k.sb("ssm_out_s", [NS, 512])
    att_d = k.dram("att_d", [NOWN * 128, 512])
    att_s = k.sb("att_s", [NS, 512])
    k.op("dve", lambda: V.memset(att_s[:], 0.0), writes=[att_s])
    x1_d = k.dram("x1_d", [NOWN * 128 + 128, D])
    selm_d = k.dram("selm_d", [NS, 8, 258])
    gts_d = k.dram("gts_d", [NS, 24])
    atts_d = k.dram("atts_d", [NS, 512])
    G_d = k.dram("G_d", [2, 8, 1408])

    ident = k.sb("ident", [128, 128])
    k.dma("sp", ident[:], ident_d[:], reads=[ident_d], writes=[ident])
    epsb = k.sb("epsb", [128, 1])
    k.op("dve", lambda: V.memset(epsb[:], EPS), writes=[epsb])
    mT = k.sb("mT", [128, 48, 5])
    sc1p = k.sb("sc1p", [128, 8, 5])
    sc2p = k.sb("sc2p", [128, 8, 5])

    def ln_rows(xt, P, st, mv, rstd, out):
        for j in range(2):
            k.op("dve", lambda j=j: V.bn_stats(st[0:P, j, :], xt[0:P, j * 512:(j + 1) * 512]), reads=[xt], writes=[st])
        k.op("dve", lambda: V.bn_aggr(mv[0:P, :], st[0:P].rearrange("p a b -> p (a b)")), reads=[st], writes=[mv])
        k.op("act", lambda: A.activation(rstd[0:P, :], mv[0:P, 1:2], AF.Sqrt, bias=epsb[0:P, 0:1], scale=1.0), reads=[mv, epsb], writes=[rstd])
        k.op("dve", lambda: V.reciprocal(rstd[0:P, :], rstd[0:P, :]), reads=[rstd], writes=[rstd])
        k.op("dve", lambda: V.tensor_scalar(out[0:P, :], xt[0:P, :], mv[0:P, 0:1], rstd[0:P, 0:1], op0=ALU.subtract, op1=ALU.mult),
             reads=[xt, mv, rstd], writes=[out])

    with k.scope():
        cT = k.sb("cTs", [128, 8, 5])
        sc = k.sb("sc", [128, 8, 5])
        b_adaT = k.sb("b_adaTs", [128, 48])
        k.dma("sp", cT[:], cT_d[:], reads=[cT_d], writes=[cT])
        k.dma("sp", b_adaT[:], b_adaT_d[:], reads=[b_adaT_d], writes=[b_adaT])
        k.op("act", lambda: A.activation(sc[:], cT[:], AF.Silu), reads=[cT], writes=[sc])
        wa = [k.sb("wa%d" % i, [128, 8, 768]) for i in range(2)]
        w_ada_v = w_ada.t.rearrange("(k p) c -> p k c", p=128)
        for tg in range(8):
            wb = wa[tg % 2]
            g = {}
            for kk in range(8):
                k.dma("sp" if kk % 2 == 0 else "act", wb[:, kk, :], w_ada_v[:, kk, tg * 768:(tg + 1) * 768], reads=[w_ada], writes=[wb], grp=g)
            ps = k.nextps()
            for t6 in range(6):
                for kk in range(8):
                    k.op("pe", lambda t6=t6, kk=kk: PE.matmul(ps[:, t6 * 8:t6 * 8 + 5], wb[:, kk, t6 * 128:(t6 + 1) * 128], sc[:, kk, :],
                                                             start=(kk == 0), stop=(kk == 7)),
                         reads=[wb, sc], writes=[ps], pe_acc=True)
            k.op("dve", lambda tg=tg, ps=ps: V.tensor_tensor(
                mT[:, tg * 6:(tg + 1) * 6, :], ps[:, 0:48].rearrange("p (a b) -> p a b", b=8)[:, :, 0:5],
                b_adaT[:, tg * 6:(tg + 1) * 6].unsqueeze(2).to_broadcast([128, 6, 5]), op=ALU.add),
                 reads=[ps, b_adaT], writes=[mT])
        k.op("dve", lambda: V.tensor_scalar(sc1p[:], mT[:, 8:16, :], 1.0, None, op0=ALU.add), reads=[mT], writes=[sc1p])
        k.op("dve", lambda: V.tensor_scalar(sc2p[:], mT[:, 32:40, :], 1.0, None, op0=ALU.add), reads=[mT], writes=[sc2p])

    with k.scope():
        w_in_sb = k.sb("w_in_sb", [128, 8, D_IN])
        w_in_v = w_in.t.rearrange("(k p) c -> p k c", p=128)
        g = {}
        for kk in range(8):
            k.dma("sp" if kk % 2 == 0 else "act", w_in_sb[:, kk, :], w_in_v[:, kk, :], reads=[w_in], writes=[w_in_sb], grp=g)
        xt = [k.sb("xt%d" % i, [128, D]) for i in range(2)]
        xn = [k.sb("xn%d" % i, [128, D]) for i in range(2)]
        st = k.sb("st", [128, 2, 6])
        mv = k.sb("mv", [128, 2])
        rstd = k.sb("rstd", [128, 1])
        xmT = [k.sb("xmT%d" % i, [128, 8, 512]) for i in range(2)]
        stg = [k.sb("stg%d" % i, [128, 512]) for i in range(14)]
        kvst = [k.sb("kvst%d" % i, [128, 768]) for i in range(2)]

        def front_group(x_d, gi, jvec, fm_tiles, tm_kv):
            xm = xmT[gi % 2]
            for tt in range(4):
                r0 = gi * 512 + tt * 128
                xb, xnb = xt[tt % 2], xn[tt % 2]
                k.dma("sp", xb[:], x_d[r0:r0 + 128, :], reads=[x_d], writes=[xb])
                ln_rows(xb, 128, st, mv, rstd, xnb)
                for h in range(2):
                    ps = k.nextps()
                    for q in range(4):
                        kk = h * 4 + q
                        k.op("pe", lambda kk=kk, q=q, ps=ps: PE.transpose(ps[:, q * 128:(q + 1) * 128], xnb[:, kk * 128:(kk + 1) * 128], ident[:]),
                             reads=[xnb, ident], writes=[ps], pe_acc=True)
                    for q in range(4):
                        kk = h * 4 + q
                        k.op("act", lambda kk=kk, q=q, ps=ps: A.activation(
                            xm[:, kk, tt * 128:(tt + 1) * 128], ps[:, q * 128:(q + 1) * 128], AF.Identity,
                            bias=mT[:, kk, jvec:jvec + 1], scale=sc1p[:, kk, jvec:jvec + 1]),
                             reads=[ps, mT, sc1p], writes=[xm])
            for si, (c0, dst, di) in enumerate(fm_tiles):
                ps = k.nextps()
                for kk in range(8):
                    k.op("pe", lambda kk=kk, ps=ps, c0=c0: PE.matmul(ps[:, :], w_in_sb[:, kk, c0:c0 + 128], xm[:, kk, :], start=(kk == 0), stop=(kk == 7)),
                         reads=[w_in_sb, xm], writes=[ps], pe_acc=True)
                sb_ = stg[si]
                if si % 2 == 0:
                    k.op("dve", lambda ps=ps, sb_=sb_: V.tensor_copy(sb_[:], ps[:]), reads=[ps], writes=[sb_])
                else:
                    k.op("act", lambda ps=ps, sb_=sb_: A.copy(sb_[:], ps[:]), reads=[ps], writes=[sb_])
                k.dma("pool", dst[di, :, gi * 512:(gi + 1) * 512], sb_[:], reads=[sb_], writes=[dst])
            if tm_kv is not None:
                kv_stage_idx, outs = tm_kv
                for tt in range(4):
                    kb_ = kvst[tt % 2]
                    for h in range(2):
                        ps = k.nextps()
                        for q in range(3):
                            sb_ = stg[kv_stage_idx[h * 3 + q]]
                            k.op("pe", lambda q=q, ps=ps, sb_=sb_: PE.transpose(ps[:, q * 128:(q + 1) * 128], sb_[:, tt * 128:(tt + 1) * 128], ident[:]),
                                 reads=[sb_, ident], writes=[ps], pe_acc=True)
                        if h == 0:
                            k.op("dve", lambda ps=ps, kb_=kb_: V.tensor_copy(kb_[:, 0:384], ps[:, 0:384]), reads=[ps], writes=[kb_])
                        else:
                            k.op("act", lambda ps=ps, kb_=kb_: A.copy(kb_[:, 384:768], ps[:, 0:384]), reads=[ps], writes=[kb_])
                    r0 = gi * 512 + tt * 128
                    for oi, od in enumerate(outs):
                        k.dma("pool", od[r0:r0 + 128, :], kb_[:, oi * 256:(oi + 1) * 256], reads=[kb_], writes=[od])

        fmA = [(128 * i, zT_u, i) for i in range(4)] + [(1024 + 128 * i, zT_kv, i) for i in range(6)]
        for gi in range(T // 512):
            front_group(xp, gi, 0, fmA, ([4, 5, 6, 7, 8, 9], [o_cmp_p, o_sel_p, o_win_p]))

        fmB = [(128 * i, zT_uo, i) for i in range(4)] + [(512 + 128 * i, zT_q, i) for i in range(4)]
        gst = k.sb("gst", [128, 4, 24])
        for gi in range(NOWN * 128 // 512):
            front_group(xo, gi, 0, fmB, None)
            xm = xmT[gi % 2]
            ps = k.nextps()
            for tt in range(4):
                for kk in range(8):
                    k.op("pe", lambda kk=kk, tt=tt, ps=ps: PE.matmul(ps[:, tt * 32:tt * 32 + 24], xm[:, kk, tt * 128:(tt + 1) * 128], w_in_sb[:, kk, 1792:1816],
                                                                     start=(kk == 0), stop=(kk == 7)),
                         reads=[xm, w_in_sb], writes=[ps], pe_acc=True)
            k.op("act", lambda ps=ps: A.activation(gst[:], ps[:, 0:128].rearrange("p (a b) -> p a b", b=32)[:, :, 0:24], AF.Sigmoid), reads=[ps], writes=[gst])
            for tt in range(4):
                r0 = gi * 512 + tt * 128
                k.dma("pool", gates_d[r0:r0 + 128, :], gst[:, tt, :], reads=[gst], writes=[gates_d])

        xst = k.sb("xst", [NS, D])
        xsn = k.sb("xsn", [NS, D])
        xsT = k.sb("xsT", [128, 8, NS])
        k.dma("sp", xst[:], xs[:], reads=[xs], writes=[xst])
        ln_rows(xst, NS, st, mv, rstd, xsn)
        ps = k.nextps()
        for kk in range(8):
            k.op("pe", lambda kk=kk: PE.transpose(ps[:, kk * NS:(kk + 1) * NS], xsn[0:NS, kk * 128:(kk + 1) * 128], ident[0:NS, 0:NS]),
                 reads=[xsn, ident], writes=[ps], pe_acc=True)
        k.op("dve", lambda: V.tensor_tensor(xsT[:], ps[:, 0:8 * NS].rearrange("p (a b) -> p a b", b=NS), sc1p[:, :, 1:5], op=ALU.mult),
             reads=[ps, sc1p], writes=[xsT])
        k.op("dve", lambda: V.tensor_tensor(xsT[:], xsT[:], mT[:, 0:8, 1:5], op=ALU.add), reads=[xsT, mT], writes=[xsT])
        for cg in range(4):
            c0 = cg * 512
            cn = min(512, D_IN - c0)
            ps = k.nextps()
            for kk in range(8):
                k.op("pe", lambda kk=kk, ps=ps, c0=c0, cn=cn: PE.matmul(ps[0:NS, 0:cn], xsT[:, kk, :], w_in_sb[:, kk, c0:c0 + cn], start=(kk == 0), stop=(kk == 7)),
                     reads=[xsT, w_in_sb], writes=[ps], pe_acc=True)
            k.op("dve", lambda ps=ps, c0=c0, cn=cn: V.tensor_copy(zs[:, c0:c0 + cn], ps[0:NS, 0:cn]), reads=[ps], writes=[zs])
        ps = k.nextps()
        for ct in range(14):
            for kk in range(8):
                k.op("pe", lambda kk=kk, ct=ct, ps=ps: PE.matmul(ps[:, ct * NS:(ct + 1) * NS], w_in_sb[:, kk, ct * 128:(ct + 1) * 128], xsT[:, kk, :],
                                                                 start=(kk == 0), stop=(kk == 7)),
                     reads=[xsT, w_in_sb], writes=[ps], pe_acc=True)
        k.op("dve", lambda ps=ps: V.tensor_copy(zsT[:], ps[:, 0:14 * NS].rearrange("p (a b) -> p a b", b=NS)), reads=[ps], writes=[zsT])
        k.dma("pool", o_cmp_s[:, :], zs[:, 1024:1280], reads=[zs], writes=[o_cmp_s])
        k.dma("pool", o_sel_s[:, :], zs[:, 1280:1536], reads=[zs], writes=[o_sel_s])
        k.dma("pool", o_win_s[:, 511, :], zs[:, 1536:1792], reads=[zs], writes=[o_win_s])
        for j in range(NS):
            k.dma("sp", o_win_s[j, 0:511, :], state_win[j, 1:512, :], reads=[state_win], writes=[o_win_s])


    def gelu_tanh(out, xin, tmp, P, reads_extra=()):
        k.op("dve", lambda: V.tensor_tensor(tmp[0:P], xin[0:P], xin[0:P], op=ALU.mult), reads=[xin], writes=[tmp])
        k.op("dve", lambda: V.tensor_scalar(tmp[0:P], tmp[0:P], 0.044715, 1.0, op0=ALU.mult, op1=ALU.add), reads=[tmp], writes=[tmp])
        k.op("dve", lambda: V.tensor_tensor(tmp[0:P], tmp[0:P], xin[0:P], op=ALU.mult), reads=[tmp, xin], writes=[tmp])
        k.op("act", lambda: A.activation(tmp[0:P], tmp[0:P], AF.Sigmoid, scale=1.5957691216057308), reads=[tmp], writes=[tmp])
        k.op("dve", lambda: V.tensor_tensor(out[0:P], tmp[0:P], xin[0:P], op=ALU.mult), reads=[tmp, xin], writes=[out])

    if stage >= 2:
      with k.scope():
        TWO_PI = 2.0 * np.pi

        def ssm_consts(src, F, tag):
            o = {n: k.sb(tag + n, [128, F]) for n in ("lr", "li", "ir", "ii", "wr", "wi", "t1", "t2", "t3")}
            ti = k.sb(tag + "ti", [128, F], I32)
            dt_, ang, mag = o["t1"], o["t2"], o["t3"]
            k.op("act", lambda: A.activation(dt_[:], src[:, 2, :], AF.Exp), reads=[src], writes=[dt_])
            k.op("dve", lambda: V.tensor_tensor(mag[:], src[:, 0, :], dt_[:], op=ALU.mult), reads=[src, dt_], writes=[mag])
            k.op("dve", lambda: V.tensor_tensor(ang[:], src[:, 1, :], dt_[:], op=ALU.mult), reads=[src, dt_], writes=[ang])

            def sin_of(out, offs):
                r, f = o["wr"], o["wi"]
                k.op("dve", lambda: V.tensor_scalar(r[:], ang[:], 1.0 / TWO_PI, offs / TWO_PI, op0=ALU.mult, op1=ALU.add), reads=[ang], writes=[r])
                k.op("dve", lambda: V.tensor_copy(ti[:], r[:]), reads=[r], writes=[ti])
                k.op("dve", lambda: V.tensor_copy(f[:], ti[:]), reads=[ti], writes=[f])
                k.op("dve", lambda: V.tensor_tensor(r[:], r[:], f[:], op=ALU.subtract), reads=[r, f], writes=[r])
                k.op("dve", lambda: V.scalar_tensor_tensor(f[:], r[:], 0.5, r[:], op0=ALU.is_gt, op1=ALU.subtract), reads=[r], writes=[f])
                k.op("dve", lambda: V.scalar_tensor_tensor(r[:], f[:], 0.5, f[:], op0=ALU.is_gt, op1=ALU.subtract), reads=[f], writes=[r])
                k.op("act", lambda: A.activation(out[:], r[:], AF.Sin, scale=TWO_PI), reads=[r], writes=[out])
            sin_of(o["li"], 0.0)
            sin_of(o["lr"], np.pi / 2)
            k.op("act", lambda: A.activation(o["ii"][:], mag[:], AF.Exp, scale=-1.0), reads=[mag], writes=[o["ii"]])
            k.op("act", lambda: A.activation(mag[:], mag[:], AF.Exp), reads=[mag], writes=[mag])
            k.op("dve", lambda: V.tensor_tensor(o["ir"][:], o["lr"][:], o["ii"][:], op=ALU.mult), reads=[o["lr"], o["ii"]], writes=[o["ir"]])
            k.op("dve", lambda: V.scalar_tensor_tensor(o["ii"][:], o["li"][:], -1.0, o["ii"][:], op0=ALU.mult, op1=ALU.mult), reads=[o["li"], o["ii"]], writes=[o["ii"]])
            k.op("dve", lambda: V.tensor_tensor(o["lr"][:], o["lr"][:], mag[:], op=ALU.mult), reads=[o["lr"], mag], writes=[o["lr"]])
            k.op("dve", lambda: V.tensor_tensor(o["li"][:], o["li"][:], mag[:], op=ALU.mult), reads=[o["li"], mag], writes=[o["li"]])
            a, b_, den = o["t1"], o["t2"], o["t3"]
            k.op("dve", lambda: V.tensor_scalar(a[:], o["lr"][:], -1.0, None, op0=ALU.add), reads=[o["lr"]], writes=[a])
            k.op("dve", lambda: V.tensor_tensor(den[:], src[:, 0, :], src[:, 0, :], op=ALU.mult), reads=[src], writes=[den])
            k.op("dve", lambda: V.tensor_tensor(b_[:], src[:, 1, :], src[:, 1, :], op=ALU.mult), reads=[src], writes=[b_])
            k.op("dve", lambda: V.tensor_tensor(den[:], den[:], b_[:], op=ALU.add), reads=[den, b_], writes=[den])
            k.op("dve", lambda: V.reciprocal(den[:], den[:]), reads=[den], writes=[den])
            k.op("dve", lambda: V.tensor_tensor(o["wr"][:], a[:], src[:, 0, :], op=ALU.mult), reads=[a, src], writes=[o["wr"]])
            k.op("dve", lambda: V.tensor_tensor(b_[:], o["li"][:], src[:, 1, :], op=ALU.mult), reads=[o["li"], src], writes=[b_])
            k.op("dve", lambda: V.tensor_tensor(o["wr"][:], o["wr"][:], b_[:], op=ALU.add), reads=[o["wr"], b_], writes=[o["wr"]])
            k.op("dve", lambda: V.tensor_tensor(o["wr"][:], o["wr"][:], den[:], op=ALU.mult), reads=[o["wr"], den], writes=[o["wr"]])
            k.op("dve", lambda: V.tensor_tensor(o["wi"][:], o["li"][:], src[:, 0, :], op=ALU.mult), reads=[o["li"], src], writes=[o["wi"]])
            k.op("dve", lambda: V.tensor_tensor(b_[:], a[:], src[:, 1, :], op=ALU.mult), reads=[a, src], writes=[b_])
            k.op("dve", lambda: V.tensor_tensor(o["wi"][:], o["wi"][:], b_[:], op=ALU.subtract), reads=[o["wi"], b_], writes=[o["wi"]])
            k.op("dve", lambda: V.tensor_tensor(o["wi"][:], o["wi"][:], den[:], op=ALU.mult), reads=[o["wi"], den], writes=[o["wi"]])
            return o

        lamc = k.sb("lamc", [128, 3, 16])
        k.dma("sp", lamc[:], lamc_d[:], reads=[lamc_d], writes=[lamc])
        cc = ssm_consts(lamc, 16, "c_")
        bbT = [k.sb("bbT%d" % i, [128, 16, 128]) for i in range(2)]
        with k.scope():
            lamr = k.sb("lamr", [128, 3, 2048])
            for i in range(3):
                k.dma("sp", lamr[:, i, :], lamr_d[i:i + 1, :].partition_broadcast(128), reads=[lamr_d], writes=[lamr])
            cr = ssm_consts(lamr, 2048, "r_")
            btp = [k.sb("btp%d" % i, [128, 2048]) for i in range(2)]
            for i in range(2):
                k.dma("act", btp[i][:], btp_d[i].rearrange("p s t -> p (s t)"), reads=[btp_d], writes=[btp[i]])
            t1 = cr["t1"]
            bre, bim = (bbT[0][:].rearrange("p s t -> p (s t)"), bbT[1][:].rearrange("p s t -> p (s t)"))
            k.op("dve", lambda: V.tensor_tensor(bre, btp[0][:], cr["wr"][:], op=ALU.mult), reads=[btp[0], cr["wr"]], writes=[bbT[0]])
            k.op("dve", lambda: V.tensor_tensor(t1[:], btp[1][:], cr["wi"][:], op=ALU.mult), reads=[btp[1], cr["wi"]], writes=[t1])
            k.op("dve", lambda: V.tensor_tensor(bre, bre, t1[:], op=ALU.subtract), reads=[bbT[0], t1], writes=[bbT[0]])
            k.op("dve", lambda: V.tensor_tensor(bim, btp[0][:], cr["wi"][:], op=ALU.mult), reads=[btp[0], cr["wi"]], writes=[bbT[1]])
            k.op("dve", lambda: V.tensor_tensor(t1[:], btp[1][:], cr["wr"][:], op=ALU.mult), reads=[btp[1], cr["wr"]], writes=[t1])
            k.op("dve", lambda: V.tensor_tensor(bim, bim, t1[:], op=ALU.add), reads=[bbT[1], t1], writes=[bbT[1]])
        ctp = [k.sb("ctp%d" % i, [128, 16, 32]) for i in range(2)]
        for i in range(2):
            k.dma("sp", ctp[i][:], ctp_d[i], reads=[ctp_d], writes=[ctp[i]])
        k.op("dve", lambda: V.tensor_scalar(ctp[1][:], ctp[1][:], -1.0, None, op0=ALU.mult), reads=[ctp[1]], writes=[ctp[1]])
        dskc = k.sb("dskc", [128, 4])
        k.dma("sp", dskc[:], dskc_d[:], reads=[dskc_d], writes=[dskc])
        diagD = k.sb("diagD", [128, 4, 128])
        for ut in range(4):
            k.op("dve", lambda ut=ut: V.tensor_scalar(diagD[:, ut, :], ident[:], dskc[:, ut:ut + 1], None, op0=ALU.mult), reads=[ident, dskc], writes=[diagD])
        w_glu = k.sb("w_glu", [128, 4, 512])
        k.dma("act", w_glu[:], w_glu_d.t.rearrange("(k p) c -> p k c", p=128), reads=[w_glu_d], writes=[w_glu])
        b_glu = k.sb("b_glu", [1, 512])
        k.dma("act", b_glu[:], b_glu_d[:], reads=[b_glu_d], writes=[b_glu])
        ones_row = k.sb("ones_row", [1, 128])
        k.op("dve", lambda: V.memset(ones_row[:], 1.0), writes=[ones_row])
        ones_b = k.sb("ones_b", [128, 128])
        k.op("dve", lambda: V.memset(ones_b[:], 1.0), writes=[ones_b])
        ones_row_b = ones_b
        parv = k.sb("parv", [128, 1])
        k.dma("sp", parv[:], parv_d[:], reads=[parv_d], writes=[parv])

        Pp = [k.sb("Pp%d" % i, [128, 16, 128]) for i in range(2)]
        Pm = [k.sb("Pm%d" % i, [128, 16, 128]) for i in range(2)]
        ptmp = k.sb("ptmp", [128, 2, 16, 64])
        L128 = [k.sb("L128_%d" % i, [128, 16]) for i in range(2)]
        L127 = [k.sb("L127_%d" % i, [128, 16]) for i in range(2)]

        def cmul_small(o_re, o_im, a_re, a_im, b_re, b_im, bufs_r, bufs_w, t_re, t_im, tb):
            k.op("dve", lambda: V.tensor_tensor(t_re, a_re, b_re, op=ALU.mult), reads=bufs_r, writes=[tb])
            k.op("dve", lambda: V.tensor_tensor(t_im, a_im, b_im, op=ALU.mult), reads=bufs_r, writes=[tb])
            k.op("dve", lambda: V.tensor_tensor(t_re, t_re, t_im, op=ALU.subtract), reads=[tb], writes=[tb])
            k.op("dve", lambda: V.tensor_tensor(t_im, a_re, b_im, op=ALU.mult), reads=bufs_r, writes=[tb])
            k.op("dve", lambda: V.tensor_tensor(o_im, a_im, b_re, op=ALU.mult), reads=bufs_r + [tb], writes=bufs_w)
            k.op("dve", lambda: V.tensor_tensor(o_im, o_im, t_im, op=ALU.add), reads=bufs_w + [tb], writes=bufs_w)
            k.op("dve", lambda: V.tensor_copy(o_re, t_re), reads=[tb], writes=bufs_w)

        def build_pow(P, l_re, l_im, lbuf, tag):
            cur = [k.sb(tag + "cur%d" % i, [128, 16]) for i in range(2)]
            nxt = [k.sb(tag + "nxt%d" % i, [128, 16]) for i in range(2)]
            tmpb = k.sb(tag + "tmpb", [128, 2, 16])
            k.op("dve", lambda: V.memset(P[0][:, :, 0:1], 1.0), writes=[P[0]])
            k.op("dve", lambda: V.memset(P[1][:, :, 0:1], 0.0), writes=[P[1]])
            k.op("dve", lambda: V.tensor_copy(cur[0][:], l_re), reads=[lbuf[0]], writes=[cur[0]])
            k.op("dve", lambda: V.tensor_copy(cur[1][:], l_im), reads=[lbuf[1]], writes=[cur[1]])
            m = 1
            while m < 128:
                br = cur[0][:].unsqueeze(2).to_broadcast([128, 16, m])
                bi = cur[1][:].unsqueeze(2).to_broadcast([128, 16, m])
                cmul_small(P[0][:, :, m:2 * m], P[1][:, :, m:2 * m], P[0][:, :, 0:m], P[1][:, :, 0:m], br, bi,
                           [P[0], P[1], cur[0], cur[1]], [P[0], P[1]], ptmp[:, 0, :, 0:m], ptmp[:, 1, :, 0:m], ptmp)
                cmul_small(nxt[0][:], nxt[1][:], cur[0][:], cur[1][:], cur[0][:], cur[1][:], [cur[0], cur[1]], [nxt[0], nxt[1]],
                           tmpb[:, 0, :], tmpb[:, 1, :], tmpb)
                cur, nxt = nxt, cur
                m *= 2
            return cur

        l128 = build_pow(Pp, cc["lr"][:], cc["li"][:], [cc["lr"], cc["li"]], "pp_")
        build_pow(Pm, cc["ir"][:], cc["ii"][:], [cc["ir"], cc["ii"]], "pm_")
        for i in range(2):
            k.op("dve", lambda i=i: V.tensor_copy(L128[i][:], l128[i][:]), reads=[l128[i]], writes=[L128[i]])
            k.op("dve", lambda i=i: V.tensor_copy(L127[i][:], Pp[i][:, :, 127]), reads=[Pp[i]], writes=[L127[i]])

        E = [k.sb("E%d" % i, [128, 16, NB]) for i in range(2)]
        uTg = [k.sb("uTg%d" % i, [128, 4, 512]) for i in range(2)]
        bus = [[k.sb("bus%d_%d" % (j, i), [128, 512]) for i in range(2)] for j in range(2)]
        pr = [k.sb("pr%d" % i, [128, 512]) for i in range(4)]

        def bu_group(src_d, gi, s, par_):
            ug = uTg[gi % 2]
            for i in range(2):
                ps = k.nextps()
                k.op("pe", lambda i=i, ps=ps: PE.matmul(ps[:, :], bbT[i][:, s, :], ug[:, s // 4, :], start=True, stop=True),
                     reads=[bbT[i], ug], writes=[ps], pe_acc=True)
                k.op("act", lambda i=i, ps=ps: A.copy(bus[par_][i][:], ps[:]), reads=[ps], writes=[bus[par_][i]])
            return bus[par_]

        def v4(ap):
            return ap.rearrange("p (a b) -> p a b", b=128)

        def bc4(ap):
            return ap.unsqueeze(1).to_broadcast([128, 4, 128])

        for gi in range(T // 512):
            ug = uTg[gi % 2]
            g = {}
            for ut in range(4):
                k.dma("sp", ug[:, ut, :], zT_u[ut, :, gi * 512:(gi + 1) * 512], reads=[zT_u], writes=[ug], grp=g)
            for s in range(16):
                b = bu_group(zT_u, gi, s, s % 2)
                pmr, pmi = bc4(Pm[0][:, s, :]), bc4(Pm[1][:, s, :])
                k.op("dve", lambda: V.tensor_tensor(v4(pr[0][:]), v4(b[0][:]), pmr, op=ALU.mult), reads=[b[0], Pm[0]], writes=[pr[0]])
                k.op("dve", lambda: V.tensor_tensor(v4(pr[1][:]), v4(b[1][:]), pmi, op=ALU.mult), reads=[b[1], Pm[1]], writes=[pr[1]])
                k.op("dve", lambda: V.tensor_tensor(pr[0][:], pr[0][:], pr[1][:], op=ALU.subtract), reads=[pr[0], pr[1]], writes=[pr[0]])
                k.op("dve", lambda: V.tensor_reduce(E[0][:, s, gi * 4:(gi + 1) * 4], v4(pr[0][:]), axis=AX.X, op=ALU.add), reads=[pr[0]], writes=[E[0]])
                k.op("dve", lambda: V.tensor_tensor(v4(pr[2][:]), v4(b[0][:]), pmi, op=ALU.mult), reads=[b[0], Pm[1]], writes=[pr[2]])
                k.op("dve", lambda: V.tensor_tensor(v4(pr[3][:]), v4(b[1][:]), pmr, op=ALU.mult), reads=[b[1], Pm[0]], writes=[pr[3]])
                k.op("dve", lambda: V.tensor_tensor(pr[2][:], pr[2][:], pr[3][:], op=ALU.add), reads=[pr[2], pr[3]], writes=[pr[2]])
                k.op("dve", lambda: V.tensor_reduce(E[1][:, s, gi * 4:(gi + 1) * 4], v4(pr[2][:]), axis=AX.X, op=ALU.add), reads=[pr[2]], writes=[E[1]])

        Hst = [k.sb("Hst%d" % i, [128, 16, NB + 1]) for i in range(2)]
        hb = k.sb("hb", [128, 4, 16])
        for i in range(2):
            k.op("dve", lambda i=i: V.memset(Hst[i][:, :, 0:1], 0.0), writes=[Hst[i]])
        for kb_ in range(NB):
            hr, hi = Hst[0][:, :, kb_], Hst[1][:, :, kb_]
            er, ei = E[0][:, :, kb_], E[1][:, :, kb_]
            k.op("dve", lambda: V.tensor_tensor(hb[:, 0, :], L127[0][:], er, op=ALU.mult), reads=[L127[0], E[0]], writes=[hb])
            k.op("dve", lambda: V.tensor_tensor(hb[:, 1, :], L127[1][:], ei, op=ALU.mult), reads=[L127[1], E[1]], writes=[hb])
            k.op("dve", lambda: V.tensor_tensor(hb[:, 0, :], hb[:, 0, :], hb[:, 1, :], op=ALU.subtract), reads=[hb], writes=[hb])
            k.op("dve", lambda: V.tensor_tensor(hb[:, 1, :], L127[0][:], ei, op=ALU.mult), reads=[L127[0], E[1]], writes=[hb])
            k.op("dve", lambda: V.tensor_tensor(hb[:, 2, :], L127[1][:], er, op=ALU.mult), reads=[L127[1], E[0]], writes=[hb])
            k.op("dve", lambda: V.tensor_tensor(hb[:, 1, :], hb[:, 1, :], hb[:, 2, :], op=ALU.add), reads=[hb], writes=[hb])
            k.op("dve", lambda: V.tensor_tensor(hb[:, 2, :], L128[0][:], hr, op=ALU.mult), reads=[L128[0], Hst[0]], writes=[hb])
            k.op("dve", lambda: V.tensor_tensor(hb[:, 3, :], L128[1][:], hi, op=ALU.mult), reads=[L128[1], Hst[1]], writes=[hb])
            k.op("dve", lambda: V.tensor_tensor(hb[:, 2, :], hb[:, 2, :], hb[:, 3, :], op=ALU.subtract), reads=[hb], writes=[hb])
            k.op("dve", lambda: V.tensor_tensor(Hst[0][:, :, kb_ + 1], hb[:, 2, :], hb[:, 0, :], op=ALU.add), reads=[hb], writes=[Hst[0]])
            k.op("dve", lambda: V.tensor_tensor(hb[:, 2, :], L128[0][:], hi, op=ALU.mult), reads=[L128[0], Hst[1]], writes=[hb])
            k.op("dve", lambda: V.tensor_tensor(hb[:, 3, :], L128[1][:], hr, op=ALU.mult), reads=[L128[1], Hst[0]], writes=[hb])
            k.op("dve", lambda: V.tensor_tensor(hb[:, 2, :], hb[:, 2, :], hb[:, 3, :], op=ALU.add), reads=[hb], writes=[hb])
            k.op("dve", lambda: V.tensor_tensor(Hst[1][:, :, kb_ + 1], hb[:, 2, :], hb[:, 1, :], op=ALU.add), reads=[hb], writes=[Hst[1]])
        for i in range(2):
            k.dma("pool", o_ssm_p[i], Hst[i][:, :, NB], reads=[Hst[i]], writes=[o_ssm_p])

        Hown = [k.sb("Hown%d" % i, [128, 16, NOWN]) for i in range(2)]
        LH = [k.sb("LH%d" % i, [128, 16, NOWN]) for i in range(2)]
        for i in range(2):
            he, ho = Hst[i][:, :, 0:NB:2], Hst[i][:, :, 1:NB:2]
            k.op("dve", lambda i=i, he=he, ho=ho: V.tensor_tensor(Hown[i][:], ho, he, op=ALU.subtract), reads=[Hst[i]], writes=[Hown[i]])
            k.op("dve", lambda i=i: V.tensor_scalar(Hown[i][:], Hown[i][:], parv[:, 0:1], None, op0=ALU.mult), reads=[Hown[i], parv], writes=[Hown[i]])
            k.op("dve", lambda i=i, he=he: V.tensor_tensor(Hown[i][:], Hown[i][:], he, op=ALU.add), reads=[Hown[i], Hst[i]], writes=[Hown[i]])
        lrb = cc["lr"][:].unsqueeze(2).to_broadcast([128, 16, NOWN])
        lib = cc["li"][:].unsqueeze(2).to_broadcast([128, 16, NOWN])
        tmpLH = k.sb("tmpLH", [128, 2, 16, NOWN])
        cmul_small(LH[0][:], LH[1][:], Hown[0][:], Hown[1][:], lrb, lib, [Hown[0], Hown[1], cc["lr"], cc["li"]], [LH[0], LH[1]],
                   tmpLH[:, 0], tmpLH[:, 1], tmpLH)

        hbuf = [[k.sb("hbuf%d_%d" % (j, i), [128, 512]) for i in range(2)] for j in range(2)]
        gx = [k.sb("gx%d" % i, [128, 512]) for i in range(2)]
        ysb = k.sb("ysb", [128, 512])
        gel = k.sb("gel", [128, 512])
        gT = k.sb("gT", [128, 4, 128])
        sso = k.sb("sso", [128, 512])

        def glu_tail(psY, P, out_buf, out_writer):
            k.op("act", lambda: A.copy(ysb[0:P, :], psY[0:P, :]), reads=[psY], writes=[ysb])
            gelu_tanh(gel, ysb, sso, P)
            ps = k.nextps()
            for kk in range(4):
                k.op("pe", lambda kk=kk, ps=ps: PE.transpose(ps[:, kk * P:(kk + 1) * P], gel[0:P, kk * 128:(kk + 1) * 128], ident[0:P, 0:P]),
                     reads=[gel, ident], writes=[ps], pe_acc=True)
            k.op("dve", lambda ps=ps: V.tensor_copy(gT[:, :, 0:P], ps[:, 0:4 * P].rearrange("p (a b) -> p a b", b=P)), reads=[ps], writes=[gT])
            ps2 = k.nextps()
            for kk in range(4):
                k.op("pe", lambda kk=kk, ps2=ps2: PE.matmul(ps2[0:P, :], gT[:, kk, 0:P], w_glu[:, kk, :], start=(kk == 0), stop=False),
                     reads=[gT, w_glu], writes=[ps2], pe_acc=True)
            k.op("pe", lambda ps2=ps2: PE.matmul(ps2[0:P, :], ones_row[0:1, 0:P], b_glu[0:1, :], start=False, stop=True),
                 reads=[ones_row, b_glu], writes=[ps2], pe_acc=True)
            k.op("act", lambda ps2=ps2: A.activation(sso[0:P, :], ps2[0:P, :], AF.Sigmoid), reads=[ps2], writes=[sso])
            k.op("dve", lambda: V.tensor_tensor(out_buf[0:P, :], sso[0:P, :], gel[0:P, :], op=ALU.mult), reads=[sso, gel], writes=[out_buf])

        if stage >= 3:
          psY = [k.psb[i] for i in range(4)]
          k.psi = 4
          def nextps_hi():
              b = k.psb[4 + (k.psi % 4)]
              k.psi += 1
              return b
          old_nextps = k.nextps
          for gi in range(NOWN * 128 // 512):
            k.nextps = nextps_hi
            ug = uTg[gi % 2]
            g = {}
            for ut in range(4):
                k.dma("sp", ug[:, ut, :], zT_uo[ut, :, gi * 512:(gi + 1) * 512], reads=[zT_uo], writes=[ug], grp=g)
            for ut in range(4):
                for blk in range(4):
                    k.op("pe", lambda ut=ut, blk=blk: PE.matmul(psY[blk][:, ut * 128:(ut + 1) * 128], ug[:, ut, blk * 128:(blk + 1) * 128], diagD[:, ut, :],
                                                                start=(ut == 0), stop=False, skip_group_check=True),
                         reads=[ug, diagD], writes=[psY[blk]], pe_acc=True)
            for s in range(16):
                b = bu_group(zT_uo, gi, s, s % 2)
                hb_ = hbuf[s % 2]
                pmr, pmi = bc4(Pm[0][:, s, :]), bc4(Pm[1][:, s, :])
                ppr, ppi = bc4(Pp[0][:, s, :]), bc4(Pp[1][:, s, :])
                k.op("dve", lambda: V.tensor_tensor(v4(pr[0][:]), v4(b[0][:]), pmr, op=ALU.mult), reads=[b[0], Pm[0]], writes=[pr[0]])
                k.op("dve", lambda: V.tensor_tensor(v4(pr[1][:]), v4(b[1][:]), pmi, op=ALU.mult), reads=[b[1], Pm[1]], writes=[pr[1]])
                k.op("dve", lambda: V.tensor_tensor(pr[0][:], pr[0][:], pr[1][:], op=ALU.subtract), reads=[pr[0], pr[1]], writes=[pr[0]])
                k.op("dve", lambda: V.tensor_tensor(v4(pr[2][:]), v4(b[0][:]), pmi, op=ALU.mult), reads=[b[0], Pm[1]], writes=[pr[2]])
                k.op("dve", lambda: V.tensor_tensor(v4(pr[3][:]), v4(b[1][:]), pmr, op=ALU.mult), reads=[b[1], Pm[0]], writes=[pr[3]])
                k.op("dve", lambda: V.tensor_tensor(pr[2][:], pr[2][:], pr[3][:], op=ALU.add), reads=[pr[2], pr[3]], writes=[pr[2]])
                for blk in range(4):
                    n = gi * 4 + blk
                    sl = slice(blk * 128, (blk + 1) * 128)
                    k.op("dve", lambda sl=sl, n=n: V.tensor_tensor_scan(gx[0][:, sl], ones_row_b[:, 0:128], pr[0][:, sl], LH[0][:, s, n:n + 1], op0=ALU.mult, op1=ALU.add),
                         reads=[pr[0], LH[0], ones_b], writes=[gx[0]])
                    k.op("dve", lambda sl=sl, n=n: V.tensor_tensor_scan(gx[1][:, sl], ones_row_b[:, 0:128], pr[2][:, sl], LH[1][:, s, n:n + 1], op0=ALU.mult, op1=ALU.add),
                         reads=[pr[2], LH[1], ones_b], writes=[gx[1]])
                k.op("dve", lambda: V.tensor_tensor(v4(pr[0][:]), v4(gx[0][:]), ppr, op=ALU.mult), reads=[gx[0], Pp[0]], writes=[pr[0]])
                k.op("dve", lambda: V.tensor_tensor(v4(pr[1][:]), v4(gx[1][:]), ppi, op=ALU.mult), reads=[gx[1], Pp[1]], writes=[pr[1]])
                k.op("dve", lambda: V.tensor_tensor(hb_[0][:], pr[0][:], pr[1][:], op=ALU.subtract), reads=[pr[0], pr[1]], writes=[hb_[0]])
                k.op("dve", lambda: V.tensor_tensor(v4(pr[2][:]), v4(gx[0][:]), ppi, op=ALU.mult), reads=[gx[0], Pp[1]], writes=[pr[2]])
                k.op("dve", lambda: V.tensor_tensor(v4(pr[3][:]), v4(gx[1][:]), ppr, op=ALU.mult), reads=[gx[1], Pp[0]], writes=[pr[3]])
                k.op("dve", lambda: V.tensor_tensor(hb_[1][:], pr[2][:], pr[3][:], op=ALU.add), reads=[pr[2], pr[3]], writes=[hb_[1]])
                for blk in range(4):
                    for i in range(2):
                        k.op("pe", lambda blk=blk, i=i: PE.matmul(psY[blk][:, s * 32:(s + 1) * 32], hb_[i][:, blk * 128:(blk + 1) * 128], ctp[i][:, s, :],
                                                                  start=False, stop=(i == 1), skip_group_check=True),
                             reads=[hb_[i], ctp[i]], writes=[psY[blk]], pe_acc=True)
            for blk in range(4):
                n = gi * 4 + blk
                glu_tail(psY[blk], 128, sso, None)
                k.dma("pool", ssm_out_d[n * 128:(n + 1) * 128, :], sso[:], reads=[sso], writes=[ssm_out_d])
          k.nextps = old_nextps
          k.psi = 0

        h0c = [k.sb("h0c%d" % i, [128, 16, NS]) for i in range(2)]
        hs = [k.sb("hs%d" % i, [128, 16, NS]) for i in range(2)]
        for i in range(2):
            k.dma("sp", h0c[i][:], h0c_d[i], reads=[h0c_d], writes=[h0c[i]])
        psb_ = [k.nextps(), k.nextps()]
        for i in range(2):
            for s in range(16):
                k.op("pe", lambda i=i, s=s: PE.matmul(psb_[i][:, s * NS:(s + 1) * NS], bbT[i][:, s, :], zsT[:, s // 4, :], start=True, stop=True),
                     reads=[bbT[i], zsT], writes=[psb_[i]], pe_acc=True)
        lrs = cc["lr"][:].unsqueeze(2).to_broadcast([128, 16, NS])
        lis = cc["li"][:].unsqueeze(2).to_broadcast([128, 16, NS])
        tmpS = k.sb("tmpS", [128, 2, 16, NS])
        cmul_small(hs[0][:], hs[1][:], h0c[0][:], h0c[1][:], lrs, lis, [h0c[0], h0c[1], cc["lr"], cc["li"]], [hs[0], hs[1]],
                   tmpS[:, 0], tmpS[:, 1], tmpS)
        for i in range(2):
            k.op("dve", lambda i=i: V.tensor_tensor(hs[i][:], hs[i][:], psb_[i][:, 0:16 * NS].rearrange("p (a b) -> p a b", b=NS), op=ALU.add),
                 reads=[hs[i], psb_[i]], writes=[hs[i]])
            k.dma("pool", o_ssm_s[i], hs[i][:], reads=[hs[i]], writes=[o_ssm_s])
        psy = k.nextps()
        for ut in range(4):
            k.op("pe", lambda ut=ut: PE.matmul(psy[0:NS, ut * 128:(ut + 1) * 128], zsT[:, ut, :], diagD[:, ut, :], start=(ut == 0), stop=False, skip_group_check=True),
                 reads=[zsT, diagD], writes=[psy], pe_acc=True)
        for s in range(16):
            for i in range(2):
                k.op("pe", lambda s=s, i=i: PE.matmul(psy[0:NS, s * 32:(s + 1) * 32], hs[i][:, s, :], ctp[i][:, s, :], start=False, stop=(i == 1), skip_group_check=True),
                     reads=[hs[i], ctp[i]], writes=[psy], pe_acc=True)
        glu_tail(psy, NS, ssm_out_s, None)

    NEG = -1.0e30
    if stage >= 4:
      with k.scope():
        KT_sel = k.sb("KT_sel", [128, T], BF16); KT_win = k.sb("KT_win", [128, T], BF16)
        V_sel = k.sb("V_sel", [128, NB, 128]); V_win = k.sb("V_win", [128, NB, 128])
        for hf_ in range(2):
            k.dma("pool", KT_sel[:, hf_ * 2048:(hf_ + 1) * 2048], zT_kv[2, :, hf_ * 2048:(hf_ + 1) * 2048], reads=[zT_kv], writes=[KT_sel])
            k.dma("pool", KT_win[:, hf_ * 2048:(hf_ + 1) * 2048], zT_kv[4, :, hf_ * 2048:(hf_ + 1) * 2048], reads=[zT_kv], writes=[KT_win])
        k.dma("sp", V_sel[:], o_sel_p.t.rearrange("(n p) c -> p n c", p=128)[:, :, 128:256], reads=[o_sel_p], writes=[V_sel])
        k.dma("act", V_win[:], o_win_p.t.rearrange("(n p) c -> p n c", p=128)[:, :, 128:256], reads=[o_win_p], writes=[V_win])
        CKT = k.sb("CKT", [128, 256])
        CV = k.sb("CV", [128, 2, 128])
        jflip = k.sb("jflip", [128, 128])
        k.dma("sp", jflip[:], jflip_d[:], reads=[jflip_d], writes=[jflip])
        onesr = k.sb("onesr", [1, 128])
        k.op("dve", lambda: V.memset(onesr[:], 1.0), writes=[onesr])

        with k.scope():
            bd1 = k.sb("bd1", [128, 64, 128]); bd2 = k.sb("bd2", [128, 2, 128])
            pel = k.sb("pel", [128, 2, 2, 16]); pb1 = k.sb("pb1", [128, 2]); pb2 = k.sb("pb2", [128, 2]); pb2r = k.sb("pb2r", [1, 128])
            k.dma("sp", bd1[:], bd1_d[:], reads=[bd1_d], writes=[bd1])
            for (sb_, d_) in ((bd2, bd2_d), (pel, pel_d), (pb1, pb1_d), (pb2, pb2_d), (pb2r, pb2r_d)):
                k.dma("act", sb_[:], d_[:], reads=[d_], writes=[sb_])
            XTc = [k.sb("XTc%d" % c, [128, T]) for c in range(2)]
            for c in range(2):
                k.dma("sp", XTc[c][:], zT_kv[c], reads=[zT_kv], writes=[XTc[c]])
            hdn = [k.sb("hdn%d" % c, [128, 256]) for c in range(2)]
            htmp = k.sb("htmp", [128, 256]); hpre = k.sb("hpre", [128, 256])
            pbias = k.sb("pbias", [128, 2])
            for c in range(2):
                psb_ = k.nextps()
                for half in range(2):
                    for j in range(16):
                        k.op("pe", lambda c=c, half=half, j=j: PE.matmul(psb_[:, 0:1], bd1[:, (c * 2 + half) * 16 + j, :], pel[:, c, half, j:j + 1],
                                                                         start=(half == 0 and j == 0), stop=(half == 1 and j == 15)),
                             reads=[bd1, pel], writes=[psb_], pe_acc=True)
                k.op("dve", lambda c=c: V.tensor_tensor(pbias[:, c:c + 1], psb_[:, 0:1], pb1[:, c:c + 1], op=ALU.add), reads=[psb_, pb1], writes=[pbias])
                ps = k.nextps()
                xv = XTc[c][:].rearrange("p (n j) -> p n j", j=16)
                for half in range(2):
                    for j in range(16):
                        k.op("pe", lambda c=c, half=half, j=j, ps=ps: PE.matmul(ps[:, 0:255], bd1[:, (c * 2 + half) * 16 + j, :], xv[:, half:half + 255, j],
                                                                                start=(half == 0 and j == 0), stop=(half == 1 and j == 15)),
                             reads=[bd1, XTc[c]], writes=[ps], pe_acc=True)
                k.op("dve", lambda c=c: V.memset(hpre[:], 0.0), writes=[hpre])
                k.op("act", lambda c=c, ps=ps: A.activation(hpre[:, 0:255], ps[:, 0:255], AF.Identity, bias=pbias[:, c:c + 1], scale=1.0), reads=[ps, pbias], writes=[hpre])
                gelu_tanh(hdn[c], hpre, htmp, 128)
            ps = k.nextps()
            k.op("pe", lambda: PE.matmul(ps[:, 0:256], bd2[:, 0, :], hdn[0][:], start=True, stop=True), reads=[bd2, hdn[0]], writes=[ps], pe_acc=True)
            k.op("act", lambda: A.activation(CKT[:], ps[:, 0:256], AF.Identity, bias=pb2[:, 0:1], scale=1.0), reads=[ps, pb2], writes=[CKT])
            for i in range(2):
                ps = k.nextps()
                k.op("pe", lambda i=i, ps=ps: PE.matmul(ps[:, 0:128], hdn[1][:, i * 128:(i + 1) * 128], bd2[:, 1, :], start=True, stop=False),
                     reads=[bd2, hdn[1]], writes=[ps], pe_acc=True)
                k.op("pe", lambda i=i, ps=ps: PE.matmul(ps[:, 0:128], onesr[0:1, :], pb2r[0:1, :], start=False, stop=True),
                     reads=[onesr, pb2r], writes=[ps], pe_acc=True)
                k.op("dve", lambda i=i, ps=ps: V.tensor_copy(CV[:, i, :], ps[:, 0:128]), reads=[ps], writes=[CV])

        Tsel = k.sb("Tsel", [128, 10, 8, 128])
        Twin = k.sb("Twin", [128, 2, 8, 128])
        Wc = k.sb("Wc", [128, 8, 34 * 8])
        k.op("dve", lambda: V.memset(Wc[:, :, 33 * 8:34 * 8], NEG), writes=[Wc])
        with k.scope():
            tb = k.sb("tb", [33, 8])
            k.op("dve", lambda: V.memset(tb[:], NEG), writes=[tb])
            k.dma("sp", tb[0:32, :], rel_bias_d[:], reads=[rel_bias_d], writes=[tb])
            oh = k.sb("oh", [33, 2, 1408])
            for w in range(2):
                k.dma("act", oh[:, w, :], oh_d[w], reads=[oh_d], writes=[oh])
            Gs = k.sb("Gs", [8, 2, 1408])
            for w in range(2):
                for c0 in range(0, 1408, 512):
                    cn = min(512, 1408 - c0)
                    ps = k.nextps()
                    k.op("pe", lambda w=w, c0=c0, cn=cn, ps=ps: PE.matmul(ps[0:8, 0:cn], tb[:, :], oh[:, w, c0:c0 + cn], start=True, stop=True),
                         reads=[tb, oh], writes=[ps], pe_acc=True)
                    k.op("dve", lambda w=w, c0=c0, cn=cn, ps=ps: V.tensor_copy(Gs[:, w, c0:c0 + cn], ps[0:8, 0:cn]), reads=[ps], writes=[Gs])
                k.dma("pool", G_d[w], Gs[:, w, :], reads=[Gs], writes=[G_d])
            Hk = [k.sb("Hk%d" % i, [128, 8, 128]) for i in range(2)]
            Y0 = 1151
            jobs = [(0, dp, Tsel, dp + 1) for dp in range(-1, 9)] + [(1, 3, Twin, 0), (1, 4, Twin, 1)]
            for ji, (w, dp, dstT, di) in enumerate(jobs):
                hk = Hk[ji % 2]
                off = Y0 - 128 * dp - 127
                src = bass.AP(G_d.t.tensor, w * 8 * 1408 + off, [[1, 128], [1408, 8], [1, 128]])
                k.dma("sp", hk[:], src, reads=[G_d], writes=[hk])
                for hh in range(2):
                    ps = k.nextps()
                    k.op("pe", lambda hh=hh, ps=ps, hk=hk: PE.matmul(ps[:, :], jflip[:], hk[:, hh * 4:(hh + 1) * 4, :].rearrange("p a b -> p (a b)"), start=True, stop=True),
                         reads=[jflip, hk], writes=[ps], pe_acc=True)
                    k.op("act" if hh else "dve",
                         (lambda hh=hh, ps=ps, dstT=dstT, di=di: A.copy(dstT[:, di, hh * 4:(hh + 1) * 4, :].rearrange("p a b -> p (a b)"), ps[:, :])) if hh else
                         (lambda hh=hh, ps=ps, dstT=dstT, di=di: V.tensor_copy(dstT[:, di, hh * 4:(hh + 1) * 4, :].rearrange("p a b -> p (a b)"), ps[:, :])),
                         reads=[ps], writes=[dstT])
            k.op("dve", lambda: V.tensor_copy(Wc[:, :, 0:192].rearrange("p h (e c) -> p h e c", c=8),
                                              Tsel[:, 9, :, 15:128:16].unsqueeze(2).to_broadcast([128, 8, 24, 8])), reads=[Tsel], writes=[Wc])
            for e in range(24, 33):
                dp = 31 - e
                k.op("dve", lambda e=e, dp=dp: V.tensor_copy(Wc[:, :, e * 8:(e + 1) * 8], Tsel[:, dp + 1, :, 15:128:16]), reads=[Tsel], writes=[Wc])

        qT = [k.sb("qT%d" % i, [128, 4, 128]) for i in range(2)]
        qTb = [k.sb("qTb%d" % i, [128, 4, 128], BF16) for i in range(2)]
        gat = [k.sb("gat%d" % i, [128, 24]) for i in range(2)]
        selc = [k.sb("selc%d" % i, [128, 3, 64]) for i in range(2)]
        S2 = [k.sb("S%d" % i, [128, T]) for i in range(2)]
        Sw2 = [k.sb("Sw%d" % i, [128, 768]) for i in range(2)]
        Pc2 = [k.sb("Pc%d" % i, [128, 256]) for i in range(2)]
        imp = k.sb("imp", [128, 264])
        sblk = k.sb("sblk", [128, 64]); sb2 = k.sb("sb2", [128, 64]); negm = k.sb("negm", [128, 64])
        m8 = k.sb("m8", [128, 8])
        sm2 = [k.sb("smalls%d" % i, [128, 16]) for i in range(2)]
        sm = sm2[0]; S = S2[0]; Sw = Sw2[0]; Pc = Pc2[0]
        PT = [k.sb("PT%d" % i, [128, 512]) for i in range(2)]
        attb = [k.sb("attb%d" % i, [128, 512]) for i in range(2)]
        psO = [k.psb[0], k.psb[1]]; psS = [k.psb[2], k.psb[3]]; psT = [k.psb[4], k.psb[5]]
        cnt = {"s": 0, "t": 0, "o": 0}

        def softmax_rows(Sap, ncols, col_max, col_neg, col_sum, col_rinv, reads):
            k.op("dve", lambda: V.tensor_reduce(sm[:, col_max:col_max + 1], Sap, axis=AX.X, op=ALU.max), reads=reads, writes=[sm])
            k.op("dve", lambda: V.tensor_scalar(sm[:, col_neg:col_neg + 1], sm[:, col_max:col_max + 1], -1.0e4, -1.0, op0=ALU.max, op1=ALU.mult), reads=[sm], writes=[sm])
            k.op("dve", lambda: V.memset(sm[:, col_sum:col_sum + 1], 0.0), writes=[sm])
            k.op("act", lambda: A.activation(Sap, Sap, AF.Exp, bias=sm[:, col_neg:col_neg + 1], scale=1.0, accum_out=sm[:, col_sum:col_sum + 1]),
                 reads=reads + [sm], writes=reads + [sm])
            k.op("dve", lambda: V.tensor_scalar(sm[:, col_rinv:col_rinv + 1], sm[:, col_sum:col_sum + 1], 1.0e-30, None, op0=ALU.max), reads=[sm], writes=[sm])
            k.op("dve", lambda: V.reciprocal(sm[:, col_rinv:col_rinv + 1], sm[:, col_rinv:col_rinv + 1]), reads=[sm], writes=[sm])

        def pv_accum(Pbuf, ntiles, vfn, po, ocol):
            for c0 in range(0, ntiles, 4):
                cn = min(4, ntiles - c0)
                pt = psT[cnt["t"] % 2]; ptb = PT[cnt["t"] % 2]; cnt["t"] += 1
                for i in range(cn):
                    k.op("pe", lambda i=i, pt=pt: PE.transpose(pt[:, i * 128:(i + 1) * 128], Pbuf[:, (c0 + i) * 128:(c0 + i + 1) * 128], ident[:]),
                         reads=[Pbuf, ident], writes=[pt], pe_acc=True)
                if cnt["t"] % 2:
                    k.op("act", lambda pt=pt, ptb=ptb, cn=cn: A.copy(ptb[:, 0:cn * 128], pt[:, 0:cn * 128]), reads=[pt], writes=[ptb])
                else:
                    k.op("dve", lambda pt=pt, ptb=ptb, cn=cn: V.tensor_copy(ptb[:, 0:cn * 128], pt[:, 0:cn * 128]), reads=[pt], writes=[ptb])
                for i in range(cn):
                    jt = c0 + i
                    k.op("pe", lambda i=i, jt=jt, ptb=ptb: PE.matmul(po[:, ocol:ocol + 64], ptb[:, i * 128:(i + 1) * 128], vfn(jt),
                                                                     start=(jt == 0), stop=(jt == ntiles - 1)),
                         reads=[ptb] + vfn.bufs, writes=[po], pe_acc=True)

        for n in range(NOWN):
            qb, gb, scb, ab = qT[n % 2], gat[n % 2], selc[n % 2], attb[n % 2]
            g = {}
            for t in range(4):
                k.dma("sp", qb[:, t, :], zT_q[t, :, n * 128:(n + 1) * 128], reads=[zT_q], writes=[qb], grp=g)
            qbb = qTb[n % 2]
            k.op("act", lambda: A.copy(qbb[:], qb[:]), reads=[qb], writes=[qbb])
            k.dma("sp", gb[:], gates_d[n * 128:(n + 1) * 128, :], reads=[gates_d], writes=[gb])
            for i in range(3):
                k.dma("act", scb[:, i, :], selc_d[i, n], reads=[selc_d], writes=[scb])
            NT = 2 * n + 2
            NKc = 8 * NT
            woff = (31 - 2 * n) * 8 + 1
            for h in range(2):
                hp = slice(64 * h, 64 * h + 64)
                k.op("dve", lambda: V.memset(imp[:], 0.0), writes=[imp])
                pos = []
                for gq in range(4):
                    hh = 4 * h + gq
                    sm = sm2[gq % 2]; Pc = Pc2[gq % 2]
                    po = psO[gq // 2]; ob = (gq % 2) * 192
                    pos.append((po, ob))
                    ps = psS[cnt["s"] % 2]; cnt["s"] += 1
                    k.op("pe", lambda ps=ps, gq=gq: PE.matmul(ps[:, 0:NKc], qb[hp, gq, :], CKT[hp, 0:NKc], start=True, stop=True),
                         reads=[qb, CKT], writes=[ps], pe_acc=True)
                    k.op("dve", lambda ps=ps, hh=hh: V.scalar_tensor_tensor(Pc[:, 0:NKc], ps[:, 0:NKc], 0.125, Wc[:, hh, woff:woff + NKc], op0=ALU.mult, op1=ALU.add),
                         reads=[ps, Wc], writes=[Pc])
                    softmax_rows(Pc[:, 0:NKc], NKc, 0, 1, 2, 3, [Pc])
                    k.op("dve", lambda: V.tensor_scalar(Pc[:, 0:NKc], Pc[:, 0:NKc], sm[:, 3:4], None, op0=ALU.mult), reads=[Pc, sm], writes=[Pc])
                    k.op("dve", lambda: V.tensor_tensor(imp[:, 1:1 + NKc], imp[:, 1:1 + NKc], Pc[:, 0:NKc], op=ALU.add), reads=[imp, Pc], writes=[imp])
                    nct = (NKc + 127) // 128
                    if NKc < nct * 128:
                        k.op("dve", lambda: V.memset(Pc[:, NKc:nct * 128], 0.0), writes=[Pc])
                    vf = lambda jt: CV[:, jt, hp]
                    vf.bufs = [CV]
                    pv_accum(Pc, nct, vf, po, ob)
                k.op("dve", lambda: V.tensor_reduce(sblk[:], imp[:, 0:256].rearrange("p (j m) -> p j m", m=4), axis=AX.X, op=ALU.add), reads=[imp], writes=[sblk])
                k.op("dve", lambda: V.tensor_tensor(sblk[:], sblk[:], imp[:, 4:260:4], op=ALU.add), reads=[sblk, imp], writes=[sblk])
                k.op("dve", lambda: V.tensor_tensor(sblk[:], sblk[:], scb[:, 0, :], op=ALU.mult), reads=[sblk, scb], writes=[sblk])
                k.op("dve", lambda: V.tensor_tensor(sblk[:], sblk[:], scb[:, 1, :], op=ALU.add), reads=[sblk, scb], writes=[sblk])
                k.op("dve", lambda: V.max(m8[:], sblk[:]), reads=[sblk], writes=[m8])
                k.op("dve", lambda: V.match_replace(sb2[:], m8[:], sblk[:], NEG), reads=[m8, sblk], writes=[sb2])
                k.op("dve", lambda: V.max(m8[:], sb2[:]), reads=[sb2], writes=[m8])
                k.op("dve", lambda: V.scalar_tensor_tensor(sb2[:], sblk[:], m8[:, 7:8], scb[:, 2, :], op0=ALU.is_ge, op1=ALU.mult), reads=[sblk, m8, scb], writes=[sb2])
                k.op("dve", lambda: V.tensor_scalar(negm[:], sb2[:], 1.0, 1.0e30, op0=ALU.subtract, op1=ALU.mult), reads=[sb2], writes=[negm])
                for gq in range(4):
                    hh = 4 * h + gq
                    sm = sm2[gq % 2]; S = S2[gq % 2]; Sw = Sw2[gq % 2]
                    po, ob = pos[gq]
                    for c0 in range(0, NT, 4):
                        cn = min(4, NT - c0)
                        ps = psS[cnt["s"] % 2]; cnt["s"] += 1
                        k.op("pe", lambda ps=ps, gq=gq, c0=c0, cn=cn: PE.matmul(ps[:, 0:cn * 128], qbb[hp, gq, :], KT_sel[hp, c0 * 128:(c0 + cn) * 128], start=True, stop=True),
                             reads=[qbb, KT_sel], writes=[ps], pe_acc=True)
                        for i in range(cn):
                            jt = c0 + i
                            di = min(2 * n - jt, 8) + 1
                            k.op("dve", lambda ps=ps, i=i, jt=jt, di=di, hh=hh: V.scalar_tensor_tensor(
                                S[:, jt * 128:(jt + 1) * 128], ps[:, i * 128:(i + 1) * 128], 0.125, Tsel[:, di, hh, :], op0=ALU.mult, op1=ALU.add),
                                 reads=[ps, Tsel], writes=[S])
                    k.op("dve", lambda: V.tensor_tensor(S[:, 0:NT * 128].rearrange("p (b c) -> p b c", c=64), S[:, 0:NT * 128].rearrange("p (b c) -> p b c", c=64),
                                                        negm[:, 0:2 * NT].unsqueeze(2).to_broadcast([128, 2 * NT, 64]), op=ALU.add), reads=[S, negm], writes=[S])
                    softmax_rows(S[:, 0:NT * 128], NT * 128, 4, 5, 6, 7, [S])
                    vf = lambda jt: V_sel[:, jt, hp]
                    vf.bufs = [V_sel]
                    pv_accum(S, NT, vf, po, ob + 64)
                    wt = [jt for jt in range(2 * n - 4, 2 * n + 2) if jt >= 0]
                    nw = len(wt)
                    for c0 in range(0, nw, 4):
                        cn = min(4, nw - c0)
                        ps = psS[cnt["s"] % 2]; cnt["s"] += 1
                        k.op("pe", lambda ps=ps, gq=gq, c0=c0, cn=cn: PE.matmul(ps[:, 0:cn * 128], qbb[hp, gq, :], KT_win[hp, wt[c0] * 128:(wt[c0] + cn) * 128], start=True, stop=True),
                             reads=[qbb, KT_win], writes=[ps], pe_acc=True)
                        for i in range(cn):
                            jt = wt[c0 + i]
                            dp = 2 * n - jt
                            bt = Twin[:, dp - 3, hh, :] if dp >= 3 else Tsel[:, dp + 1, hh, :]
                            k.op("dve", lambda ps=ps, i=i, c0=c0, bt=bt: V.scalar_tensor_tensor(
                                Sw[:, (c0 + i) * 128:(c0 + i + 1) * 128], ps[:, i * 128:(i + 1) * 128], 0.125, bt, op0=ALU.mult, op1=ALU.add),
                                 reads=[ps, Tsel, Twin], writes=[Sw])
                    softmax_rows(Sw[:, 0:nw * 128], nw * 128, 8, 9, 10, 11, [Sw])
                    vf = lambda jt: V_win[:, wt[jt], hp]
                    vf.bufs = [V_win]
                    pv_accum(Sw, nw, vf, po, ob + 128)
                    k.op("dve", lambda hh=hh: V.tensor_tensor(sm[:, 12:13], sm[:, 7:8], gb[:, hh * 3 + 1:hh * 3 + 2], op=ALU.mult), reads=[sm, gb], writes=[sm])
                    k.op("dve", lambda hh=hh: V.tensor_tensor(sm[:, 13:14], sm[:, 11:12], gb[:, hh * 3 + 2:hh * 3 + 3], op=ALU.mult), reads=[sm, gb], writes=[sm])
                    oc = ab[:, hh * 64:(hh + 1) * 64]
                    k.op("dve", lambda hh=hh, po=po, oc=oc, ob=ob: V.tensor_scalar(oc, po[:, ob:ob + 64], gb[:, hh * 3:hh * 3 + 1], None, op0=ALU.mult), reads=[po, gb], writes=[ab])
                    k.op("dve", lambda po=po, oc=oc: V.scalar_tensor_tensor(oc, po[:, ob + 64:ob + 128], sm[:, 12:13], oc, op0=ALU.mult, op1=ALU.add), reads=[po, sm, ab], writes=[ab])
                    k.op("dve", lambda po=po, oc=oc: V.scalar_tensor_tensor(oc, po[:, ob + 128:ob + 192], sm[:, 13:14], oc, op0=ALU.mult, op1=ALU.add), reads=[po, sm, ab], writes=[ab])
            k.dma("pool", att_d[n * 128:(n + 1) * 128, :], ab[:], reads=[ab], writes=[att_d])

    if stage >= 6:
      with k.scope():
        def ld(name, d_, shape, dt=F32, q="sp"):
            b = k.sb(name, shape, dt)
            k.dma(q, b[:], d_[:], reads=[d_], writes=[b])
            return b
        bd1 = ld("s_bd1", bd1_d, [128, 64, 128]); bd2 = ld("s_bd2", bd2_d, [128, 2, 128], q="act")
        pel = ld("s_pel", pel_d, [128, 2, 2, 16], q="act"); pb1 = ld("s_pb1", pb1_d, [128, 2], q="act")
        pb2 = ld("s_pb2", pb2_d, [128, 2], q="act"); pb2r = ld("s_pb2r", pb2r_d, [1, 128], q="act")
        ohc = ld("s_ohc", ohc_d, [33, 1024]); ohs = ld("s_ohs", ohs_d, [33, 9, 128], q="act"); ohw = ld("s_ohw", ohw_d, [33, 5, 128], q="act")
        gsum = ld("s_gsum", gsum_d, [8, 2]); gexp = ld("s_gexp", gexp_d, [2, 8]); selcs = ld("s_selcs", selcs_d, [2, 2, 260])
        half2 = ld("s_half2", half2_d, [2, 128]); m01 = ld("s_m01", m01_d, [8, 2]); pidx = ld("s_pidx", pidx_d, [128, 1])
        tb = k.sb("s_tb", [33, 8])
        k.op("dve", lambda: V.memset(tb[:], NEG), writes=[tb])
        k.dma("sp", tb[0:32, :], rel_bias_d[:], reads=[rel_bias_d], writes=[tb])
        onesr = k.sb("s_onesr", [1, 128]); ones8 = k.sb("s_ones8", [8, 128]); onesc = k.sb("s_onesc", [128, 1])
        for b_ in (onesr, ones8, onesc):
            k.op("dve", lambda b_=b_: V.memset(b_[:], 1.0), writes=[b_])
        pti = k.sb("s_pti", [128, NS * 128], I32); ptf = k.sb("s_ptf", [128, NS * 128]); idx = k.sb("s_idx", [128, NS * 128], I32)
        k.dma("sp", pti[:], pt_d[0:1, :].partition_broadcast(128), reads=[pt_d], writes=[pti])
        k.op("dve", lambda: V.tensor_copy(ptf[:], pti[:]), reads=[pti], writes=[ptf])
        k.op("dve", lambda: V.tensor_scalar(ptf[:], ptf[:], 128.0, pidx[:, 0:1], op0=ALU.mult, op1=ALU.add), reads=[ptf, pidx], writes=[ptf])
        k.op("dve", lambda: V.tensor_copy(idx[:], ptf[:]), reads=[ptf], writes=[idx])
        idxK = k.sb("s_idxK", [128, NS * 128], I32); idxV = k.sb("s_idxV", [128, NS * 128], I32)
        k.op("dve", lambda: V.tensor_scalar(ptf[:], ptf[:], 2.0, None, op0=ALU.mult), reads=[ptf], writes=[ptf])
        k.op("dve", lambda: V.tensor_copy(idxK[:], ptf[:]), reads=[ptf], writes=[idxK])
        k.op("dve", lambda: V.tensor_scalar(ptf[:], ptf[:], 1.0, None, op0=ALU.add), reads=[ptf], writes=[ptf])
        k.op("dve", lambda: V.tensor_copy(idxV[:], ptf[:]), reads=[ptf], writes=[idxV])
        pbias = k.sb("s_pbias", [128, 2])
        for c in range(2):
            psb_ = k.nextps()
            for half in range(2):
                for j_ in range(16):
                    k.op("pe", lambda c=c, half=half, j_=j_: PE.matmul(psb_[:, 0:1], bd1[:, (c * 2 + half) * 16 + j_, :], pel[:, c, half, j_:j_ + 1],
                                                                      start=(half == 0 and j_ == 0), stop=(half == 1 and j_ == 15)),
                         reads=[bd1, pel], writes=[psb_], pe_acc=True)
            k.op("dve", lambda c=c: V.tensor_tensor(pbias[:, c:c + 1], psb_[:, 0:1], pb1[:, c:c + 1], op=ALU.add), reads=[psb_, pb1], writes=[pbias])
        Qbd = k.sb("s_Qbd", [128, NS, 8])
        k.op("dve", lambda: V.memset(Qbd[:], 0.0), writes=[Qbd])
        k.op("dve", lambda: V.tensor_scalar(Qbd[0:64, :, 0:4], zsT[0:64, 4:8, :].rearrange("p g j -> p j g"), 0.125, None, op0=ALU.mult), reads=[zsT], writes=[Qbd])
        k.op("dve", lambda: V.tensor_scalar(Qbd[64:128, :, 4:8], zsT[64:128, 4:8, :].rearrange("p g j -> p j g"), 0.125, None, op0=ALU.mult), reads=[zsT], writes=[Qbd])
        gsb = k.sb("s_gsb", [NS, 24]); g8 = k.sb("s_g8", [8, NS, 3])
        k.op("act", lambda: A.activation(gsb[:], zs[:, 1792:1816], AF.Sigmoid), reads=[zs], writes=[gsb])
        k.dma("pool", gts_d[:, :], gsb[:], reads=[gsb], writes=[gts_d])
        k.dma("pool", g8[:], gts_d.t.rearrange("j (h r) -> h j r", r=3), reads=[gts_d], writes=[g8])

        gt_ = [k.sb("s_gt%d" % i, [128, 256]) for i in range(3)]
        CKTs = k.sb("s_CKT", [128, 1024]); CVs = k.sb("s_CV", [128, 8, 128])
        Sc = k.sb("s_Sc", [8, 1024]); PcT = k.sb("s_PcT", [128, 8, 8])
        impP = k.sb("s_impP", [2, 1040]); sbk = k.sb("s_sbk", [2, 260]); sbk2 = k.sb("s_sbk2", [2, 260]); m8s = k.sb("s_m8", [2, 8])
        sel8 = k.sb("s_sel8", [8, 258]); B2 = k.sb("s_B2", [2, 129, 8]); nmT = k.sb("s_nmT", [128, 129, 8])
        ST = k.sb("s_ST", [128, 129, 8]); KTu = k.sb("s_KTu", [128, 512])
        knew = k.sb("s_knew", [128, 128])
        hold = {}
        sms = k.sb("s_sms", [128, 24]); pm = k.sb("s_pm", [128, 8]); dg = k.sb("s_dg", [8, 8]); nb = k.sb("s_nb", [128, 8])
        o8 = k.sb("s_o8", [8, 3, 64]); ot = k.sb("s_ot", [8, 128]); attj = k.sb("s_attj", [8, 64])
        k.op("dve", lambda: V.memset(knew[:], 0.0), writes=[knew])

        def sel_half(dst, src128, reads):
            k.op("dve", lambda: V.tensor_scalar(dst, src128[:, 0:64], m01[:, 0:1], None, op0=ALU.mult), reads=reads + [m01], writes=[o8])
            k.op("dve", lambda: V.scalar_tensor_tensor(dst, src128[:, 64:128], m01[:, 1:2], dst, op0=ALU.mult, op1=ALU.add), reads=reads + [m01, o8], writes=[o8])

        def attend_T(NP, oidx):
            Sv = ST[:, 0:NP, :]
            k.op("dve", lambda: V.tensor_reduce(pm[:], Sv.rearrange("p n h -> p h n"), axis=AX.X, op=ALU.max), reads=[ST], writes=[pm])
            ps = k.nextps()
            k.op("pe", lambda: PE.transpose(ps[0:8, 0:128], pm[:, :], ident[:]), reads=[pm, ident], writes=[ps], pe_acc=True)
            k.op("dve", lambda: V.tensor_reduce(sms[0:8, 0:1], ps[0:8, 0:128], axis=AX.X, op=ALU.max), reads=[ps], writes=[sms])
            k.op("dve", lambda: V.tensor_scalar(sms[0:8, 1:2], sms[0:8, 0:1], -1.0e4, -1.0, op0=ALU.max, op1=ALU.mult), reads=[sms], writes=[sms])
            k.op("dve", lambda: V.tensor_scalar(dg[:], ident[0:8, 0:8], sms[0:8, 1:2], None, op0=ALU.mult), reads=[ident, sms], writes=[dg])
            ps2 = k.nextps()
            k.op("pe", lambda: PE.matmul(ps2[:, 0:8], ones8[:, :], dg[:, :], start=True, stop=True), reads=[ones8, dg], writes=[ps2], pe_acc=True)
            k.op("dve", lambda: V.tensor_copy(nb[:], ps2[:, 0:8]), reads=[ps2], writes=[nb])
            k.op("dve", lambda: V.tensor_tensor(Sv, Sv, nb[:].unsqueeze(1).to_broadcast([128, NP, 8]), op=ALU.add), reads=[ST, nb], writes=[ST])
            k.op("act", lambda: A.activation(Sv, Sv, AF.Exp), reads=[ST], writes=[ST])
            k.op("dve", lambda: V.tensor_reduce(pm[:], Sv.rearrange("p n h -> p h n"), axis=AX.X, op=ALU.add), reads=[ST], writes=[pm])
            ps3 = k.nextps()
            k.op("pe", lambda: PE.matmul(ps3[0:8, 0:1], pm[:, :], onesc[:, :], start=True, stop=True), reads=[pm, onesc], writes=[ps3], pe_acc=True)
            k.op("dve", lambda: V.tensor_scalar(sms[0:8, 2:3], ps3[0:8, 0:1], 1.0e-30, None, op0=ALU.max), reads=[ps3], writes=[sms])
            k.op("dve", lambda: V.reciprocal(sms[0:8, 2:3], sms[0:8, 2:3]), reads=[sms], writes=[sms])
            ps4 = k.nextps()
            for pg in range(NP):
                k.op("pe", lambda pg=pg: PE.matmul(ps4[0:8, 0:128], ST[:, pg, :], hold["V_all"][:, pg, :], start=(pg == 0), stop=(pg == NP - 1)),
                     reads=[ST, hold["V_all"]], writes=[ps4], pe_acc=True)
            k.op("dve", lambda: V.tensor_scalar(ot[:], ps4[0:8, 0:128], sms[0:8, 2:3], None, op0=ALU.mult), reads=[ps4, sms], writes=[ot])
            sel_half(o8[:, oidx, :], ot, [ot])

        def score_pages(j, kt_tiles, pg0, oh_idx_fn):
            npg = len(kt_tiles)
            for c0 in range(0, npg, 64):
                cn = min(64, npg - c0)
                ps = k.nextps()
                for i in range(cn):
                    k.op("pe", lambda i=i, ps=ps: PE.matmul(ps[:, i * 8:(i + 1) * 8], kt_tiles[c0 + i][0], Qbd[:, j, :], start=True, stop=False),
                         reads=[kt_tiles[c0 + i][1], Qbd], writes=[ps], pe_acc=True)
                    oa, ob_ = oh_idx_fn(pg0 + c0 + i)
                    k.op("pe", lambda i=i, ps=ps, oa=oa: PE.matmul(ps[:, i * 8:(i + 1) * 8], oa, tb[:, :], start=False, stop=True),
                         reads=[ob_, tb], writes=[ps], pe_acc=True)
                k.op("dve", lambda ps=ps, c0=c0, cn=cn: V.tensor_copy(ST[:, pg0 + c0:pg0 + c0 + cn, :], ps[:, 0:cn * 8].rearrange("p (n h) -> p n h", h=8)),
                     reads=[ps], writes=[ST])

        for j in range(NS):
          with k.scope():
            XT = [k.sb("s_XT%d" % c, [128, 4096]) for c in range(2)]
            fs = [[k.sb("s_fs%d_%d" % (c, hf), [128, 1024]) for hf in range(2)] for c in range(2)]
            hdn = [k.sb("s_hdn%d" % c, [128, 1024]) for c in range(2)]
            htmp = k.sb("s_htmp", [128, 1024])
            for u in range(4):
                for pg4 in range(0, 32, 2):
                    psk = k.nextps(); psv = k.nextps()
                    for i in range(2):
                        pg = u * 32 + pg4 + i
                        gtile = gt_[pg % 3]
                        k.dma("pool", None, None, reads=[pool_cmp, idx], writes=[gtile],
                              fn=lambda pg=pg, gtile=gtile: G.indirect_dma_start(out=gtile[:], out_offset=None, in_=pool_cmp[:, :],
                                                                                 in_offset=bass.IndirectOffsetOnAxis(ap=idx[:, j * 128 + pg:j * 128 + pg + 1], axis=0)))
                        k.op("pe", lambda i=i, gtile=gtile: PE.transpose(psk[:, i * 128:(i + 1) * 128], gtile[:, 0:128], ident[:]), reads=[gtile, ident], writes=[psk], pe_acc=True)
                        k.op("pe", lambda i=i, gtile=gtile: PE.transpose(psv[:, i * 128:(i + 1) * 128], gtile[:, 128:256], ident[:]), reads=[gtile, ident], writes=[psv], pe_acc=True)
                    o0 = pg4 * 128
                    k.op("dve", lambda psk=psk, o0=o0: V.tensor_copy(XT[0][:, o0:o0 + 256], psk[:, 0:256]), reads=[psk], writes=[XT[0]])
                    k.op("act", lambda psv=psv, o0=o0: A.copy(XT[1][:, o0:o0 + 256], psv[:, 0:256]), reads=[psv], writes=[XT[1]])
                for c in range(2):
                    xv = XT[c][:].rearrange("p (n j) -> p n j", j=16)
                    for half in range(2):
                        ps = k.nextps()
                        for j_ in range(16):
                            k.op("pe", lambda c=c, half=half, j_=j_, ps=ps: PE.matmul(ps[:, 0:256], bd1[:, (c * 2 + half) * 16 + j_, :], xv[:, :, j_], start=(j_ == 0), stop=(j_ == 15)),
                                 reads=[bd1, XT[c]], writes=[ps], pe_acc=True)
                        if half:
                            k.op("act", lambda ps=ps, c=c, half=half: A.copy(fs[c][half][:, u * 256:(u + 1) * 256], ps[:, 0:256]), reads=[ps], writes=[fs[c][half]])
                        else:
                            k.op("dve", lambda ps=ps, c=c, half=half: V.tensor_copy(fs[c][half][:, u * 256:(u + 1) * 256], ps[:, 0:256]), reads=[ps], writes=[fs[c][half]])
            for c in range(2):
                k.op("dve", lambda c=c: V.memset(fs[c][0][:, 1023:1024], 0.0), writes=[fs[c][0]])
                k.op("dve", lambda c=c: V.tensor_tensor(fs[c][0][:, 0:1023], fs[c][0][:, 0:1023], fs[c][1][:, 1:1024], op=ALU.add), reads=[fs[c][0], fs[c][1]], writes=[fs[c][0]])
                k.op("act", lambda c=c: A.activation(fs[c][0][:, 0:1023], fs[c][0][:, 0:1023], AF.Identity, bias=pbias[:, c:c + 1], scale=1.0), reads=[fs[c][0], pbias], writes=[fs[c][0]])
                gelu_tanh(hdn[c], fs[c][0], htmp, 128)
            for hf in range(2):
                ps = k.nextps()
                k.op("pe", lambda hf=hf, ps=ps: PE.matmul(ps[:, :], bd2[:, 0, :], hdn[0][:, hf * 512:(hf + 1) * 512], start=True, stop=True), reads=[bd2, hdn[0]], writes=[ps], pe_acc=True)
                k.op("act", lambda hf=hf, ps=ps: A.activation(CKTs[:, hf * 512:(hf + 1) * 512], ps[:, :], AF.Identity, bias=pb2[:, 0:1], scale=1.0), reads=[ps, pb2], writes=[CKTs])
            for i in range(8):
                ps = k.nextps()
                k.op("pe", lambda i=i, ps=ps: PE.matmul(ps[:, 0:128], hdn[1][:, i * 128:(i + 1) * 128], bd2[:, 1, :], start=True, stop=False), reads=[bd2, hdn[1]], writes=[ps], pe_acc=True)
                k.op("pe", lambda i=i, ps=ps: PE.matmul(ps[:, 0:128], onesr[0:1, :], pb2r[0:1, :], start=False, stop=True), reads=[onesr, pb2r], writes=[ps], pe_acc=True)
                k.op("dve", lambda i=i, ps=ps: V.tensor_copy(CVs[:, i, :], ps[:, 0:128]), reads=[ps], writes=[CVs])
            for hf in range(2):
                ps = k.nextps()
                k.op("pe", lambda hf=hf, ps=ps: PE.matmul(ps[0:8, :], Qbd[:, j, :], CKTs[:, hf * 512:(hf + 1) * 512], start=True, stop=False), reads=[Qbd, CKTs], writes=[ps], pe_acc=True)
                k.op("pe", lambda hf=hf, ps=ps: PE.matmul(ps[0:8, :], tb[:, :], ohc[:, hf * 512:(hf + 1) * 512], start=False, stop=True), reads=[tb, ohc], writes=[ps], pe_acc=True)
                k.op("dve", lambda hf=hf, ps=ps: V.tensor_copy(Sc[:, hf * 512:(hf + 1) * 512], ps[0:8, :]), reads=[ps], writes=[Sc])
            k.op("dve", lambda: V.tensor_reduce(sms[0:8, 4:5], Sc[:], axis=AX.X, op=ALU.max), reads=[Sc], writes=[sms])
            k.op("dve", lambda: V.tensor_scalar(sms[0:8, 5:6], sms[0:8, 4:5], -1.0e4, -1.0, op0=ALU.max, op1=ALU.mult), reads=[sms], writes=[sms])
            k.op("dve", lambda: V.memset(sms[0:8, 6:7], 0.0), writes=[sms])
            k.op("act", lambda: A.activation(Sc[:], Sc[:], AF.Exp, bias=sms[0:8, 5:6], scale=1.0, accum_out=sms[0:8, 6:7]), reads=[Sc, sms], writes=[Sc, sms])
            k.op("dve", lambda: V.tensor_scalar(sms[0:8, 7:8], sms[0:8, 6:7], 1.0e-30, None, op0=ALU.max), reads=[sms], writes=[sms])
            k.op("dve", lambda: V.reciprocal(sms[0:8, 7:8], sms[0:8, 7:8]), reads=[sms], writes=[sms])
            k.op("dve", lambda: V.tensor_scalar(Sc[:], Sc[:], sms[0:8, 7:8], None, op0=ALU.mult), reads=[Sc, sms], writes=[Sc])
            k.op("dve", lambda: V.memset(impP[:], 0.0), writes=[impP])
            for hf in range(2):
                ps = k.nextps()
                k.op("pe", lambda hf=hf, ps=ps: PE.matmul(ps[0:2, :], gsum[:, :], Sc[:, hf * 512:(hf + 1) * 512], start=True, stop=True), reads=[gsum, Sc], writes=[ps], pe_acc=True)
                k.op("dve", lambda hf=hf, ps=ps: V.tensor_copy(impP[:, 1 + hf * 512:1 + (hf + 1) * 512], ps[0:2, :]), reads=[ps], writes=[impP])
            ps = k.nextps()
            for i in range(8):
                k.op("pe", lambda i=i: PE.transpose(ps[:, i * 8:(i + 1) * 8], Sc[:, i * 128:(i + 1) * 128], ident[0:8, 0:8]), reads=[Sc, ident], writes=[ps], pe_acc=True)
            k.op("dve", lambda: V.tensor_copy(PcT[:], ps[:, 0:64].rearrange("p (a b) -> p a b", b=8)), reads=[ps], writes=[PcT])
            ps = k.nextps()
            for i in range(8):
                k.op("pe", lambda i=i: PE.matmul(ps[0:8, 0:128], PcT[:, i, :], CVs[:, i, :], start=(i == 0), stop=(i == 7)), reads=[PcT, CVs], writes=[ps], pe_acc=True)
            k.op("dve", lambda: V.tensor_copy(ot[:], ps[0:8, 0:128]), reads=[ps], writes=[ot])
            sel_half(o8[:, 0, :], ot, [ot])
            k.op("dve", lambda: V.tensor_reduce(sbk[:, 0:257], impP[:, 0:1028].rearrange("p (j m) -> p j m", m=4), axis=AX.X, op=ALU.add), reads=[impP], writes=[sbk])
            k.op("dve", lambda: V.memset(sbk[:, 257:260], 0.0), writes=[sbk])
            k.op("dve", lambda: V.tensor_tensor(sbk[:, 0:257], sbk[:, 0:257], impP[:, 4:1032:4], op=ALU.add), reads=[sbk, impP], writes=[sbk])
            k.op("dve", lambda: V.tensor_tensor(sbk[:], sbk[:], selcs[:, 0, :], op=ALU.mult), reads=[sbk, selcs], writes=[sbk])
            k.op("dve", lambda: V.tensor_tensor(sbk[:], sbk[:], selcs[:, 1, :], op=ALU.add), reads=[sbk, selcs], writes=[sbk])
            k.op("dve", lambda: V.max(m8s[:], sbk[:]), reads=[sbk], writes=[m8s])
            k.op("dve", lambda: V.match_replace(sbk2[:], m8s[:], sbk[:], NEG), reads=[m8s, sbk], writes=[sbk2])
            k.op("dve", lambda: V.max(m8s[:], sbk2[:]), reads=[sbk2], writes=[m8s])
            k.op("dve", lambda: V.tensor_scalar(sbk2[:], sbk[:], m8s[:, 7:8], None, op0=ALU.is_ge), reads=[sbk, m8s], writes=[sbk2])
            ps = k.nextps()
            k.op("pe", lambda: PE.matmul(ps[0:8, 0:258], gexp[:, :], sbk2[:, 0:258], start=True, stop=True), reads=[gexp, sbk2], writes=[ps], pe_acc=True)
            k.op("dve", lambda: V.tensor_copy(sel8[:], ps[0:8, 0:258]), reads=[ps], writes=[sel8])
            k.dma("pool", selm_d[j], sel8[:], reads=[sel8], writes=[selm_d])
            for kk2 in range(2):
                srcap = bass.AP(selm_d.t.tensor, j * 8 * 258 + kk2, [[1, 1], [2, 129], [258, 8]])
                k.dma("pool", B2[kk2:kk2 + 1, :, :], srcap, reads=[selm_d], writes=[B2])
            B2f = B2[:].rearrange("k n h -> k (n h)")
            for c0 in range(0, 1032, 512):
                cn = min(512, 1032 - c0)
                ps = k.nextps()
                k.op("pe", lambda c0=c0, cn=cn, ps=ps: PE.matmul(ps[:, 0:cn], half2[:, :], B2f[:, c0:c0 + cn], start=True, stop=True), reads=[half2, B2], writes=[ps], pe_acc=True)
                k.op("dve", lambda c0=c0, cn=cn, ps=ps: V.tensor_scalar(nmT[:].rearrange("p n h -> p (n h)")[:, c0:c0 + cn], ps[:, 0:cn], 1.0, 1.0e30, op0=ALU.subtract, op1=ALU.mult),
                     reads=[ps], writes=[nmT])
          with k.scope():
            V_all = k.sb("s_Vall", [128, 129, 128])
            hold["V_all"] = V_all
            for pg4 in range(0, 128, 4):
                psk = k.nextps()
                for i in range(4):
                    pg = pg4 + i
                    gtile = gt_[pg % 3]
                    k.dma("pool", None, None, reads=[pool_sel, idxK], writes=[gtile],
                          fn=lambda pg=pg, gtile=gtile: G.indirect_dma_start(out=gtile[:, 0:128], out_offset=None, in_=pool_sel[:, :],
                                                                             in_offset=bass.IndirectOffsetOnAxis(ap=idxK[:, j * 128 + pg:j * 128 + pg + 1], axis=0)))
                    k.dma("pool", None, None, reads=[pool_sel, idxV], writes=[V_all],
                          fn=lambda pg=pg: G.indirect_dma_start(out=V_all[:, pg, :], out_offset=None, in_=pool_sel[:, :],
                                                                in_offset=bass.IndirectOffsetOnAxis(ap=idxV[:, j * 128 + pg:j * 128 + pg + 1], axis=0)))
                    k.op("pe", lambda i=i, gtile=gtile: PE.transpose(psk[:, i * 128:(i + 1) * 128], gtile[:, 0:128], ident[:]), reads=[gtile, ident], writes=[psk], pe_acc=True)
                k.op("act", lambda psk=psk: A.copy(KTu[:], psk[:, :]), reads=[psk], writes=[KTu])
                score_pages(j, [(KTu[:, i * 128:(i + 1) * 128], KTu) for i in range(4)], pg4,
                            lambda pg: ((ohs[:, 0, :], ohs) if pg <= 120 else (ohs[:, pg - 120, :], ohs)))
            k.op("dve", lambda: V.tensor_copy(knew[:, 0:1], zsT[:, 10, j:j + 1]), reads=[zsT], writes=[knew])
            score_pages(j, [(knew[:, :], knew)], 128, lambda pg: (ohs[:, 8, :], ohs))
            k.op("dve", lambda: V.memset(V_all[:, 128, :], 0.0), writes=[V_all])
            k.dma("sp", V_all[0:1, 128, :], o_sel_s[j:j + 1, 128:256], reads=[o_sel_s], writes=[V_all])
            k.op("dve", lambda: V.tensor_tensor(ST[:], ST[:], nmT[:], op=ALU.add), reads=[ST, nmT], writes=[ST])
            attend_T(129, 1)
            k.dma("sp", V_all[:, 0:4, :], state_win[j].rearrange("(n p) c -> p n c", p=128)[:, :, 128:256], reads=[state_win], writes=[V_all])
            wk = k.sb("s_wk%d" % j, [128, 4, 128])
            k.dma("act", wk[:], state_win[j].rearrange("(n p) c -> p n c", p=128)[:, :, 0:128], reads=[state_win], writes=[wk])
            psk = k.nextps()
            for i in range(4):
                k.op("pe", lambda i=i: PE.transpose(psk[:, i * 128:(i + 1) * 128], wk[:, i, :], ident[:]), reads=[wk, ident], writes=[psk], pe_acc=True)
            k.op("act", lambda: A.copy(KTu[:], psk[:, :]), reads=[psk], writes=[KTu])
            score_pages(j, [(KTu[:, i * 128:(i + 1) * 128], KTu) for i in range(4)], 0, lambda pg: (ohw[:, pg, :], ohw))
            k.op("dve", lambda: V.tensor_copy(knew[:, 0:1], zsT[:, 12, j:j + 1]), reads=[zsT], writes=[knew])
            score_pages(j, [(knew[:, :], knew)], 4, lambda pg: (ohw[:, 4, :], ohw))
            k.op("dve", lambda: V.memset(V_all[:, 4, :], 0.0), writes=[V_all])
            k.dma("sp", V_all[0:1, 4, :], o_win_s[j:j + 1, 511, 128:256], reads=[o_win_s], writes=[V_all])
            attend_T(5, 2)
            k.op("dve", lambda: V.tensor_scalar(attj[:], o8[:, 0, :], g8[:, j, 0:1], None, op0=ALU.mult), reads=[o8, g8], writes=[attj])
            k.op("dve", lambda: V.scalar_tensor_tensor(attj[:], o8[:, 1, :], g8[:, j, 1:2], attj[:], op0=ALU.mult, op1=ALU.add), reads=[o8, g8, attj], writes=[attj])
            k.op("dve", lambda: V.scalar_tensor_tensor(attj[:], o8[:, 2, :], g8[:, j, 2:3], attj[:], op0=ALU.mult, op1=ALU.add), reads=[o8, g8, attj], writes=[attj])
            k.dma("pool", atts_d[j].rearrange("(h f) -> h f", f=64), attj[:], reads=[attj], writes=[atts_d])
        k.dma("sp", att_s[:], atts_d[:, :], reads=[atts_d], writes=[att_s])

    DN_ALPHA = float(2.0 ** 0.25)
    NBLK = NOWN + 1
    NTOK = NBLK * 128
    if stage >= 5:
      xm2T_d = k.dram("xm2T_d", [8, 128, NTOK], BF16)
      moe_d = k.dram("moe_d", [NTOK, D])
      with k.scope():
        Gall = k.sb("Gall", [128, NBLK, 32])
        st = k.sb("st5", [128, 2, 6]); mv = k.sb("mv5", [128, 2]); rstd = k.sb("rstd5", [128, 1])

        def build_rowv(names):
            srcs = {"g1": (mT, 16), "g2": (mT, 40), "sc2": (sc2p, 0), "sh2": (mT, 24)}
            rowv = {}
            bcol = k.sb("bcol", [128, 128])
            for nm in names:
                srcb, t0 = srcs[nm]
                rowv[nm + "p"] = k.sb("rowv_" + nm + "p", [128, D]); rowv[nm + "s"] = k.sb("rowv_" + nm + "s", [NS, D])
                for half in range(2):
                    psp = k.nextps(); pss = k.nextps()
                    for q in range(4):
                        tq = half * 4 + q
                        k.op("dve", lambda tq=tq, srcb=srcb, t0=t0: V.tensor_copy(bcol[:], srcb[:, t0 + tq, 0:1].to_broadcast([128, 128])), reads=[srcb], writes=[bcol])
                        k.op("pe", lambda q=q, psp=psp: PE.matmul(psp[:, q * 128:(q + 1) * 128], bcol[:], ident[:], start=True, stop=True),
                             reads=[bcol, ident], writes=[psp], pe_acc=True)
                        k.op("pe", lambda q=q, pss=pss, tq=tq, srcb=srcb, t0=t0: PE.matmul(pss[0:NS, q * 128:(q + 1) * 128], srcb[:, t0 + tq, 1:5], ident[:], start=True, stop=True),
                             reads=[srcb, ident], writes=[pss], pe_acc=True)
                    k.op("act", lambda nm=nm, half=half, psp=psp: A.copy(rowv[nm + "p"][:, half * 512:(half + 1) * 512], psp[:, :]), reads=[psp], writes=[rowv[nm + "p"]])
                    k.op("dve", lambda nm=nm, half=half, pss=pss: V.tensor_copy(rowv[nm + "s"][0:NS, half * 512:(half + 1) * 512], pss[0:NS, :]), reads=[pss], writes=[rowv[nm + "s"]])
            return rowv

        with k.scope():
            rowv = build_rowv(["g1", "sc2", "sh2"])
            lnr = k.sb("lnr", [128, 2, D])
            for i in range(2):
                k.dma("sp", lnr[:, i, :], lnrows_d[i:i + 1, :].partition_broadcast(128), reads=[lnrows_d], writes=[lnr])
            w_router = k.sb("w_router", [128, 8, 32])
            k.dma("sp", w_router[:], w_router_d.t.rearrange("(k p) c -> p k c", p=128), reads=[w_router_d], writes=[w_router])
            b_router = k.sb("b_router", [128, 32])
            k.dma("sp", b_router[:], b_router_d[0:1, :].partition_broadcast(128), reads=[b_router_d], writes=[b_router])
            w_out_sb = k.sb("w_out_sb", [128, 8, D])
            g = {}
            for kk in range(8):
                k.dma("sp" if kk % 2 == 0 else "act", w_out_sb[:, kk, :], w_out_d.t.rearrange("(k p) c -> p k c", p=128)[:, kk, :], reads=[w_out_d], writes=[w_out_sb], grp=g)
            cat = [k.sb("cat%d" % i, [128, D]) for i in range(2)]
            catT = k.sb("catT", [128, 8, 128])
            xtb = [k.sb("xt5_%d" % i, [128, D]) for i in range(2)]
            zb = k.sb("zb", [128, D]); x1b = k.sb("x1b", [128, D]); xnb = k.sb("xnb5", [128, D]); xm2 = k.sb("xm2", [128, D])
            xm2T = [k.sb("xm2T%d" % i, [128, 8, 128]) for i in range(2)]
            xm2Tb = [k.sb("xm2Tb%d" % i, [128, 8, 128], BF16) for i in range(2)]
            lg = k.sb("lg", [128, 32]); m8r = k.sb("m8r", [128, 8]); ew = k.sb("ew", [128, 8]); wk4 = k.sb("wk4", [128, 4])
            ohk = k.sb("ohk", [128, 4, 32])
            xm2T_v = xm2T_d.t.rearrange("k p t -> p k t")

            for blk in range(NBLK):
                samp = (blk == NOWN)
                P = NS if samp else 128
                sfx = "s" if samp else "p"
                cb, xb, xmT = cat[blk % 2], xtb[blk % 2], xm2T[blk % 2]
                if samp:
                    k.op("dve", lambda: V.tensor_copy(cb[0:P, 0:512], ssm_out_s[0:P, :]), reads=[ssm_out_s], writes=[cb])
                    k.op("dve", lambda: V.tensor_copy(cb[0:P, 512:1024], att_s[0:P, :]), reads=[att_s], writes=[cb])
                    k.dma("act", xb[0:P, :], xs_rows[0:P, :], reads=[xs_rows], writes=[xb])
                else:
                    k.dma("sp", cb[:, 0:512], ssm_out_d[blk * 128:(blk + 1) * 128, :], reads=[ssm_out_d], writes=[cb])
                    k.dma("sp", cb[:, 512:1024], att_d[blk * 128:(blk + 1) * 128, :], reads=[att_d], writes=[cb])
                    k.dma("act", xb[:, :], xo[blk * 128:(blk + 1) * 128, :], reads=[xo], writes=[xb])
                for hlf in range(2):
                    ps = k.nextps()
                    for q in range(4):
                        kk = hlf * 4 + q
                        k.op("pe", lambda kk=kk, q=q, ps=ps: PE.transpose(ps[:, q * P:(q + 1) * P], cb[0:P, kk * 128:(kk + 1) * 128], ident[0:P, 0:P]),
                             reads=[cb, ident], writes=[ps], pe_acc=True)
                    k.op("act" if hlf else "dve",
                         (lambda ps=ps, hlf=hlf: A.copy(catT[:, hlf * 4:(hlf + 1) * 4, 0:P], ps[:, 0:4 * P].rearrange("p (a b) -> p a b", b=P))) if hlf else
                         (lambda ps=ps, hlf=hlf: V.tensor_copy(catT[:, hlf * 4:(hlf + 1) * 4, 0:P], ps[:, 0:4 * P].rearrange("p (a b) -> p a b", b=P))),
                         reads=[ps], writes=[catT])
                for hlf in range(2):
                    ps = k.nextps()
                    for kk in range(8):
                        k.op("pe", lambda kk=kk, ps=ps, hlf=hlf: PE.matmul(ps[0:P, :], catT[:, kk, 0:P], w_out_sb[:, kk, hlf * 512:(hlf + 1) * 512], start=(kk == 0), stop=(kk == 7)),
                             reads=[catT, w_out_sb], writes=[ps], pe_acc=True)
                    cs = slice(hlf * 512, (hlf + 1) * 512)
                    k.op("dve", lambda ps=ps, cs=cs: V.tensor_tensor(zb[0:P, cs], ps[0:P, :], rowv["g1" + sfx][0:P, cs], op=ALU.mult), reads=[ps, rowv["g1" + sfx]], writes=[zb])
                k.op("dve", lambda: V.scalar_tensor_tensor(zb[0:P, :], xb[0:P, :], DN_ALPHA, zb[0:P, :], op0=ALU.mult, op1=ALU.add), reads=[xb, zb], writes=[zb])
                ln_rows(zb, P, st, mv, rstd, x1b)
                k.op("dve", lambda: V.tensor_tensor(x1b[0:P, :], x1b[0:P, :], lnr[0:P, 0, :], op=ALU.mult), reads=[x1b, lnr], writes=[x1b])
                k.op("dve", lambda: V.tensor_tensor(x1b[0:P, :], x1b[0:P, :], lnr[0:P, 1, :], op=ALU.add), reads=[x1b, lnr], writes=[x1b])
                k.dma("pool", x1_d[blk * 128:blk * 128 + P, :], x1b[0:P, :], reads=[x1b], writes=[x1_d])
                ln_rows(x1b, P, st, mv, rstd, xnb)
                k.op("dve", lambda: V.tensor_tensor(xm2[0:P, :], xnb[0:P, :], rowv["sc2" + sfx][0:P, :], op=ALU.mult), reads=[xnb, rowv["sc2" + sfx]], writes=[xm2])
                k.op("dve", lambda: V.tensor_tensor(xm2[0:P, :], xm2[0:P, :], rowv["sh2" + sfx][0:P, :], op=ALU.add), reads=[xm2, rowv["sh2" + sfx]], writes=[xm2])
                for hlf in range(2):
                    ps = k.nextps()
                    for q in range(4):
                        kk = hlf * 4 + q
                        k.op("pe", lambda kk=kk, q=q, ps=ps: PE.transpose(ps[:, q * P:(q + 1) * P], xm2[0:P, kk * 128:(kk + 1) * 128], ident[0:P, 0:P]),
                             reads=[xm2, ident], writes=[ps], pe_acc=True)
                    k.op("act", lambda ps=ps, hlf=hlf: A.copy(xmT[:, hlf * 4:(hlf + 1) * 4, 0:P], ps[:, 0:4 * P].rearrange("p (a b) -> p a b", b=P)), reads=[ps], writes=[xmT])
                xmTb = xm2Tb[blk % 2]
                k.op("act", lambda: A.copy(xmTb[:, :, 0:P], xmT[:, :, 0:P]), reads=[xmT], writes=[xmTb])
                k.dma("pool", xm2T_v[:, :, blk * 128:blk * 128 + P], xmTb[:, :, 0:P], reads=[xmTb], writes=[xm2T_d])
                ps = k.nextps()
                for kk in range(8):
                    k.op("pe", lambda kk=kk, ps=ps: PE.matmul(ps[0:P, 0:32], xmT[:, kk, 0:P], w_router[:, kk, :], start=(kk == 0), stop=(kk == 7)),
                         reads=[xmT, w_router], writes=[ps], pe_acc=True)
                k.op("dve", lambda ps=ps: V.tensor_tensor(lg[0:P, :], ps[0:P, 0:32], b_router[0:P, :], op=ALU.add), reads=[ps, b_router], writes=[lg])
                k.op("dve", lambda: V.max(m8r[0:P, :], lg[0:P, :]), reads=[lg], writes=[m8r])
                k.op("dve", lambda: V.tensor_scalar(ew[0:P, 4:5], m8r[0:P, 0:1], -1.0, None, op0=ALU.mult), reads=[m8r], writes=[ew])
                k.op("dve", lambda: V.memset(ew[0:P, 5:6], 0.0), writes=[ew])
                k.op("act", lambda: A.activation(ew[0:P, 0:4], m8r[0:P, 0:4], AF.Exp, bias=ew[0:P, 4:5], scale=1.0, accum_out=ew[0:P, 5:6]), reads=[m8r, ew], writes=[ew])
                k.op("dve", lambda: V.reciprocal(ew[0:P, 6:7], ew[0:P, 5:6]), reads=[ew], writes=[ew])
                k.op("dve", lambda: V.tensor_scalar(wk4[0:P, :], ew[0:P, 0:4], ew[0:P, 6:7], None, op0=ALU.mult), reads=[ew], writes=[wk4])
                for kq in range(4):
                    k.op("dve", lambda kq=kq: V.tensor_scalar(ohk[0:P, kq, :], lg[0:P, :], m8r[0:P, kq:kq + 1], None, op0=ALU.is_equal), reads=[lg, m8r], writes=[ohk])
                k.op("dve", lambda: V.tensor_scalar(Gall[0:P, blk, :], ohk[0:P, 0, :], wk4[0:P, 0:1], None, op0=ALU.mult), reads=[ohk, wk4], writes=[Gall])
                for kq in range(1, 4):
                    k.op("dve", lambda kq=kq: V.scalar_tensor_tensor(Gall[0:P, blk, :], ohk[0:P, kq, :], wk4[0:P, kq:kq + 1], Gall[0:P, blk, :], op0=ALU.mult, op1=ALU.add),
                         reads=[ohk, wk4, Gall], writes=[Gall])

        with k.scope():
            b_guT = k.sb("b_guT", [128, 32, 16])
            k.dma("sp", b_guT[:], b_guT_d[:], reads=[b_guT_d], writes=[b_guT])
            onesr5 = k.sb("onesr5", [1, 128])
            k.op("dve", lambda: V.memset(onesr5[:], 1.0), writes=[onesr5])
            HT = 1028
            xmh = k.sb("xmh", [128, 8, HT], BF16); yacc = k.sb("yacc", [128, 9, D]); hhT = k.sb("hhT", [128, 8, HT], BF16)
            wgs = [k.sb("wgs%d" % i, [128, 8, 256]) for i in range(3)]
            wgu = [k.sb("wgu%d" % i, [128, 8, 256], BF16) for i in range(3)]
            wds = [k.sb("wds%d" % i, [128, D]) for i in range(2)]
            wdn2 = [k.sb("wdn%d" % i, [128, 8, D], BF16) for i in range(2)]; bdn = [k.sb("bdn%d" % i, [1, D]) for i in range(2)]
            gt = [k.sb("gt%d" % i, [128, 512]) for i in range(2)]; ut_ = [k.sb("ut%d" % i, [128, 512]) for i in range(2)]
            sg = [k.sb("sg%d" % i, [128, 512]) for i in range(2)]
            pc = 0; ac = 0
            for hf_ in range(2):
                blk0 = 8 * hf_
                ntok = 1024 if hf_ == 0 else 1028
                chunks = [(0, 512), (512, 512)] + ([(1024, 4)] if hf_ == 1 else [])
                tiles = [(i * 128, 128) for i in range(8)] + ([(1024, 4)] if hf_ == 1 else [])
                k.dma("sp", xmh[:, :, 0:ntok], xm2T_v[:, :, blk0 * 128:blk0 * 128 + ntok], reads=[xm2T_d], writes=[xmh])
                k.op("dve", lambda: V.memset(yacc[:], 0.0), writes=[yacc])
                for e in range(32):
                    bd = bdn[e % 2]
                    wdn = wdn2[e % 2]
                    g = {}
                    for f in range(8):
                        ws_ = wds[f % 2]
                        k.dma("act", ws_[:], w_dn_d[e, f * 128:(f + 1) * 128, :], reads=[w_dn_d], writes=[ws_])
                        k.op("act", lambda f=f, ws_=ws_: A.copy(wdn[:, f, :], ws_[:]), reads=[ws_], writes=[wdn])
                    k.dma("act", bd[:], b_dn_d[e:e + 1, :], reads=[b_dn_d], writes=[bd])
                    wv = w_gu_d[e].rearrange("(k p) c -> p k c", p=128)
                    for f in range(8):
                        wb = wgu[pc % 3]; wst = wgs[pc % 3]; pc += 1
                        g = {}
                        k.dma("sp", wst[:, :, 0:128], wv[:, :, f * 128:(f + 1) * 128], reads=[w_gu_d], writes=[wst], grp=g)
                        k.dma("sp", wst[:, :, 128:256], wv[:, :, 1024 + f * 128:1024 + (f + 1) * 128], reads=[w_gu_d], writes=[wst], grp=g)
                        k.op("act", lambda wb=wb, wst=wst: A.copy(wb[:].rearrange("p a b -> p (a b)"), wst[:].rearrange("p a b -> p (a b)")), reads=[wst], writes=[wb])
                        for (c0, cn) in chunks:
                            gt_, u_, s_ = gt[ac % 2], ut_[ac % 2], sg[ac % 2]; ac += 1
                            psg = k.nextps(); psu = k.nextps()
                            for kk in range(8):
                                k.op("pe", lambda kk=kk, psg=psg, wb=wb, c0=c0, cn=cn: PE.matmul(psg[:, 0:cn], wb[:, kk, 0:128], xmh[:, kk, c0:c0 + cn], start=(kk == 0), stop=(kk == 7)),
                                     reads=[wb, xmh], writes=[psg], pe_acc=True)
                            for kk in range(8):
                                k.op("pe", lambda kk=kk, psu=psu, wb=wb, c0=c0, cn=cn: PE.matmul(psu[:, 0:cn], wb[:, kk, 128:256], xmh[:, kk, c0:c0 + cn], start=(kk == 0), stop=(kk == 7)),
                                     reads=[wb, xmh], writes=[psu], pe_acc=True)
                            k.op("dve", lambda psg=psg, f=f, cn=cn, gt_=gt_: V.tensor_scalar(gt_[:, 0:cn], psg[:, 0:cn], b_guT[:, e, f:f + 1], 7.0, op0=ALU.add, op1=ALU.min), reads=[psg, b_guT], writes=[gt_])
                            k.op("dve", lambda psu=psu, f=f, cn=cn, u_=u_: V.tensor_scalar(u_[:, 0:cn], psu[:, 0:cn], b_guT[:, e, 8 + f:9 + f], 7.0, op0=ALU.add, op1=ALU.min), reads=[psu, b_guT], writes=[u_])
                            k.op("dve", lambda cn=cn, u_=u_: V.tensor_scalar(u_[:, 0:cn], u_[:, 0:cn], -7.0, 1.0, op0=ALU.max, op1=ALU.add), reads=[u_], writes=[u_])
                            k.op("act", lambda cn=cn, gt_=gt_, s_=s_: A.activation(s_[:, 0:cn], gt_[:, 0:cn], AF.Silu, scale=1.702), reads=[gt_], writes=[s_])
                            k.op("dve", lambda f=f, c0=c0, cn=cn, u_=u_, s_=s_: V.scalar_tensor_tensor(hhT[:, f, c0:c0 + cn], s_[:, 0:cn], 1.0 / 1.702, u_[:, 0:cn], op0=ALU.mult, op1=ALU.mult),
                                 reads=[s_, u_], writes=[hhT])
                    for ti, (t0, tn) in enumerate(tiles):
                        for hlf in range(2):
                            ps = k.nextps()
                            for f in range(8):
                                k.op("pe", lambda f=f, ps=ps, t0=t0, tn=tn, hlf=hlf: PE.matmul(ps[0:tn, :], hhT[:, f, t0:t0 + tn], wdn[:, f, hlf * 512:(hlf + 1) * 512], start=(f == 0), stop=False),
                                     reads=[hhT, wdn], writes=[ps], pe_acc=True)
                            k.op("pe", lambda ps=ps, hlf=hlf, tn=tn: PE.matmul(ps[0:tn, :], onesr5[0:1, 0:tn], bd[0:1, hlf * 512:(hlf + 1) * 512], start=False, stop=True),
                                 reads=[onesr5, bd], writes=[ps], pe_acc=True)
                            k.op("dve", lambda ps=ps, hlf=hlf, tn=tn, ti=ti: V.scalar_tensor_tensor(
                                yacc[0:tn, ti, hlf * 512:(hlf + 1) * 512], ps[0:tn, :], Gall[0:tn, blk0 + ti, e:e + 1], yacc[0:tn, ti, hlf * 512:(hlf + 1) * 512], op0=ALU.mult, op1=ALU.add),
                                 reads=[ps, Gall, yacc], writes=[yacc])
                for ti, (t0, tn) in enumerate(tiles):
                    r0 = (blk0 + ti) * 128
                    k.dma("pool", moe_d[r0:r0 + tn, :], yacc[0:tn, ti, :], reads=[yacc], writes=[moe_d])

        with k.scope():
            rowv = build_rowv(["g2"])
            lnr = k.sb("lnr2", [128, 2, D])
            for i in range(2):
                k.dma("sp", lnr[:, i, :], lnrows_d[2 + i:3 + i, :].partition_broadcast(128), reads=[lnrows_d], writes=[lnr])
            accs = [k.sb("acc%d" % i, [128, D]) for i in range(2)]; x1rs = [k.sb("x1r%d" % i, [128, D]) for i in range(2)]; yo = k.sb("yo", [128, D])
            for blk in range(NBLK):
                samp = (blk == NOWN)
                P = NS if samp else 128
                sfx = "s" if samp else "p"
                acc, x1r = accs[blk % 2], x1rs[blk % 2]
                k.dma("sp", acc[0:P, :], moe_d[blk * 128:blk * 128 + P, :], reads=[moe_d], writes=[acc])
                k.dma("act", x1r[0:P, :], x1_d[blk * 128:blk * 128 + P, :], reads=[x1_d], writes=[x1r])
                k.op("dve", lambda: V.tensor_tensor(acc[0:P, :], acc[0:P, :], rowv["g2" + sfx][0:P, :], op=ALU.mult), reads=[acc, rowv["g2" + sfx]], writes=[acc])
                k.op("dve", lambda: V.scalar_tensor_tensor(acc[0:P, :], x1r[0:P, :], DN_ALPHA, acc[0:P, :], op0=ALU.mult, op1=ALU.add), reads=[x1r, acc], writes=[acc])
                ln_rows(acc, P, st, mv, rstd, yo)
                k.op("dve", lambda: V.tensor_tensor(yo[0:P, :], yo[0:P, :], lnr[0:P, 0, :], op=ALU.mult), reads=[yo, lnr], writes=[yo])
                k.op("dve", lambda: V.tensor_tensor(yo[0:P, :], yo[0:P, :], lnr[0:P, 1, :], op=ALU.add), reads=[yo, lnr], writes=[yo])
                if samp:
                    k.dma("pool", o_y_s[:, :], yo[0:P, :], reads=[yo], writes=[o_y_s])
                else:
                    k.dma("pool", o_y_p[blk * 128:(blk + 1) * 128, :], yo[:, :], reads=[yo], writes=[o_y_p])

    if dbg:
        for nm, src, shp in (("dbg_att", att_d, [NOWN * 128, 512]), ("dbg_ssm", ssm_out_d, [NOWN * 128, 512]), ("dbg_x1", x1_d, [NOWN * 128 + 128, D]), ("dbg_att_s", atts_d, [NS, 512]), ("dbg_moe", moe_d, [NTOK, D])):
            o = dout(nm, shp)
            k.dma("sp", o[:, :], src[:, :], reads=[src], writes=[o])
    k.finish()
    return nc, k


def _q_perm():
    perm = []
    for t in range(4):
        perm += list(range(512 + 64 * t, 512 + 64 * t + 64))
        perm += list(range(512 + 64 * (4 + t), 512 + 64 * (4 + t) + 64))
    return np.array(perm)


_CACHE = {}


def _bucket_table(nmax):
    try:
        import jax, jax.numpy as jnp
        with jax.default_device(jax.devices("cpu")[0]):
            n = jnp.arange(nmax + 1)
            nf = jnp.maximum(n, 1).astype(jnp.float32)
            lg = 16 + (jnp.log(nf / 16) / math.log(1024 / 16) * 16).astype(jnp.int32)
            out = np.asarray(jnp.where(n < 16, n, jnp.minimum(lg, 31)))
        return out.astype(np.int64)
    except Exception:
        n = np.arange(nmax + 1)
        nf = np.maximum(n, 1).astype(np.float32)
        lg = 16 + (np.log(nf / np.float32(16)) / np.float32(math.log(64.0)) * np.float32(16)).astype(np.int32)
        return np.where(n < 16, n, np.minimum(lg, 31)).astype(np.int64)


def _sample_consts():
    f32 = np.float32
    bt = _bucket_table(20000)
    def onehot(dists):
        dists = np.asarray(dists)
        o = np.zeros((33,) + dists.shape, f32)
        b = np.where(dists < 0, 32, bt[np.clip(dists, 0, 20000)])
        np.put_along_axis(o, b[None], 1.0, axis=0)
        return o
    n = np.arange(1024)
    ohc = onehot(np.where(n <= 1022, 16384 - (16 * n + 31), -1))
    tok = np.arange(128)
    ohs = np.zeros((33, 9, 128), f32)
    ohs[:, 0, :] = onehot(np.full(128, 5000))
    for i in range(1, 8):
        ohs[:, i, :] = onehot(16384 - ((120 + i) * 128 + tok))
    ohs[:, 8, :] = onehot(np.where(tok == 0, 0, -1))
    ohw = np.zeros((33, 5, 128), f32)
    for i in range(4):
        ohw[:, i, :] = onehot(512 - (i * 128 + tok))
    ohw[:, 4, :] = onehot(np.where(tok == 0, 0, -1))
    gsum = np.zeros((8, 2), f32); gsum[0:4, 0] = 1; gsum[4:8, 1] = 1
    selcs = np.zeros((2, 2, 260), f32)
    selcs[:, 0, :257] = 1.0
    selcs[:, 1, 257:] = -1.0
    for fb in (0, 255, 256):
        selcs[:, 0, fb] = 0.0; selcs[:, 1, fb] = 1.0e4
    half2 = np.zeros((2, 128), f32); half2[0, :64] = 1; half2[1, 64:] = 1
    m01 = np.zeros((8, 2), f32); m01[0:4, 0] = 1; m01[4:8, 1] = 1
    return dict(pidx=np.arange(128, dtype=f32).reshape(128, 1), ohc=np.ascontiguousarray(ohc), ohs=ohs, ohw=ohw, gsum=gsum,
                gexp=np.ascontiguousarray(gsum.T), selcs=selcs, half2=half2, m01=m01)


def _nsa_consts(par):
    f32 = np.float32
    YG, Y0 = 1408, 1151
    bt = _bucket_table(4096)
    y = np.arange(YG)
    d = Y0 - y + 128 * par
    oh = np.zeros((2, 33, YG), f32)
    for w in range(2):
        bad = (d < 0) | ((d > 512) if w == 1 else False)
        b = np.where(bad, 32, bt[np.clip(d, 0, 4096)])
        oh[w, b, y] = 1.0
    selc = np.zeros((3, NOWN, 128, 64), f32)
    for n in range(NOWN):
        qpos = 128 * (2 * n + par) + np.arange(128)
        cur = (qpos // 64)[:, None]
        blk = np.arange(64)[None, :]
        cm = (blk <= cur)
        fm = ((blk == 0) | (blk == cur) | (blk == cur - 1)) & cm
        selc[0, n] = (cm & ~fm)
        selc[1, n] = 1.0e4 * fm - ((~fm) & (~cm))
        selc[2, n] = cm
    return np.ascontiguousarray(oh), np.ascontiguousarray(selc)


def kernel(**inp):
    f32 = np.float32
    A_ = lambda a: np.ascontiguousarray(np.asarray(a), dtype=None)
    x_prompt = np.asarray(inp["x_prompt"], f32)
    x_sample = np.asarray(inp["x_sample"], f32)
    w_in_full = np.asarray(inp["w_in"], f32)[0]
    cols = np.arange(D_IN)
    cols[512:1024] = _q_perm()
    w_in_p = np.ascontiguousarray(w_in_full[:, cols])
    w_ada = np.ascontiguousarray(np.asarray(inp["w_ada"], f32)[0])
    b_ada = np.asarray(inp["b_ada"], f32)[0]
    b_adaT = np.ascontiguousarray(b_ada.reshape(48, 128).T)
    b_ada_row = np.ascontiguousarray(b_ada.reshape(1, -1))
    c_prompt = np.asarray(inp["c_prompt"], f32)
    c_sample = np.asarray(inp["c_sample"], f32)
    state_win = np.asarray(inp["state_win_kv"], f32)[0].reshape(32, 512, 256)
    ident = np.eye(128, dtype=f32)
    lam_re = np.asarray(inp["lam_re"], f32)[0]; lam_im = np.asarray(inp["lam_im"], f32)[0]
    log_dt = np.asarray(inp["log_dt"], f32)[0]
    ldt_full = np.repeat(log_dt[:, None], 64, 1)
    def col_layout(a):
        return np.ascontiguousarray(a.reshape(16, 2, 64).transpose(1, 2, 0).reshape(128, 16))
    lamc = np.ascontiguousarray(np.stack([col_layout(lam_re), col_layout(lam_im), col_layout(ldt_full)], 1))
    lamr = np.ascontiguousarray(np.stack([lam_re.reshape(-1), lam_im.reshape(-1), ldt_full.reshape(-1)], 0))
    btp = np.zeros((2, 128, 16, 128), f32)
    ctp = np.zeros((2, 128, 16, 32), f32)
    for i, (bsrc, csrc) in enumerate(((inp["b_re"], inp["c_re"]), (inp["b_im"], inp["c_im"]))):
        bsrc = np.asarray(bsrc, f32)[0]; csrc = np.asarray(csrc, f32)[0]
        for g_ in range(32):
            s_, gl = g_ // 2, g_ % 2
            ut = s_ // 4
            r0 = (g_ - 8 * ut) * 16
            btp[i, r0:r0 + 16, s_, gl * 64:(gl + 1) * 64] = bsrc[g_].T
            ctp[i, gl * 64:(gl + 1) * 64, s_, gl * 16:(gl + 1) * 16] = csrc[g_].T
    dskc = np.ascontiguousarray(np.asarray(inp["d_skip"], f32)[0].reshape(4, 128).T)
    w_glu = np.ascontiguousarray(np.asarray(inp["w_glu"], f32)[0])
    b_glu = np.ascontiguousarray(np.asarray(inp["b_glu"], f32)[0].reshape(1, 512))
    st_re = np.asarray(inp["state_ssm_re"], f32)[0]; st_im = np.asarray(inp["state_ssm_im"], f32)[0]
    def h0_layout(a4):
        return a4.reshape(4, 16, 2, 64).transpose(2, 3, 1, 0).reshape(128, 16, 4)

    rel_bias = np.ascontiguousarray(np.asarray(inp["rel_bias"], f32))
    jflip = np.ascontiguousarray(np.eye(128, dtype=f32)[::-1])
    w1 = np.asarray(inp["phi_w1"], f32)[0]
    w2 = np.asarray(inp["phi_w2"], f32)[0]
    ppe = np.asarray(inp["phi_pe"], f32)[0]
    pb1v = np.asarray(inp["phi_b1"], f32)[0]; pb2v = np.asarray(inp["phi_b2"], f32)[0]
    bd1 = np.zeros((128, 64, 128), f32); bd2 = np.zeros((128, 2, 128), f32)
    for h_ in range(2):
        bd1[h_ * 64:(h_ + 1) * 64, :, h_ * 64:(h_ + 1) * 64] = w1.reshape(64, 64, 64).transpose(1, 0, 2)
        bd2[h_ * 64:(h_ + 1) * 64, :, h_ * 64:(h_ + 1) * 64] = w2.transpose(1, 0, 2)
    pel = np.ascontiguousarray(np.tile(ppe.reshape(2, 2, 16, 64).transpose(3, 0, 1, 2), (2, 1, 1, 1)))
    pb1 = np.ascontiguousarray(np.tile(pb1v.T, (2, 1))); pb2 = np.ascontiguousarray(np.tile(pb2v.T, (2, 1)))
    pb2r = np.ascontiguousarray(np.tile(pb2v[1], 2).reshape(1, 128))
    nsa_c = [_nsa_consts(p_) for p_ in range(2)]
    smp_c = _sample_consts()
    pool_cmp = np.asarray(inp["cache_cmp_kv"], f32)[0].reshape(5120 * 128, 256)
    pool_sel = np.asarray(inp["cache_sel_kv"], f32)[0].reshape(5120 * 128 * 2, 128)
    page_table = np.asarray(inp["page_table"]).astype(np.int32)
    w_out = np.ascontiguousarray(np.asarray(inp["w_out"], f32)[0])
    lnrows = np.ascontiguousarray(np.stack([np.asarray(inp[n_], f32)[0] for n_ in ("ln1_g", "ln1_b", "ln2_g", "ln2_b")], 0))
    w_router = np.ascontiguousarray(np.asarray(inp["w_router"], f32)[0])
    b_router = np.ascontiguousarray(np.asarray(inp["b_router"], f32)[0].reshape(1, 32))
    w_gu = np.ascontiguousarray(np.asarray(inp["w_gate_up"], f32)[0])
    b_guT = np.ascontiguousarray(np.asarray(inp["b_gate_up"], f32)[0].reshape(32, 16, 128).transpose(2, 0, 1))
    w_dn = np.ascontiguousarray(np.asarray(inp["w_down"], f32)[0])
    b_dn = np.ascontiguousarray(np.asarray(inp["b_down"], f32)[0])
    triu = np.ascontiguousarray(np.triu(np.ones((128, 128), f32), 1))
    eoff = (np.arange(32, dtype=f32) * 384.0).reshape(1, 32)

    if "prog" not in _CACHE:
        _CACHE["prog"] = build_program()
    nc, kb = _CACHE["prog"]

    in_maps = []
    for c in range(8):
        b = c // 2
        cv = np.concatenate([c_prompt[b:b + 1], c_sample[4 * c:4 * c + 4]], 0)
        cT = np.ascontiguousarray(cv.T.reshape(8, 128, 5).transpose(1, 0, 2))
        in_maps.append(dict(
            xp=np.ascontiguousarray(x_prompt[b]),
            xs=np.ascontiguousarray(x_sample[4 * c:4 * c + 4, 0, :]),
            cT=cT, w_ada=w_ada, b_adaT=b_adaT, b_ada_row=b_ada_row, w_in=w_in_p, ident=ident,
            state_win=np.ascontiguousarray(state_win[4 * c:4 * c + 4]),
            xo=np.ascontiguousarray(x_prompt[b].reshape(16, 2, 128, D)[:, c % 2].reshape(2048, D)),
            parv=np.full((128, 1), float(c % 2), f32),
            lamc=lamc, lamr=lamr, btp=btp, ctp=ctp, dskc=dskc, w_glu=w_glu, b_glu=b_glu,
            h0c=np.ascontiguousarray(np.stack([h0_layout(st_re[4 * c:4 * c + 4]), h0_layout(st_im[4 * c:4 * c + 4])], 0)),
            rel_bias=rel_bias, oh=nsa_c[c % 2][0], jflip=jflip, selc=nsa_c[c % 2][1],
            bd1=bd1, bd2=bd2, pel=pel, pb1=pb1, pb2=pb2, pb2r=pb2r,
            w_out=w_out, lnrows=lnrows, w_router=w_router, b_router=b_router, w_gate_up=w_gu, b_guT=b_guT,
            w_down=w_dn, b_down=b_dn,
            pool_cmp=pool_cmp, pool_sel=pool_sel, pt=np.ascontiguousarray(page_table[4 * c:4 * c + 4].reshape(1, 512)),
            **smp_c,
        ))
    res = run_bass_kernel_spmd(nc, in_maps, core_ids=list(range(8)))
    R = res.results
    _CACHE["last"] = R

    def g(c, name):
        return np.asarray(R[c][name], f32)

    kvs = (1, 4, T, 2, 2, 64)
    y_prompt = np.zeros((4, 16, 2, 128, D), f32)
    for c in range(8):
        y_prompt[c // 2, :, c % 2] = g(c, "o_y_p").reshape(16, 128, D)
    y_prompt = y_prompt.reshape(4, T, D)
    y_sample = np.concatenate([g(c, "o_y_s") for c in range(8)]).reshape(32, 1, D)
    cmp_p = np.stack([g(2 * b, "o_cmp_p") for b in range(4)]).reshape(kvs)
    sel_p = np.stack([g(2 * b, "o_sel_p") for b in range(4)]).reshape(kvs)
    win_p = np.stack([g(2 * b, "o_win_p")[T - 512:] for b in range(4)]).reshape(1, 4, 512, 2, 2, 64)
    cmp_s = np.concatenate([g(c, "o_cmp_s") for c in range(8)]).reshape(1, 32, 1, 2, 2, 64)
    sel_s = np.concatenate([g(c, "o_sel_s") for c in range(8)]).reshape(1, 32, 1, 2, 2, 64)
    win_s = np.concatenate([g(c, "o_win_s") for c in range(8)]).reshape(1, 32, 512, 2, 2, 64)
    def from_col(a):
        return a.reshape(2, 64, 16).transpose(2, 0, 1).reshape(32, 64)
    re_p = np.stack([from_col(g(2 * b, "o_ssm_p")[0]) for b in range(4)])[None]
    im_p = np.stack([from_col(g(2 * b, "o_ssm_p")[1]) for b in range(4)])[None]
    def from_col_s(a):
        return a.reshape(2, 64, 16, 4).transpose(3, 2, 0, 1).reshape(4, 32, 64)
    re_s = np.concatenate([from_col_s(g(c, "o_ssm_s")[0]) for c in range(8)])[None]
    im_s = np.concatenate([from_col_s(g(c, "o_ssm_s")[1]) for c in range(8)])[None]
    return (y_prompt, y_sample, cmp_p, cmp_s, sel_p, sel_s, win_p, win_s, re_p, im_p, re_s, im_s)
```
